# Optimizing a Trainium2 kernel written in Bass

```python
import jax, jax.numpy as jnp
from jax import lax
import numpy as np

D_MODEL = 1024
BATCH = 16
SEQ = 2048
DEPTH = 2

CHUNK = 64
N_EVEN = (DEPTH + 1) // 2
N_ODD = DEPTH // 2
D_A = 512
CONV_A = 31
D_B = 512
CONV_B = 3
D_C = 512
HEADS_C = 8
SGU_BLOCK = 128
D_D = 512
HEADS_D = 8
HEAD_DIM_D = D_D // HEADS_D
LEFT_CHUNKS = 8
BAND_LEN = (LEFT_CHUNKS + 1) * CHUNK
MAX_REL = 256
W_IN_AB = 2 * D_A + 3 * D_B
W_IN_CD = 2 * D_C + 3 * D_D
N_GROUPS = 4
EXPERTS_PER_GROUP = 8
N_EXPERTS = N_GROUPS * EXPERTS_PER_GROUP
TOP_K = 2
D_EXPERT = 512
EXPERT_BLOCK = 256
ALPHA = (2 * DEPTH) ** 0.25
BETA = (8 * DEPTH) ** -0.25
LN_EPS = 1e-5
NEG_INF = -1e30

kernel_name = "chunk_causal_hybrid_conv_sgu_attn_hmoe"


def layer_norm(x, g, b):
    xf = x.astype(jnp.float32)
    mu = jnp.mean(xf, axis=-1, keepdims=True)
    var = jnp.mean(jnp.square(xf - mu), axis=-1, keepdims=True)
    return ((xf - mu) * lax.rsqrt(var + LN_EPS)).astype(x.dtype) * g + b


def causal_dwconv(x, w):
    k, c = w.shape
    return lax.conv_general_dilated(
        x, w[:, None, :].astype(x.dtype), window_strides=(1,), padding=[(k - 1, 0)],
        dimension_numbers=("NWC", "WIO", "NWC"), feature_group_count=c)


def conv_shortconv_mixer(x, w_in, b_in, a_dw, a_dw_b, a_ln_g, a_ln_b, b_dw, w_out):
    h = x @ w_in + b_in
    a_val, a_gate, g_b, g_c, b_h = jnp.split(
        h, [D_A, 2 * D_A, 2 * D_A + D_B, 2 * D_A + 2 * D_B], axis=-1)
    a = a_val * jax.nn.sigmoid(a_gate)
    a = causal_dwconv(a, a_dw) + a_dw_b
    a = jax.nn.silu(layer_norm(a, a_ln_g, a_ln_b))
    s = g_b * causal_dwconv(g_c * b_h, b_dw)
    return jnp.concatenate([a, s], axis=-1) @ w_out


def sgu_chunk_attention_mixer(x, w_in, b_in, c_ln_g, c_ln_b, c_ws, c_ws_b, d_rel_bias, w_out):
    bt, s_len, _ = x.shape
    h = x @ w_in + b_in
    u, v, q, k, val = jnp.split(
        h, [D_C, 2 * D_C, 2 * D_C + D_D, 2 * D_C + 2 * D_D], axis=-1)

    v = layer_norm(v, c_ln_g, c_ln_b)
    nb = s_len // SGU_BLOCK
    v = v.reshape(bt, nb, SGU_BLOCK, HEADS_C, D_C // HEADS_C)
    pos = jnp.arange(SGU_BLOCK)
    sgu_mask = (pos[None, :] // CHUNK) <= (pos[:, None] // CHUNK)
    ws = jnp.where(sgu_mask[None], c_ws, jnp.zeros((), c_ws.dtype))
    gate = jnp.einsum("hij,bnjhc->bnihc", ws, v) + c_ws_b.T[None, None, :, :, None]
    c_out = u * gate.reshape(bt, s_len, D_C)

    n_chunks = s_len // CHUNK
    q = (q * (HEAD_DIM_D ** -0.5)).reshape(bt, n_chunks, CHUNK, HEADS_D, HEAD_DIM_D)
    q = jnp.transpose(q, (1, 0, 2, 3, 4))
    pad = ((0, 0), (LEFT_CHUNKS * CHUNK, 0), (0, 0), (0, 0))
    kp = jnp.pad(k.reshape(bt, s_len, HEADS_D, HEAD_DIM_D), pad)
    vp = jnp.pad(val.reshape(bt, s_len, HEADS_D, HEAD_DIM_D), pad)
    qi = jnp.arange(CHUNK)[:, None]
    kj = jnp.arange(BAND_LEN)[None, :]
    rel_idx = jnp.clip(qi + LEFT_CHUNKS * CHUNK - kj, -MAX_REL, MAX_REL) + MAX_REL
    rel = d_rel_bias[:, rel_idx].astype(jnp.float32)

    def one_chunk(args):
        n, qn = args
        kb = lax.dynamic_slice_in_dim(kp, n * CHUNK, BAND_LEN, axis=1)
        vb = lax.dynamic_slice_in_dim(vp, n * CHUNK, BAND_LEN, axis=1)
        sc = jnp.einsum("bqhd,bkhd->bhqk", qn, kb, preferred_element_type=jnp.float32) + rel
        key_ok = (n * CHUNK + jnp.arange(BAND_LEN)) >= LEFT_CHUNKS * CHUNK
        sc = jnp.where(key_ok[None, None, None, :], sc, NEG_INF)
        p = jax.nn.softmax(sc, axis=-1).astype(vb.dtype)
        return jnp.einsum("bhqk,bkhd->bqhd", p, vb)

    o = lax.map(one_chunk, (jnp.arange(n_chunks), q))
    o = jnp.transpose(o, (1, 0, 2, 3, 4)).reshape(bt, s_len, D_D)
    return jnp.concatenate([c_out, o], axis=-1) @ w_out


def grouped_expert_ffn(xt, expert_idx, w_gate, w_up, w_down):
    n_tok, k_sel = expert_idx.shape
    m = n_tok * k_sel
    d = xt.shape[-1]
    n_exp = w_gate.shape[0]
    flat_e = expert_idx.reshape(-1)
    order = jnp.argsort(flat_e)
    sorted_e = flat_e[order]
    counts = jnp.bincount(flat_e, length=n_exp)
    padded = ((counts + EXPERT_BLOCK - 1) // EXPERT_BLOCK) * EXPERT_BLOCK
    starts = jnp.cumsum(counts) - counts
    pad_ends = jnp.cumsum(padded)
    pad_starts = pad_ends - padded
    dest = pad_starts[sorted_e] + (jnp.arange(m) - starts[sorted_e])
    n_blocks = -(-m // EXPERT_BLOCK) + n_exp
    xp = jnp.zeros((n_blocks * EXPERT_BLOCK, d), xt.dtype).at[dest].set(xt[order // k_sel])
    block_e = jnp.searchsorted(pad_ends, jnp.arange(n_blocks) * EXPERT_BLOCK, side="right")
    block_e = jnp.minimum(block_e, n_exp - 1)

    def one_block(args):
        xb, e = args
        hb = jax.nn.silu(xb @ w_gate[e]) * (xb @ w_up[e])
        return hb @ w_down[e]

    yp = lax.map(one_block, (xp.reshape(n_blocks, EXPERT_BLOCK, d), block_e))
    y_sorted = yp.reshape(n_blocks * EXPERT_BLOCK, d)[dest]
    y = jnp.zeros((m, d), y_sorted.dtype).at[order].set(y_sorted)
    return y.reshape(n_tok, k_sel, d)


def hierarchical_moe(x, rg_w, rg_b, re_w, re_b, w_gate, w_up, w_down):
    bt, s_len, d = x.shape
    xt = x.reshape(-1, d)
    g_prob = jax.nn.softmax((xt @ rg_w + rg_b).astype(jnp.float32), axis=-1)
    g_top, g_idx = lax.top_k(g_prob, 1)
    e_all = (jnp.einsum("nd,gde->nge", xt, re_w) + re_b).astype(jnp.float32)
    e_logits = jnp.take_along_axis(e_all, g_idx[:, :, None], axis=1)[:, 0]
    e_top, e_idx = lax.top_k(jax.nn.softmax(e_logits, axis=-1), TOP_K)
    e_w = e_top / jnp.sum(e_top, axis=-1, keepdims=True)
    weights = (g_top * e_w).astype(x.dtype)
    expert_idx = g_idx * EXPERTS_PER_GROUP + e_idx
    y = grouped_expert_ffn(xt, expert_idx, w_gate, w_up, w_down)
    return jnp.einsum("nk,nkd->nd", weights, y).reshape(bt, s_len, d)


def setup_inputs(seed: int = 0) -> dict:
    key = jax.random.key(seed)
    ks = iter(jax.random.split(key, 32))

    def nrm(shape, scale):
        return jax.random.normal(next(ks), shape, jnp.float32) * scale

    return {
        "x": nrm((BATCH, SEQ, D_MODEL), 1.0),
        "ab_w_in": nrm((N_EVEN, D_MODEL, W_IN_AB), D_MODEL ** -0.5),
        "ab_b_in": nrm((N_EVEN, W_IN_AB), 0.02),
        "a_dw": nrm((N_EVEN, CONV_A, D_A), CONV_A ** -0.5),
        "a_dw_b": nrm((N_EVEN, D_A), 0.02),
        "a_ln_g": 1.0 + nrm((N_EVEN, D_A), 0.02),
        "a_ln_b": nrm((N_EVEN, D_A), 0.02),
        "b_dw": nrm((N_EVEN, CONV_B, D_B), CONV_B ** -0.5),
        "ab_w_out": nrm((N_EVEN, D_A + D_B, D_MODEL), BETA * (D_A + D_B) ** -0.5),
        "cd_w_in": nrm((N_ODD, D_MODEL, W_IN_CD), D_MODEL ** -0.5),
        "cd_b_in": nrm((N_ODD, W_IN_CD), 0.02),
        "c_ln_g": 1.0 + nrm((N_ODD, D_C), 0.02),
        "c_ln_b": nrm((N_ODD, D_C), 0.02),
        "c_ws": nrm((N_ODD, HEADS_C, SGU_BLOCK, SGU_BLOCK), SGU_BLOCK ** -0.5),
        "c_ws_b": 1.0 + nrm((N_ODD, HEADS_C, SGU_BLOCK), 0.02),
        "d_rel_bias": nrm((N_ODD, HEADS_D, 2 * MAX_REL + 1), 0.1),
        "cd_w_out": nrm((N_ODD, D_C + D_D, D_MODEL), BETA * (D_C + D_D) ** -0.5),
        "mix_ln_g": 1.0 + nrm((DEPTH, D_MODEL), 0.02),
        "mix_ln_b": nrm((DEPTH, D_MODEL), 0.02),
        "moe_rg_w": nrm((DEPTH, D_MODEL, N_GROUPS), D_MODEL ** -0.5),
        "moe_rg_b": nrm((DEPTH, N_GROUPS), 0.01),
        "moe_re_w": nrm((DEPTH, N_GROUPS, D_MODEL, EXPERTS_PER_GROUP), D_MODEL ** -0.5),
        "moe_re_b": nrm((DEPTH, N_GROUPS, EXPERTS_PER_GROUP), 0.01),
        "moe_w_gate": nrm((DEPTH, N_EXPERTS, D_MODEL, D_EXPERT), D_MODEL ** -0.5),
        "moe_w_up": nrm((DEPTH, N_EXPERTS, D_MODEL, D_EXPERT), D_MODEL ** -0.5),
        "moe_w_down": nrm((DEPTH, N_EXPERTS, D_EXPERT, D_MODEL), BETA * D_EXPERT ** -0.5),
        "ffn_ln_g": 1.0 + nrm((DEPTH, D_MODEL), 0.02),
        "ffn_ln_b": nrm((DEPTH, D_MODEL), 0.02),
    }


def reference(x, ab_w_in, ab_b_in, a_dw, a_dw_b, a_ln_g, a_ln_b, b_dw, ab_w_out,
              cd_w_in, cd_b_in, c_ln_g, c_ln_b, c_ws, c_ws_b, d_rel_bias, cd_w_out,
              mix_ln_g, mix_ln_b, moe_rg_w, moe_rg_b, moe_re_w, moe_re_b,
              moe_w_gate, moe_w_up, moe_w_down, ffn_ln_g, ffn_ln_b):
    for layer in range(DEPTH):
        i = layer // 2
        if layer % 2 == 0:
            mix = conv_shortconv_mixer(x, ab_w_in[i], ab_b_in[i], a_dw[i], a_dw_b[i],
                                       a_ln_g[i], a_ln_b[i], b_dw[i], ab_w_out[i])
        else:
            mix = sgu_chunk_attention_mixer(x, cd_w_in[i], cd_b_in[i], c_ln_g[i], c_ln_b[i],
                                            c_ws[i], c_ws_b[i], d_rel_bias[i], cd_w_out[i])
        x = layer_norm(ALPHA * x + mix, mix_ln_g[layer], mix_ln_b[layer])
        ffn = hierarchical_moe(x, moe_rg_w[layer], moe_rg_b[layer], moe_re_w[layer],
                               moe_re_b[layer], moe_w_gate[layer], moe_w_up[layer],
                               moe_w_down[layer])
        x = layer_norm(ALPHA * x + ffn, ffn_ln_g[layer], ffn_ln_b[layer])
    return x
```

```python
import numpy as np
from contextlib import ExitStack
import concourse.bass as bass
import concourse.mybir as mybir
from concourse.bass_utils import run_bass_kernel_spmd

F32 = mybir.dt.float32
BF16 = mybir.dt.bfloat16
I32 = mybir.dt.int32
AF = mybir.ActivationFunctionType
ALU = mybir.AluOpType
AX = mybir.AxisListType

NCORES = 8
D = 1024
SEQ = 2048
BPC = 2
T = BPC * SEQ
NT = T // 128
BLK = 512
NBLK = T // BLK
NE = 32
CAP = 384
NSLOT = NE * CAP
ALPHA = float(4 ** 0.25)
EPS = 1e-5
DEX = 512
import os
SKIP = set(os.environ.get("M1_SKIP", "").split(","))
ATT_T = os.environ.get("ATT_T", "1") == "1"


class Buf:
    __slots__ = ("name", "xw", "cw", "readers")

    def __init__(self, name=""):
        self.name = name
        self.xw = []
        self.cw = []
        self.readers = []

    def reset(self):
        self.xw = []
        self.cw = []
        self.readers = []


class Op:
    __slots__ = ("eng", "fn", "reads", "writes", "cwrites", "key", "deps", "signal", "seq", "kcount", "idx")

    def __init__(self, eng, fn, reads, writes, cwrites, key):
        self.eng = eng
        self.fn = fn
        self.reads = reads
        self.writes = writes
        self.cwrites = cwrites
        self.key = key
        self.deps = None
        self.signal = False
        self.seq = None
        self.kcount = None


class Ring:
    def __init__(self, items):
        self.items = items
        self.i = 0

    def next(self):
        it = self.items[self.i % len(self.items)]
        self.i += 1
        return it


class Prog:
    ENGS = ("pe", "act", "dve", "pool", "sp")

    def __init__(self, nc):
        self.nc = nc
        self.gstack = ExitStack()
        self.pstack = None
        self.ops = []
        self.bufs = []
        self.sems = {}
        self.eng_seq = {e: 0 for e in self.ENGS}
        self.key_count = {}
        self.phase_no = 0
        self.total_ops = 0
        self.uid = 0
        self.pending = None
        self._in_chain = False

    def sb(self, shape, dtype, name=None, glob=False):
        self.uid += 1
        st = self.gstack if glob else self.pstack
        return st.enter_context(self.nc.sbuf_tensor("%s_%d" % (name or "t", self.uid), list(shape), dtype))

    def ps(self, shape, dtype=F32, name=None):
        self.uid += 1
        return self.pstack.enter_context(self.nc.psum_tensor("%s_%d" % (name or "p", self.uid), list(shape), dtype))

    def buf(self, name=""):
        b = Buf(name)
        self.bufs.append(b)
        return b

    def sbb(self, shape, dtype, name=None, glob=False):
        return self.sb(shape, dtype, name, glob), self.buf(name or "")

    def psb(self, shape, dtype=F32, name=None):
        return self.ps(shape, dtype, name), self.buf(name or "")

    def sem(self, name):
        if name not in self.sems:
            self.sems[name] = self.gstack.enter_context(self.nc.semaphore(name))
        return self.sems[name]

    def op(self, eng, fn, reads=(), writes=(), cwrites=(), key=None):
        o = Op(eng, fn, tuple(reads), tuple(writes), tuple(cwrites), key)
        o.idx = len(self.ops)
        self.ops.append(o)
        if self.pending is not None and not self._in_chain and eng == "dve":
            self._in_chain = True
            try:
                next(self.pending)
            except StopIteration:
                self.pending = None
            self._in_chain = False
        return o

    def drain(self):
        if self.pending is not None:
            self._in_chain = True
            for _ in self.pending:
                pass
            self._in_chain = False
            self.pending = None

    def dma(self, eng, out, in_, reads=(), writes=(), cwrites=(), key=None, **kw):
        assert key is not None
        return self.op(eng, lambda e: e.dma_start(out=out, in_=in_, **kw), reads, writes, cwrites, key)

    def bound_reg(self, eng):
        if self._breg is None:
            self._breg = eng.to_reg(NSLOT - 1)
        return self._breg

    def begin_phase(self):
        self.pstack = ExitStack()
        self._breg = None
        self.ops = []
        for b in self.bufs:
            b.reset()

    def end_phase(self):
        self.drain()
        nc = self.nc
        ops = self.ops
        for o in ops:
            deps = set()
            for b in o.reads:
                deps.update(b.xw)
                deps.update(b.cw)
            for b in o.writes:
                deps.update(b.xw)
                deps.update(b.cw)
                deps.update(b.readers)
            for b in o.cwrites:
                deps.update(b.xw)
                deps.update(b.readers)
            deps.discard(o.idx)
            o.deps = deps
            for b in o.reads:
                b.readers.append(o.idx)
            for b in o.writes:
                b.xw = [o.idx]
                b.cw = []
                b.readers = []
            for b in o.cwrites:
                b.cw.append(o.idx)
        for o in ops:
            latest = {}
            ddeps = []
            for d in o.deps:
                p = ops[d]
                if p.key is not None:
                    ddeps.append(p)
                elif not (p.eng == "pe" and o.eng == "pe"):
                    q = latest.get(p.eng)
                    if q is None or q.idx < p.idx:
                        latest[p.eng] = p
            for p in latest.values():
                p.signal = True
            o.deps = ddeps + list(latest.values())
        last_compute = {}
        for o in ops:
            if o.key is None:
                last_compute[o.eng] = o
        for o in last_compute.values():
            o.signal = True
        for o in ops:
            if o.key is None and o.signal:
                self.eng_seq[o.eng] += 1
                o.seq = self.eng_seq[o.eng]
        waits = [None] * len(ops)
        kc = self.key_count
        keys_used = set()
        for o in ops:
            w = {}
            for p in o.deps:
                if p.key is not None:
                    nm = "k_" + p.key
                    v = 16 * kc[p.key]
                else:
                    if not p.signal:
                        continue
                    nm = "e_" + p.eng
                    v = p.seq
                if w.get(nm, 0) < v:
                    w[nm] = v
            waits[o.idx] = w
            if o.key is not None:
                kc[o.key] = kc.get(o.key, 0) + 1
                keys_used.add(o.key)
        for o in ops:
            for nm in waits[o.idx]:
                self.sem(nm)
            if o.key is not None:
                self.sem("k_" + o.key)
            elif o.signal:
                self.sem("e_" + o.eng)
        fin = {}
        for k in sorted(keys_used):
            fin["k_" + k] = 16 * kc[k]
        for e, o in last_compute.items():
            fin["e_" + e] = o.seq
        sems = self.sems
        by_eng = {e: [o for o in ops if o.eng == e] for e in self.ENGS}

        def run(ename, eng):
            waited = {}
            for o in by_eng[ename]:
                for nm, v in waits[o.idx].items():
                    if waited.get(nm, 0) >= v:
                        continue
                    waited[nm] = v
                    eng.wait_ge(sems[nm], v)
                ins = o.fn(eng)
                if o.key is not None:
                    ins.then_inc(sems["k_" + o.key], 16)
                elif o.signal:
                    ins.then_inc(sems["e_" + o.eng], 1)
            for nm, v in fin.items():
                if nm == "e_" + ename:
                    continue
                eng.wait_ge(sems[nm], v)
            if ename == "pool" and self._breg is not None:
                eng.free_register(self._breg)
                self._breg = None

        with nc.Block() as block:
            @block.tensor
            def _(e):
                run("pe", e)

            @block.scalar
            def _(e):
                run("act", e)

            @block.vector
            def _(e):
                run("dve", e)

            @block.gpsimd
            def _(e):
                run("pool", e)

            @block.sync
            def _(e):
                run("sp", e)
        self.total_ops += len(ops)
        self.pstack.close()
        self.pstack = None
        self.phase_no += 1

    def finish(self):
        self.gstack.close()


def mm(P, out, lhsT, rhs, start, stop, reads, pbuf):
    if start:
        P.op("pe", lambda e: e.matmul(out=out, lhsT=lhsT, rhs=rhs, start=True, stop=stop), reads=reads, writes=[pbuf])
    else:
        P.op("pe", lambda e: e.matmul(out=out, lhsT=lhsT, rhs=rhs, start=False, stop=stop), reads=reads, cwrites=[pbuf])


def tr(P, out, in_, ident, first, reads, pbuf):
    if first:
        P.op("pe", lambda e: e.transpose(out=out, in_=in_, identity=ident), reads=reads, writes=[pbuf])
    else:
        P.op("pe", lambda e: e.transpose(out=out, in_=in_, identity=ident), reads=reads, cwrites=[pbuf])


def act(P, out, in_, func, reads, writes, bias=None, scale=None, cwrites=(), accum_out=None):
    kw = {}
    if bias is not None:
        kw["bias"] = bias
    if scale is not None:
        kw["scale"] = scale
    if accum_out is not None:
        kw["accum_out"] = accum_out
    P.op("act", lambda e: e.activation(out=out, in_=in_, func=func, **kw), reads=reads, writes=writes, cwrites=cwrites)


def ts(P, eng, out, in0, s1, s2, op0, op1, reads, writes, cwrites=()):
    if s2 is None:
        P.op(eng, lambda e: e.tensor_scalar(out=out, in0=in0, scalar1=s1, scalar2=None, op0=op0), reads=reads, writes=writes, cwrites=cwrites)
    else:
        P.op(eng, lambda e: e.tensor_scalar(out=out, in0=in0, scalar1=s1, scalar2=s2, op0=op0, op1=op1), reads=reads, writes=writes, cwrites=cwrites)


def tt(P, eng, out, in0, in1, op, reads, writes, cwrites=()):
    P.op(eng, lambda e: e.tensor_tensor(out=out, in0=in0, in1=in1, op=op), reads=reads, writes=writes, cwrites=cwrites)


def stt(P, out, in0, scalar, in1, op0, op1, reads, writes, cwrites=()):
    P.op("dve", lambda e: e.scalar_tensor_tensor(out=out, in0=in0, scalar=scalar, in1=in1, op0=op0, op1=op1),
         reads=reads, writes=writes, cwrites=cwrites)


def cp(P, eng, out, in_, reads, writes, cwrites=()):
    if eng == "act":
        P.op("act", lambda e: e.activation(out=out, in_=in_, func=AF.Copy), reads=reads, writes=writes, cwrites=cwrites)
    else:
        P.op(eng, lambda e: e.tensor_copy(out=out, in_=in_), reads=reads, writes=writes, cwrites=cwrites)


class G:
    pass


def setup_globals(P, g):
    g.ident_f, g.b_ident_f = P.sbb([128, 128], F32, "identf", glob=True)
    g.ident_b, g.b_ident_b = P.sbb([128, 128], BF16, "identb", glob=True)
    g.U_b, g.b_U = P.sbb([128, 128], BF16, "U", glob=True)
    g.ones_b, g.b_ones = P.sbb([128, 128], BF16, "ones", glob=True)
    g.eC, g.b_eC = P.sbb([128, NE], F32, "eC", glob=True)
    g.runc, g.b_runc = P.sbb([128, NE], F32, "runc", glob=True)
    g.posi, g.b_posi = P.sbb([128, NT, 2], I32, "posi", glob=True)
    g.wts, g.b_wts = P.sbb([128, NT, 2], F32, "wts", glob=True)
    g.eCi, g.b_eCi = P.sbb([128, NE], I32, "eCi", glob=True)


def emit_const_init(P, g):
    P.op("pool", lambda e: e.memset(g.ident_f[:], 0.0), writes=[g.b_ident_f])
    P.op("pool", lambda e: e.affine_select(out=g.ident_f[:], in_=g.ident_f[:], pattern=[[-1, 128]],
                                           compare_op=ALU.not_equal, fill=1.0, base=0, channel_multiplier=1),
         writes=[g.b_ident_f])
    cp(P, "pool", g.ident_b[:], g.ident_f[:], [g.b_ident_f], [g.b_ident_b])
    P.op("pool", lambda e: e.memset(g.ones_b[:], 1.0), writes=[g.b_ones])
    P.op("pool", lambda e: e.affine_select(out=g.U_b[:], in_=g.ones_b[:], pattern=[[1, 128]],
                                           compare_op=ALU.is_gt, fill=0.0, base=0, channel_multiplier=-1),
         reads=[g.b_ones], writes=[g.b_U])
    P.op("pool", lambda e: e.iota(g.eCi[:], pattern=[[CAP, NE]], base=0, channel_multiplier=0), writes=[g.b_eCi])
    cp(P, "pool", g.eC[:], g.eCi[:], [g.b_eCi], [g.b_eC])


def emit_zero_fill(P, g, dr):
    zt, b_zt = P.sbb([128, D], BF16, "zt")
    P.op("pool", lambda e: e.memset(zt[:], 0.0), writes=[b_zt])
    Xv = dr["Xs"].rearrange("(n p) d -> p n d", p=128)
    for n in range(NSLOT // 128):
        P.dma("sp", Xv[:, n, :], zt[:], reads=[b_zt], writes=[g.b_Xs] if n == 0 else (), cwrites=() if n == 0 else [g.b_Xs], key="zf")


def alloc_tail(P, g, L):
    t = G()
    t.bc, t.b_bc = P.sbb([128, 2 * D + 36], F32, "bc")
    t.wr, t.b_wr = P.sbb([128, 8, 36], F32, "wr")
    t.x1b = P.sb([128, 4, D], BF16, "x1b")
    t.b_x1b = [P.buf("x1b%d" % i) for i in range(4)]
    t.x1T = Ring([P.sbb([128, 8, 128], F32, "x1T") for _ in range(1)])
    t.lnr = Ring([make_ln_scratch(P) for _ in range(4)])
    t.sm, t.b_sm = P.sbb([128, 928], F32, "small")
    t.oh, t.b_oh = P.sbb([128, 4, NE], BF16, "oh")
    t.ptr = Ring([P.psb([128, 512], F32, "ptrT") for _ in range(2)])
    t.psm, t.b_psm = P.psb([128, 512], F32, "psm")
    return t


def load_tail_consts(P, g, t, dr, L):
    P.dma("sp", t.bc[:], dr["bcm%d" % L][:, :], writes=[t.b_bc], key="cst")
    P.dma("sp", t.wr[:], dr["wr%d" % L].rearrange("(c p) n -> p c n", p=128), writes=[t.b_wr], key="cst")


def make_ln_scratch(P):
    sc = G()
    sc.st, sc.b_st = P.sbb([128, 2, 6], F32, "bnst")
    sc.mv, sc.b_mv = P.sbb([128, 2], F32, "mv")
    sc.rstd, sc.b_rstd = P.sbb([128, 1], F32, "rstd")
    sc.nmr, sc.b_nmr = P.sbb([128, 1], F32, "nmr")
    return sc


def emit_ln_rows(P, t, src, b_src, dst, b_dst, goff, boff):
    sc = t.lnr.next()
    for h in range(2):
        P.op("dve", lambda e, h=h: e.bn_stats(out=sc.st[:, h, :], in_=src[:, h * 512:(h + 1) * 512]),
             reads=[b_src], writes=[sc.b_st] if h == 0 else (), cwrites=() if h == 0 else [sc.b_st])
    P.op("dve", lambda e: e.bn_aggr(out=sc.mv[:], in_=sc.st[:].rearrange("p a b -> p (a b)")), reads=[sc.b_st], writes=[sc.b_mv])
    act(P, sc.rstd[:], sc.mv[:, 1:2], AF.Sqrt, [sc.b_mv], [sc.b_rstd], bias=EPS)
    P.op("dve", lambda e: e.reciprocal(out=sc.rstd[:], in_=sc.rstd[:]), reads=[sc.b_rstd], writes=[sc.b_rstd])
    stt(P, sc.nmr[:], sc.mv[:, 0:1], -1.0, sc.rstd[:, 0:1], ALU.mult, ALU.mult, [sc.b_mv, sc.b_rstd], [sc.b_nmr])
    act(P, dst, src, AF.Identity, [b_src, sc.b_rstd, sc.b_nmr], [b_dst], bias=sc.nmr[:, 0:1], scale=sc.rstd[:, 0:1])
    tt(P, "dve", dst, dst, t.bc[:, goff:goff + D], ALU.mult, [b_dst, t.b_bc], [b_dst])
    tt(P, "pool", dst, dst, t.bc[:, boff:boff + D], ALU.add, [b_dst, t.b_bc], [b_dst])


def emit_tail_block(P, g, t, xb, b_xt, catT, b_cat, wout, b_wout, pmm, gb, dr):
    sm = t.sm
    S = [t.b_sm]
    for i in range(4):
        tile = gb * 4 + i
        xi = xb[:, i, :]
        for hf in range(2):
            pm, b_pm = pmm.next()
            for c in range(8):
                mm(P, pm[:], catT[:, c, i * 128:(i + 1) * 128], wout[:, c, hf * 512:(hf + 1) * 512], c == 0, c == 7,
                   [b_cat[c], b_wout], b_pm)
            stt(P, xb[:, i, hf * 512:(hf + 1) * 512], xb[:, i, hf * 512:(hf + 1) * 512], ALPHA, pm[:], ALU.mult, ALU.add,
                [b_xt[i], b_pm], [b_xt[i]])
        emit_ln_rows(P, t, xi, b_xt[i], xi, b_xt[i], 0, D)
        P.dma("sp", dr["X1"][tile * 128:(tile + 1) * 128, :], xi, reads=[b_xt[i]], cwrites=[g.b_X1], key="x1st%d" % i)
    P.drain()
    for i in range(4):
        cp(P, "act", t.x1b[:, i, :], xb[:, i, :], [b_xt[i]], [t.b_x1b[i]])
    for i in range(4):
        x1T, b_x1T = t.x1T.next()
        for half in range(2):
            pt, b_pt = t.ptr.next()
            for cc in range(4):
                c = half * 4 + cc
                tr(P, pt[:, cc * 128:(cc + 1) * 128], xb[:, i, c * 128:(c + 1) * 128], g.ident_f[:], cc == 0,
                   [b_xt[i], g.b_ident_f], b_pt)
            cp(P, "act" if half == 0 else "dve", x1T[:, half * 4:(half + 1) * 4, :],
               pt[:].rearrange("p (c n) -> p c n", c=4), [b_pt], [b_x1T] if half == 0 else (),
               cwrites=() if half == 0 else [b_x1T])
        for c in range(8):
            o_ = t.psm[:, i * 36:(i + 1) * 36]
            if i == 0 and c == 0:
                P.op("pe", lambda e, o_=o_, l=x1T[:, c, :], r=t.wr[:, c, :]: e.matmul(out=o_, lhsT=l, rhs=r, start=True, stop=False),
                     reads=[b_x1T, t.b_wr], writes=[t.b_psm])
            else:
                P.op("pe", lambda e, o_=o_, l=x1T[:, c, :], r=t.wr[:, c, :], st=(c == 0), sp=(c == 7):
                     e.matmul(out=o_, lhsT=l, rhs=r, start=st, stop=sp), reads=[b_x1T, t.b_wr], cwrites=[t.b_psm])
    P.pending = router_chain(P, g, t, gb, dr)


def router_chain(P, g, t, gb, dr):
    sm = t.sm
    S = [t.b_sm]
    LG = sm[:, 0:144].rearrange("p (a b) -> p a b", a=4)
    gmax = sm[:, 144:148]
    gd = sm[:, 148:164].rearrange("p (a b) -> p a b", a=4)
    gsel = sm[:, 164:180].rearrange("p (a b) -> p a b", a=4)
    gexp = sm[:, 180:196].rearrange("p (a b) -> p a b", a=4)
    gsum = sm[:, 196:200]
    gtop = sm[:, 200:204]
    pen = sm[:, 204:220].rearrange("p (a b) -> p a b", a=4)
    masked = sm[:, 220:348]
    top8 = sm[:, 348:380].rearrange("p (a b) -> p a b", a=4)
    oh1 = sm[:, 380:508].rearrange("p (a b) -> p a b", a=4)
    oh2 = sm[:, 508:636].rearrange("p (a b) -> p a b", a=4)
    dd = sm[:, 636:640]
    ee = sm[:, 640:644]
    den = sm[:, 644:648]
    wtmp = sm[:, 648:656].rearrange("p (a b) -> p a b", a=4)
    rank = sm[:, 656:784].rearrange("p (a b) -> p a b", a=4)
    tmp = sm[:, 784:912].rearrange("p (a b) -> p a b", a=4)
    posf = sm[:, 912:920].rearrange("p (a b) -> p a b", a=4)
    valid = sm[:, 920:928].rearrange("p (a b) -> p a b", a=4)
    tt(P, "dve", LG, t.psm[:, 0:144].rearrange("p (a b) -> p a b", a=4),
       t.bc[:, 2 * D:2 * D + 36].unsqueeze(1).to_broadcast([128, 4, 36]), ALU.add, [t.b_psm, t.b_bc], S)
    yield
    P.op("dve", lambda e: e.tensor_reduce(out=gmax, in_=LG[:, :, 0:4], axis=AX.X, op=ALU.max), reads=S, writes=S)
    yield
    tt(P, "dve", gd, LG[:, :, 0:4], gmax.unsqueeze(2).to_broadcast([128, 4, 4]), ALU.subtract, S, S)
    yield
    ts(P, "dve", gsel, gd, 0.0, None, ALU.is_equal, None, S, S)
    yield
    act(P, gexp, gd, AF.Exp, S, S)
    yield
    P.op("dve", lambda e: e.tensor_reduce(out=gsum, in_=gexp, axis=AX.X, op=ALU.add), reads=S, writes=S)
    yield
    P.op("dve", lambda e: e.reciprocal(out=gtop, in_=gsum), reads=S, writes=S)
    yield
    ts(P, "dve", pen, gsel, 1e30, -1e30, ALU.mult, ALU.add, S, S)
    yield
    tt(P, "dve", masked.rearrange("p (a b c) -> p a b c", a=4, b=4), LG[:, :, 4:36].rearrange("p a (b c) -> p a b c", b=4),
       pen.unsqueeze(3).to_broadcast([128, 4, 4, 8]), ALU.add, S, S)
    yield
    for i in range(4):
        P.op("dve", lambda e, i=i: e.max(out=top8[:, i, :], in_=masked[:, i * 32:(i + 1) * 32]), reads=S, writes=S)
        yield
    m3 = masked.rearrange("p (a b) -> p a b", a=4)
    tt(P, "dve", oh1, m3, top8[:, :, 0:1].to_broadcast([128, 4, 32]), ALU.is_equal, S, S)
    yield
    tt(P, "dve", oh2, m3, top8[:, :, 1:2].to_broadcast([128, 4, 32]), ALU.is_equal, S, S)
    yield
    tt(P, "dve", dd, top8[:, :, 1], top8[:, :, 0], ALU.subtract, S, S)
    yield
    act(P, ee, dd, AF.Exp, S, S)
    yield
    ts(P, "dve", den, ee, 1.0, None, ALU.add, None, S, S)
    yield
    P.op("dve", lambda e: e.reciprocal(out=den, in_=den), reads=S, writes=S)
    yield
    tt(P, "dve", wtmp[:, :, 0], den, gtop, ALU.mult, S, S)
    yield
    tt(P, "dve", wtmp[:, :, 1], wtmp[:, :, 0], ee, ALU.mult, S, S)
    yield
    tt(P, "dve", t.oh[:], oh1, oh2, ALU.add, S, [t.b_oh])
    yield
    for i in range(4):
        o_ = t.psm[:, 256 + i * 32:256 + (i + 1) * 32]
        seq = [(g.U_b, i)] + [(g.ones_b, j) for j in range(i)]
        for n_, (lt, j) in enumerate(seq):
            P.op("pe", lambda e, o_=o_, l=lt[:], r=t.oh[:, j, :], st=(n_ == 0), sp=(n_ == len(seq) - 1):
                 e.matmul(out=o_, lhsT=l, rhs=r, start=st, stop=sp), reads=[g.b_U, g.b_ones, t.b_oh], cwrites=[t.b_psm])
            yield
    for j in range(4):
        P.op("pe", lambda e, j=j: e.matmul(out=t.psm[:, 448:480], lhsT=g.ones_b[:], rhs=t.oh[:, j, :], start=(j == 0), stop=(j == 3)),
             reads=[g.b_ones, t.b_oh], cwrites=[t.b_psm])
        yield
    tt(P, "dve", rank, t.psm[:, 256:384].rearrange("p (a b) -> p a b", a=4), g.runc[:].unsqueeze(1).to_broadcast([128, 4, NE]),
       ALU.add, [t.b_psm, g.b_runc], S)
    yield
    tt(P, "dve", g.runc[:], g.runc[:], t.psm[:, 448:480], ALU.add, [t.b_psm, g.b_runc], [g.b_runc])
    yield
    ts(P, "dve", tmp, rank, float(CAP), 1.0e6, ALU.is_ge, ALU.mult, S, S)
    yield
    tt(P, "dve", rank, rank, tmp, ALU.add, S, S)
    yield
    tt(P, "dve", rank, rank, g.eC[:].unsqueeze(1).to_broadcast([128, 4, NE]), ALU.add, S + [g.b_eC], S)
    yield
    tt(P, "dve", tmp, rank, oh1, ALU.mult, S, S)
    yield
    P.op("dve", lambda e: e.tensor_reduce(out=posf[:, :, 0], in_=tmp, axis=AX.X, op=ALU.add), reads=S, writes=S)
    yield
    tt(P, "dve", tmp, rank, oh2, ALU.mult, S, S)
    yield
    P.op("dve", lambda e: e.tensor_reduce(out=posf[:, :, 1], in_=tmp, axis=AX.X, op=ALU.add), reads=S, writes=S)
    yield
    ts(P, "dve", valid, posf, float(NSLOT) - 0.5, None, ALU.is_lt, None, S, S)
    yield
    tt(P, "dve", g.wts[:, gb * 4:(gb + 1) * 4, :], wtmp, valid, ALU.mult, S, [g.b_wts])
    yield
    cp(P, "dve", g.posi[:, gb * 4:(gb + 1) * 4, :], posf, S, [g.b_posi])
    yield
    Xs = dr["Xs"]
    for i in range(4):
        tile = gb * 4 + i
        for k in range(2):
            P.op("pool", lambda e, k=k, tile=tile, i=i: e.indirect_dma_start(
                out=Xs[:, :], out_offset=bass.IndirectOffsetOnAxis(ap=g.posi[:, tile, k:k + 1], axis=0),
                in_=t.x1b[:, i, :], in_offset=None, bounds_check=P.bound_reg(e), oob_is_err=False),
                reads=[t.b_x1b[i], g.b_posi], cwrites=[g.b_Xs], key="sc%d" % i)
            yield


def load_weight_bf16(P, dst, b_dst, src_ap, n_k, ncols, key):
    first = True
    for c0 in range(0, ncols, 512):
        cw = min(512, ncols - c0)
        P.dma("pool", dst[:, :, c0:c0 + cw], src_ap.rearrange("(c p) n -> p c n", p=128)[:, :, c0:c0 + cw],
              writes=[b_dst] if first else (), cwrites=() if first else [b_dst], key=key)
        first = False


def phase_m0(P, g, dr):
    P.begin_phase()
    if not hasattr(g, "inited"):
        emit_const_init(P, g)
        g.inited = True
    P.op("pool", lambda e: e.memset(g.runc[:], 0.0), writes=[g.b_runc])
    t = alloc_tail(P, g, 0)
    load_tail_consts(P, g, t, dr, 0)
    pv, b_pv = P.sbb([128, 168], F32, "pv0")
    P.dma("sp", pv[:], dr["pv0"][:, :], writes=[b_pv], key="cst")
    win, b_win = P.sbb([128, 8, 2560], BF16, "win")
    wout, b_wout = P.sbb([128, 8, 1024], BF16, "wout")
    load_weight_bf16(P, win, b_win, dr["ab_w_in"], 8, 2560, "win")
    load_weight_bf16(P, wout, b_wout, dr["ab_w_out"], 8, 1024, "wout")
    dg, b_dg = P.sbb([128, 4 * 31, 128], BF16, "diag")
    for j in range(4 * 31):
        ts(P, "pool" if j % 2 else "dve", dg[:, j, :], g.ident_f[:], pv[:, 20 + j:21 + j], None, ALU.mult, None,
           [g.b_ident_f, b_pv], [b_dg] if j == 0 else (), cwrites=() if j == 0 else [b_dg])
    onesM, b_onesM = P.sbb([128, 128], BF16, "onesM")
    P.op("pool", lambda e: e.memset(onesM[:], 1.0 / 512.0), writes=[b_onesM])

    xring = Ring([(P.sb([128, 4, D], F32, "xblk"), [P.buf("xt%d" % i) for i in range(4)]) for _ in range(2)])
    xT, b_xT = P.sbb([128, 8, BLK], BF16, "xT")
    Ain, b_Ain = P.sbb([128, 4, 30 + BLK], BF16, "Ain")
    Bin, b_Bin = P.sbb([128, 4, 2 + BLK], F32, "Bin")
    sig = Ring([P.sbb([128, BLK], F32, "sig") for _ in range(1)])
    y32, b_y32 = P.sbb([128, 4, BLK], F32, "y32")
    ybf, b_ybf = P.sbb([128, 4, BLK], BF16, "ybf")
    ysq, b_ysq = P.sbb([128, 4, BLK], BF16, "ysq")
    mean, b_mean = P.sbb([128, BLK], F32, "mean")
    rstd, b_rstd = P.sbb([128, BLK], F32, "rstdA")
    tmpA = Ring([P.sbb([128, BLK], F32, "tmpA") for _ in range(1)])
    catT, b_catT = P.sbb([128, 8, BLK], BF16, "catT")
    b_cat = [P.buf("cat%d" % i) for i in range(8)]
    gc = Ring([P.sbb([128, BLK], F32, "gc") for _ in range(1)])
    acc = Ring([P.sbb([128, BLK], F32, "acc") for _ in range(1)])
    pmm = Ring([P.psb([128, 512], F32, "pmm") for _ in range(3)])
    pst0, b_pst0 = P.psb([128, 512], F32, "pst0")
    pst1, b_pst1 = P.psb([128, 512], F32, "pst1")
    ptr = t.ptr

    x_dr = dr["x"]

    def load_x(gbn):
        xb_, b_ = xring.items[gbn % 2]
        P.dma("sp", xb_[:], x_dr[gbn * BLK:(gbn + 1) * BLK, :].rearrange("(i p) d -> p i d", p=128),
              writes=b_, key="xblk%d" % (gbn % 2))

    for gb in range(NBLK):
        seq_start = (gb % (SEQ // BLK)) == 0
        if gb == 0:
            load_x(0)
        if gb + 1 < NBLK:
            load_x(gb + 1)
        if gb == 0:
            emit_zero_fill(P, g, dr)
        xblk, b_xt = xring.items[gb % 2]
        for c in range(8):
            pt, b_pt = ptr.next()
            for i in range(4):
                tr(P, pt[:, i * 128:(i + 1) * 128], xblk[:, i, c * 128:(c + 1) * 128], g.ident_f[:], i == 0,
                   [b_xt[i], g.b_ident_f], b_pt)
            cp(P, "act" if c % 2 == 0 else "dve", xT[:, c, :], pt[:], [b_pt], [b_xT] if c == 0 else (),
               cwrites=() if c == 0 else [b_xT])
        if seq_start:
            P.op("pool", lambda e: e.memset(Ain[:, :, 0:30], 0.0), writes=[b_Ain])
            P.op("pool", lambda e: e.memset(Bin[:, :, 0:2], 0.0), writes=[b_Bin])

        def inproj(m):
            pm, b_pm = pmm.next()
            for c in range(8):
                mm(P, pm[:], win[:, c, m * 128:(m + 1) * 128], xT[:, c, :], c == 0, c == 7, [b_win, b_xT], b_pm)
            return pm, b_pm

        for q in range(4):
            pm, b_pm = inproj(4 + q)
            sg, b_sg = sig.next()
            act(P, sg[:], pm[:], AF.Sigmoid, [b_pm, b_pv], [b_sg], bias=pv[:, 4 + q:5 + q])
            pm2, b_pm2 = inproj(q)
            stt(P, Ain[:, q, 30:30 + BLK], pm2[:], pv[:, q:q + 1], sg[:], ALU.add, ALU.mult,
                [b_pm2, b_pv, b_sg], (), cwrites=[b_Ain])
        for q in range(4):
            pm, b_pm = pmm.next()
            for k in range(31):
                mm(P, pm[:], dg[:, q * 31 + k, :], Ain[:, q, k:k + BLK], k == 0, k == 30, [b_dg, b_Ain], b_pm)
            act(P, y32[:, q, :], pm[:], AF.Identity, [b_pm, b_pv], [b_y32] if q == 0 else (), bias=pv[:, 144 + q:145 + q],
                cwrites=() if q == 0 else [b_y32])
            act(P, ysq[:, q, :], pm[:], AF.Square, [b_pm, b_pv], [b_ysq] if q == 0 else (), bias=pv[:, 144 + q:145 + q],
                cwrites=() if q == 0 else [b_ysq])
            cp(P, "dve", ybf[:, q, :], y32[:, q, :], [b_y32], [b_ybf] if q == 0 else (), cwrites=() if q == 0 else [b_ybf])
        cp(P, "pool", Ain[:, :, 0:30], Ain[:, :, BLK:BLK + 30], [b_Ain], [b_Ain])
        for q in range(4):
            mm(P, pst0[:], onesM[:], ybf[:, q, :], q == 0, q == 3, [b_onesM, b_ybf], b_pst0)
        for q in range(4):
            mm(P, pst1[:], onesM[:], ysq[:, q, :], q == 0, q == 3, [b_onesM, b_ysq], b_pst1)
        cp(P, "act", mean[:], pst0[:], [b_pst0], [b_mean])
        tt(P, "dve", rstd[:], mean[:], mean[:], ALU.mult, [b_mean], [b_rstd])
        tt(P, "dve", rstd[:], pst1[:], rstd[:], ALU.subtract, [b_pst1, b_rstd], [b_rstd])
        ts(P, "dve", rstd[:], rstd[:], 0.0, None, ALU.max, None, [b_rstd], [b_rstd])
        act(P, rstd[:], rstd[:], AF.Sqrt, [b_rstd], [b_rstd], bias=EPS)
        P.op("dve", lambda e: e.reciprocal(out=rstd[:], in_=rstd[:]), reads=[b_rstd], writes=[b_rstd])
        for q in range(4):
            tA, b_tA = tmpA.next()
            tt(P, "dve", tA[:], y32[:, q, :], mean[:], ALU.subtract, [b_y32, b_mean], [b_tA])
            tt(P, "pool", tA[:], tA[:], rstd[:], ALU.mult, [b_tA, b_rstd], [b_tA])
            act(P, catT[:, q, :], tA[:], AF.Silu, [b_tA, b_pv], [b_cat[q]], bias=pv[:, 152 + q:153 + q], scale=pv[:, 148 + q:149 + q])
        for q in range(4):
            pm, b_pm = inproj(12 + q)
            gcq, b_gc = gc.next()
            act(P, gcq[:], pm[:], AF.Identity, [b_pm, b_pv], [b_gc], bias=pv[:, 12 + q:13 + q])
            pm2, b_pm2 = inproj(16 + q)
            stt(P, Bin[:, q, 2:2 + BLK], pm2[:], pv[:, 16 + q:17 + q], gcq[:], ALU.add, ALU.mult,
                [b_pm2, b_pv, b_gc], (), cwrites=[b_Bin])
            ac, b_ac = acc.next()
            ts(P, "dve", ac[:], Bin[:, q, 0:BLK], pv[:, 156 + q * 3:157 + q * 3], None, ALU.mult, None, [b_Bin, b_pv], [b_ac])
            stt(P, ac[:], Bin[:, q, 1:1 + BLK], pv[:, 157 + q * 3:158 + q * 3], ac[:], ALU.mult, ALU.add, [b_Bin, b_pv, b_ac], [b_ac])
            stt(P, ac[:], Bin[:, q, 2:2 + BLK], pv[:, 158 + q * 3:159 + q * 3], ac[:], ALU.mult, ALU.add, [b_Bin, b_pv, b_ac], [b_ac])
            pm3, b_pm3 = inproj(8 + q)
            stt(P, catT[:, 4 + q, :], pm3[:], pv[:, 8 + q:9 + q], ac[:], ALU.add, ALU.mult, [b_pm3, b_pv, b_ac], [b_cat[4 + q]])
        cp(P, "pool", Bin[:, :, 0:2], Bin[:, :, BLK:BLK + 2], [b_Bin], [b_Bin])
        emit_tail_block(P, g, t, xblk, b_xt, catT, b_cat, wout, b_wout, pmm, gb, dr)
    P.end_phase()


def phase_m1(P, g, dr):
    P.begin_phase()
    if not hasattr(g, "inited"):
        emit_const_init(P, g)
        g.inited = True
    P.op("pool", lambda e: e.memset(g.runc[:], 0.0), writes=[g.b_runc])
    t = alloc_tail(P, g, 1)
    load_tail_consts(P, g, t, dr, 1)
    pv, b_pv = P.sbb([128, 12], F32, "pv1")
    P.dma("sp", pv[:], dr["pv1"][:, :], writes=[b_pv], key="cst")
    bx, b_bx = P.sbb([128, 2048], F32, "bc1x")
    P.dma("sp", bx[:], dr["bc1x"][:, :], writes=[b_bx], key="cst")
    win, b_win = P.sbb([128, 8, 2560], BF16, "win")
    wout, b_wout = P.sbb([128, 8, 1024], BF16, "wout")
    load_weight_bf16(P, win, b_win, dr["cd_w_in"], 8, 2560, "win")
    load_weight_bf16(P, wout, b_wout, dr["cd_w_out"], 8, 1024, "wout")
    wsT, b_wsT = P.sbb([128, 8, 128], BF16, "wsT")
    P.dma("pool", wsT[:].rearrange("p h i -> p (h i)"), dr["wsT"][:, :], writes=[b_wsT], key="wsT")
    P.op("pool", lambda e: e.memset(wsT[64:128, :, 0:64], 0.0), writes=[b_wsT])
    wsb, b_wsb = P.sbb([128, 4, 128], F32, "wsb")
    P.dma("sp", wsb[:].rearrange("p a b -> p (a b)"), dr["wsb"][:, :], writes=[b_wsb], key="cst")
    relP, b_relP = P.sbb([128, 8, 640], F32, "relP")
    P.dma("sp", relP[:].rearrange("p a b -> p (a b)"), dr["relP"][:, :], writes=[b_relP], key="cst")

    xblk = P.sb([128, 4, D], F32, "xblk")
    b_xt = [P.buf("xt%d" % i) for i in range(4)]
    xT, b_xT = P.sbb([128, 8, BLK], BF16, "xT")
    vt, b_vt = P.sbb([128, 512], F32, "vt")
    vn, b_vn = P.sbb([128, 4, 512], BF16, "vn")
    st1, b_st1 = P.sbb([128, 6], F32, "st1")
    mv1, b_mv1 = P.sbb([128, 2], F32, "mv1")
    rs1, b_rs1 = P.sbb([128, 1], F32, "rs1")
    qz, b_qz = P.sbb([128, 8, BLK], BF16, "qz")
    P.op("pool", lambda e: e.memset(qz[:].rearrange("p a b -> p (a b)"), 0.0), writes=[b_qz])
    osb, b_osb = (None, None) if ATT_T else P.sbb([128, 264], F32, "osb")
    rden, b_rden = P.sbb([128, 512], F32, "rden")
    kT = P.sb([128, 4, SEQ], BF16, "kT")
    b_kT = [P.buf("kT%d" % j) for j in range(4)]
    Va = P.sb([128, 16, 8, 64 if ATT_T else 66], BF16, "Va")
    b_Va = [P.buf("Va%d" % j) for j in range(4)]
    gt, b_gt = P.sbb([128, 512], F32, "gt")
    catT = P.sb([128, 8, BLK], BF16, "catT")
    b_cat = [P.buf("cat%d" % i) for i in range(8)]
    sT = Ring([P.sbb([128, 640], F32, "sT") for _ in range(2)])
    pT = Ring([P.sbb([128, 640], BF16, "pT") for _ in range(2)])
    otok, b_otok = (None, None) if ATT_T else P.sbb([128, 512], F32, "otok")
    rcp, b_rcp = (None, None) if ATT_T else P.sbb([128, 8], F32, "rcp")
    pmm = Ring([P.psb([128, 512], F32, "pmm") for _ in range(3)])
    pO = [P.psb([128, 512], F32, "pO") for _ in range(2)]
    ptr = t.ptr
    spairs = Ring([(pmm.items[0], pmm.items[1]), (pmm.items[2], ptr.items[0])])
    P.op("pool", lambda e: e.memset(Va[:].rearrange("p a b c -> p (a b c)"), 1.0), writes=b_Va)

    x_dr = dr["X2"]
    for gb in range(NBLK):
        jj = gb % 4
        P.dma("sp", xblk[:], x_dr[gb * BLK:(gb + 1) * BLK, :].rearrange("(i p) d -> p i d", p=128),
              reads=[g.b_X2], writes=b_xt, key="xblk0")
        if gb == 0:
            emit_zero_fill(P, g, dr)
        for c in range(8):
            pt, b_pt = ptr.next()
            for i in range(4):
                tr(P, pt[:, i * 128:(i + 1) * 128], xblk[:, i, c * 128:(c + 1) * 128], g.ident_f[:], i == 0,
                   [b_xt[i], g.b_ident_f], b_pt)
            cp(P, "act" if c % 2 == 0 else "dve", xT[:, c, :], pt[:], [b_pt], [b_xT] if c == 0 else (),
               cwrites=() if c == 0 else [b_xT])

        def inproj_fm(m):
            pm, b_pm = pmm.next()
            for c in range(8):
                mm(P, pm[:], win[:, c, m * 128:(m + 1) * 128], xT[:, c, :], c == 0, c == 7, [b_win, b_xT], b_pm)
            return pm, b_pm

        def inproj_tm(i, col0):
            pm, b_pm = pmm.next()
            for c in range(8):
                mm(P, pm[:], xT[:, c, i * 128:(i + 1) * 128], win[:, c, col0:col0 + 512], c == 0, c == 7, [b_win, b_xT], b_pm)
            return pm, b_pm

        for i in range(4):
            pm, b_pm = inproj_tm(i, 512)
            tt(P, "dve", vt[:], pm[:], bx[:, 0:512], ALU.add, [b_pm, b_bx], [b_vt])
            P.op("dve", lambda e: e.bn_stats(out=st1[:], in_=vt[:]), reads=[b_vt], writes=[b_st1])
            P.op("dve", lambda e: e.bn_aggr(out=mv1[:], in_=st1[:]), reads=[b_st1], writes=[b_mv1])
            act(P, rs1[:], mv1[:, 1:2], AF.Sqrt, [b_mv1], [b_rs1], bias=EPS)
            P.op("dve", lambda e: e.reciprocal(out=rs1[:], in_=rs1[:]), reads=[b_rs1], writes=[b_rs1])
            ts(P, "dve", vt[:], vt[:], mv1[:, 0:1], rs1[:, 0:1], ALU.subtract, ALU.mult, [b_vt, b_mv1, b_rs1], [b_vt])
            tt(P, "pool", vt[:], vt[:], bx[:, 512:1024], ALU.mult, [b_vt, b_bx], [b_vt])
            tt(P, "pool", vn[:, i, :], vt[:], bx[:, 1024:1536], ALU.add, [b_vt, b_bx], [b_vn] if i == 0 else (),
               cwrites=() if i == 0 else [b_vn])
        for i in range(4):
            tl = jj * 4 + i
            pm, b_pm = inproj_tm(i, 2048)
            tt(P, "dve", Va[:, tl, :, 0:64], pm[:].rearrange("p (h c) -> p h c", h=8),
               bx[:, 1536:2048].rearrange("p (h c) -> p h c", h=8), ALU.add, [b_pm, b_bx],
               [b_Va[jj]] if i == 0 else (), cwrites=() if i == 0 else [b_Va[jj]])
        for qc in range(4):
            pm, b_pm = inproj_fm(12 + qc)
            act(P, kT[:, qc, jj * BLK:(jj + 1) * BLK], pm[:], AF.Identity, [b_pm, b_pv], [b_kT[jj]] if qc == 0 else (),
                bias=pv[:, 8 + qc:9 + qc], cwrites=() if qc == 0 else [b_kT[jj]])
        for qc in range(4):
            pm, b_pm = inproj_fm(8 + qc)
            for hh in range(2):
                ps_ = slice(hh * 64, (hh + 1) * 64)
                ts(P, "dve", qz[ps_, 2 * qc + hh, :], pm[ps_, :], pv[ps_, 4 + qc:5 + qc], 0.125, ALU.add, ALU.mult,
                   [b_pm, b_pv], [b_qz] if (qc == 0 and hh == 0) else (), cwrites=() if (qc == 0 and hh == 0) else [b_qz])
        if "sgu" in SKIP:
            for qc in range(4):
                P.op("pool", lambda e, qc=qc: e.memset(catT[:, qc, :], 0.0), writes=[b_cat[qc]])
        for qc in ([] if "sgu" in SKIP else range(4)):
            pgm, b_pgm = pmm.next()
            first = True
            for hh in range(2):
                h = 2 * qc + hh
                for i in range(4):
                    outap = pgm[hh * 64:(hh + 1) * 64, i * 128:(i + 1) * 128]
                    lhsT = vn[:, i, h * 64:(h + 1) * 64]
                    rhs = wsT[:, h, :]
                    if first:
                        P.op("pe", lambda e, o=outap, l=lhsT, r=rhs: e.matmul(out=o, lhsT=l, rhs=r, start=True, stop=True),
                             reads=[b_vn, b_wsT], writes=[b_pgm])
                        first = False
                    else:
                        P.op("pe", lambda e, o=outap, l=lhsT, r=rhs: e.matmul(out=o, lhsT=l, rhs=r, start=True, stop=True),
                             reads=[b_vn, b_wsT], cwrites=[b_pgm])
            tt(P, "dve", gt[:].rearrange("p (a b) -> p a b", a=4), pgm[:].rearrange("p (a b) -> p a b", a=4),
               wsb[:, qc, :].unsqueeze(1).to_broadcast([128, 4, 128]), ALU.add, [b_pgm, b_wsb], [b_gt])
            pm, b_pm = inproj_fm(qc)
            stt(P, catT[:, qc, :], pm[:], pv[:, qc:qc + 1], gt[:], ALU.add, ALU.mult, [b_pm, b_pv, b_gt], [b_cat[qc]])
        if "attn" in SKIP:
            for qc in range(4):
                P.op("pool", lambda e, qc=qc: e.memset(catT[:, 4 + qc, :], 0.0), writes=[b_cat[4 + qc]])
        for pr in ([] if "attn" in SKIP else range(4)):
            t0 = jj * 4 + pr
            slots = [s_ for s_ in range(5) if t0 - 4 + s_ >= 0]
            kbufs = list({b_kT[(t0 - 4 + s_) // 4] for s_ in slots})
            vbufs = list({b_Va[(t0 - 4 + s_) // 4] for s_ in slots})
            lo = slots[0]
            pend = []

            def flush_pv():
                while pend:
                    pend.pop(0)()

            for h in range(8):
                qc = h // 2
                (pa, b_pa), (pb, b_pb) = spairs.next()
                firstA = True
                for s_ in slots:
                    tk = t0 - 4 + s_
                    lhsT = kT[:, qc, tk * 128:(tk + 1) * 128]
                    rhs = qz[:, h, pr * 128:(pr + 1) * 128]
                    if s_ < 4:
                        outap, bb = pa[:, s_ * 128:(s_ + 1) * 128], b_pa
                        fw = firstA
                        firstA = False
                    else:
                        outap, bb = pb[:, 0:128], b_pb
                        fw = True
                    P.op("pe", lambda e, o=outap, l=lhsT, r=rhs: e.matmul(out=o, lhsT=l, rhs=r, start=True, stop=True),
                         reads=kbufs + [b_qz], writes=[bb] if fw else (), cwrites=() if fw else [bb])
                sTt, b_sT = sT.next()
                pTt, b_pT = pT.next()
                if lo < 4:
                    tt(P, "dve", sTt[:, lo * 128:512], pa[:, lo * 128:512], relP[:, h, lo * 128:512], ALU.add,
                       [b_pa, b_relP], [b_sT])
                    tt(P, "dve", sTt[:, 512:640], pb[:, 0:128], relP[:, h, 512:640], ALU.add, [b_pb, b_relP], (), cwrites=[b_sT])
                else:
                    tt(P, "dve", sTt[:, 512:640], pb[:, 0:128], relP[:, h, 512:640], ALU.add, [b_pb, b_relP], [b_sT])
                act(P, pTt[:, lo * 128:640], sTt[:, lo * 128:640], AF.Exp, [b_sT], [b_pT])
                if ATT_T:
                    def pv_fn(h=h, qc=qc, pTt=pTt, b_pT=b_pT):
                        po = (h % 2) * 64
                        (pN, b_pN), (pD, b_pD) = pO[0], pO[1]
                        for which, (pX, b_pX) in enumerate(((pN, b_pN), (pD, b_pD))):
                            for oi, s_ in enumerate(slots):
                                tk = t0 - 4 + s_
                                lhsT = Va[:, tk, h, 0:64] if which == 0 else g.ones_b[:, 0:64]
                                fw = (h == 0 and oi == 0)
                                P.op("pe", lambda e, o=pX[po:po + 64, qc * 128:(qc + 1) * 128], l=lhsT,
                                     r=pTt[:, s_ * 128:(s_ + 1) * 128], st=(oi == 0), sp=(oi == len(slots) - 1):
                                     e.matmul(out=o, lhsT=l, rhs=r, start=st, stop=sp),
                                     reads=(vbufs if which == 0 else [g.b_ones]) + [b_pT],
                                     writes=[b_pX] if fw else (), cwrites=() if fw else [b_pX])
                    flush_pv()
                    pend.append(pv_fn)
                    if h == 7:
                        flush_pv()
                    continue
                pOt, b_pO = pO[h // 4]
                c0 = (h % 4) * 66
                if "pv" in SKIP:
                    continue
                for oi, s_ in enumerate(slots):
                    tk = t0 - 4 + s_
                    fw = (h % 4 == 0 and oi == 0)
                    P.op("pe", lambda e, o=pOt[:, c0:c0 + 66], l=pTt[:, s_ * 128:(s_ + 1) * 128], r=Va[:, tk, h, :],
                         st=(oi == 0), sp=(oi == len(slots) - 1): e.matmul(out=o, lhsT=l, rhs=r, start=st, stop=sp),
                         reads=vbufs + [b_pT], writes=[b_pO] if fw else (), cwrites=() if fw else [b_pO])
                if h % 4 == 3 and "norm" not in SKIP:
                    hb = h // 4
                    cp(P, "act", osb[:], pOt[:, 0:264], [b_pO], [b_osb])
                    ov3 = osb[:].rearrange("p (h c) -> p h c", c=66)
                    P.op("dve", lambda e, ov3=ov3, hb=hb: e.reciprocal(out=rcp[:, hb * 4:(hb + 1) * 4], in_=ov3[:, :, 64]),
                         reads=[b_osb], writes=[b_rcp])
                    tt(P, "dve", otok[:, hb * 256:(hb + 1) * 256].rearrange("p (h c) -> p h c", c=64), ov3[:, :, 0:64],
                       rcp[:, hb * 4:(hb + 1) * 4].unsqueeze(2).to_broadcast([128, 4, 64]), ALU.mult, [b_osb, b_rcp],
                       [b_otok] if hb == 0 else (), cwrites=() if hb == 0 else [b_otok])
            if ATT_T:
                (pN, b_pN), (pD, b_pD) = pO[0], pO[1]
                P.op("dve", lambda e, pD=pD: e.reciprocal(out=rden[:], in_=pD[:]), reads=[b_pD], writes=[b_rden])
                for qc in range(4):
                    tt(P, "dve", catT[:, 4 + qc, pr * 128:(pr + 1) * 128], pN[:, qc * 128:(qc + 1) * 128],
                       rden[:, qc * 128:(qc + 1) * 128], ALU.mult, [b_pN, b_rden],
                       [b_cat[4 + qc]] if pr == 0 else (), cwrites=() if pr == 0 else [b_cat[4 + qc]])
                continue
            if "pv" in SKIP or "norm" in SKIP:
                if pr == 0:
                    for qc in range(4):
                        P.op("pool", lambda e, qc=qc: e.memset(catT[:, 4 + qc, :], 0.0), writes=[b_cat[4 + qc]])
                continue
            pt, b_pt = ptr.next()
            for qc in range(4):
                tr(P, pt[:, qc * 128:(qc + 1) * 128], otok[:, qc * 128:(qc + 1) * 128], g.ident_f[:], qc == 0,
                   [b_otok, g.b_ident_f], b_pt)
            for qc in range(4):
                cp(P, "act" if qc % 2 == 0 else "dve", catT[:, 4 + qc, pr * 128:(pr + 1) * 128], pt[:, qc * 128:(qc + 1) * 128],
                   [b_pt], [b_cat[4 + qc]] if pr == 0 else (), cwrites=() if pr == 0 else [b_cat[4 + qc]])
        emit_tail_block(P, g, t, xblk, b_xt, catT, b_cat, wout, b_wout, pmm, gb, dr)
    P.end_phase()

def phase_e(P, g, dr, L):
    P.begin_phase()
    NI = CAP // 128
    NW = 3
    sets = []
    for i in range(NW):
        sets.append((P.sbb([128, 8, DEX], BF16, "wg"), P.sbb([128, 8, DEX], BF16, "wu"), P.sbb([128, 4, D], BF16, "wd"),
                     P.sbb([128, NI, D], BF16, "xe")))
    xeT, b_xeT = P.sbb([128, 8, CAP], BF16, "xeT")
    sgt = Ring([P.sbb([128, CAP], F32, "sgt") for _ in range(2)])
    hT, b_hT = P.sbb([128, 4, CAP], BF16, "hT")
    yt = Ring([P.sbb([128, D], F32, "yt") for _ in range(3)])
    ptb = Ring([P.psb([128, 512], BF16, "ptb") for _ in range(2)])
    pg = Ring([P.psb([128, 512], F32, "pg") for _ in range(2)])
    pu = Ring([P.psb([128, 512], F32, "pu") for _ in range(2)])
    py = Ring([P.psb([128, 512], F32, "py") for _ in range(2)])
    Wg, Wu, Wd = dr["moe_w_gate"], dr["moe_w_up"], dr["moe_w_down"]
    Xs, Ys = dr["Xs"], dr["Ys"]

    def issue(ex):
        s_ = ex % NW
        (wgt, b_wg), (wut, b_wu), (wdt, b_wd), (xet, b_xe) = sets[s_]
        P.dma("sp", xet[:], Xs[ex * CAP:(ex + 1) * CAP, :].rearrange("(i p) d -> p i d", p=128),
              reads=[g.b_Xs], writes=[b_xe], key="xe%d" % s_)
        load_weight_bf16(P, wgt, b_wg, Wg[L, ex], 8, DEX, "wg%d" % s_)
        load_weight_bf16(P, wut, b_wu, Wu[L, ex], 8, DEX, "wu%d" % s_)
        load_weight_bf16(P, wdt, b_wd, Wd[L, ex], 4, D, "wd%d" % s_)

    for ex in range(min(NW - 1, NE)):
        issue(ex)
    for ex in range(NE):
        if ex + NW - 1 < NE:
            issue(ex + NW - 1)
        (wgt, b_wg), (wut, b_wu), (wdt, b_wd), (xet, b_xe) = sets[ex % NW]
        for c in range(8):
            pt, b_pt = ptb.next()
            for i in range(NI):
                tr(P, pt[:, i * 128:(i + 1) * 128], xet[:, i, c * 128:(c + 1) * 128], g.ident_b[:], i == 0,
                   [b_xe, g.b_ident_b], b_pt)
            cp(P, "act" if c % 2 == 0 else "dve", xeT[:, c, :], pt[:, 0:CAP], [b_pt], [b_xeT] if c == 0 else (),
               cwrites=() if c == 0 else [b_xeT])
        for m in range(4):
            pgt, b_pg = pg.next()
            put, b_pu = pu.next()
            for c in range(8):
                mm(P, pgt[:, 0:CAP], wgt[:, c, m * 128:(m + 1) * 128], xeT[:, c, :], c == 0, c == 7, [b_wg, b_xeT], b_pg)
            for c in range(8):
                mm(P, put[:, 0:CAP], wut[:, c, m * 128:(m + 1) * 128], xeT[:, c, :], c == 0, c == 7, [b_wu, b_xeT], b_pu)
            sg, b_sg = sgt.next()
            act(P, sg[:], pgt[:, 0:CAP], AF.Silu, [b_pg], [b_sg])
            tt(P, "dve", hT[:, m, :], sg[:], put[:, 0:CAP], ALU.mult, [b_sg, b_pu], [b_hT] if m == 0 else (),
               cwrites=() if m == 0 else [b_hT])
        for i in range(NI):
            ytile, b_yt = yt.next()
            for h in range(2):
                pyt, b_py = py.next()
                for m in range(4):
                    mm(P, pyt[:], hT[:, m, i * 128:(i + 1) * 128], wdt[:, m, h * 512:(h + 1) * 512], m == 0, m == 3,
                       [b_hT, b_wd], b_py)
                cp(P, "act" if h == 0 else "dve", ytile[:, h * 512:(h + 1) * 512], pyt[:], [b_py],
                   [b_yt] if h == 0 else (), cwrites=() if h == 0 else [b_yt])
            r0 = ex * CAP + i * 128
            P.dma("sp", Ys[r0:r0 + 128, :], ytile[:], reads=[b_yt], cwrites=[g.b_Ys], key="yst%d" % (yt.i % 3))
    P.end_phase()


def phase_c(P, g, dr, L, out_ap, b_out):
    P.begin_phase()
    t = G()
    t.bc, t.b_bc = P.sbb([128, 2 * D], F32, "bc")
    P.dma("sp", t.bc[:], dr["bcf%d" % L][:, :], writes=[t.b_bc], key="cst")
    t.lnr = Ring([make_ln_scratch(P) for _ in range(3)])
    NR = 4
    x1r = [P.sbb([128, D], F32, "x1c") for _ in range(NR)]
    y0r = [P.sbb([128, D], F32, "y0") for _ in range(NR)]
    y1r = [P.sbb([128, D], F32, "y1") for _ in range(NR)]
    rr = Ring([P.sbb([128, D], F32, "rc") for _ in range(2)])
    orr = Ring([P.sbb([128, D], F32, "oc") for _ in range(3)])
    Ys = dr["Ys"]
    for (yy, b_yy) in y0r + y1r:
        P.op("pool", lambda e, yy=yy: e.memset(yy[:], 0.0), writes=[b_yy])

    def issue_loads(tile):
        s_ = tile % NR
        x1, b_x1 = x1r[s_]
        P.dma("sp", x1[:], dr["X1"][tile * 128:(tile + 1) * 128, :], reads=[g.b_X1], writes=[b_x1], key="cx%d" % s_)
        for k, (yy, b_yy) in enumerate((y0r[s_], y1r[s_])):
            P.op("pool", lambda e, yy=yy, k=k, tile=tile: e.indirect_dma_start(
                out=yy[:], out_offset=None, in_=Ys[:, :],
                in_offset=bass.IndirectOffsetOnAxis(ap=g.posi[:, tile, k:k + 1], axis=0),
                bounds_check=P.bound_reg(e), oob_is_err=False),
                reads=[g.b_Ys, g.b_posi], writes=[b_yy], key="cy%d_%d" % (k, s_))

    NPRE = 3
    for tile in range(min(NPRE, NT)):
        issue_loads(tile)
    for tile in range(NT):
        if tile + NPRE < NT:
            issue_loads(tile + NPRE)
        s_ = tile % NR
        x1, b_x1 = x1r[s_]
        y0, b_y0 = y0r[s_]
        y1, b_y1 = y1r[s_]
        r, b_r = rr.next()
        act(P, r[:], x1[:], AF.Copy, [b_x1], [b_r], scale=ALPHA)
        stt(P, r[:], y0[:], g.wts[:, tile, 0:1], r[:], ALU.mult, ALU.add, [b_y0, g.b_wts, b_r], [b_r])
        stt(P, r[:], y1[:], g.wts[:, tile, 1:2], r[:], ALU.mult, ALU.add, [b_y1, g.b_wts, b_r], [b_r])
        o, b_o = orr.next()
        emit_ln_rows(P, t, r[:], b_r, o[:], b_o, 0, D)
        P.dma("sp", out_ap[tile * 128:(tile + 1) * 128, :], o[:], reads=[b_o], cwrites=[b_out], key="co%d" % (orr.i % 3))
    P.end_phase()


def build_program(n_layers=1, debug=False):
    nc = bass.Bass("TRN2", target_bir_lowering=False)
    dr = {}

    def din(name, shape, dt=F32):
        dr[name] = nc.dram_tensor(name, list(shape), dt, kind="ExternalInput").ap()

    din("x", [T, D])
    din("ab_w_in", [D, 2560])
    din("ab_w_out", [D, D])
    din("pv0", [128, 168])
    din("cd_w_in", [D, 2560])
    din("cd_w_out", [D, D])
    din("pv1", [128, 12])
    din("bc1x", [128, 2048])
    din("wsT", [128, 1024])
    din("wsb", [128, 512])
    din("relP", [128, 8 * 640])
    for L in range(2):
        din("bcm%d" % L, [128, 2 * D + 36])
        din("bcf%d" % L, [128, 2 * D])
        din("wr%d" % L, [D, 36])
    din("moe_w_gate", [2, NE, D, DEX])
    din("moe_w_up", [2, NE, D, DEX])
    din("moe_w_down", [2, NE, DEX, D])
    dr["out"] = nc.dram_tensor("out", [T, D], F32, kind="ExternalOutput").ap()
    kind = "ExternalOutput" if debug else "Internal"
    dr["X1"] = nc.dram_tensor("X1", [T, D], F32, kind=kind).ap()
    dr["Xs"] = nc.dram_tensor("Xs", [NSLOT, D], BF16, kind=kind).ap()
    dr["Ys"] = nc.dram_tensor("Ys", [NSLOT, D], F32, kind=kind).ap()
    dr["X2"] = nc.dram_tensor("X2", [T, D], F32, kind="Internal").ap()
    P = Prog(nc)
    g = G()
    setup_globals(P, g)
    g.b_X1 = P.buf("X1")
    g.b_Xs = P.buf("Xs")
    g.b_Ys = P.buf("Ys")
    g.b_X2 = P.buf("X2")
    g.b_out = P.buf("out")
    phase_m0(P, g, dr)
    phase_e(P, g, dr, 0)
    if n_layers == 1:
        phase_c(P, g, dr, 0, dr["out"], g.b_out)
    else:
        phase_c(P, g, dr, 0, dr["X2"], g.b_X2)
        phase_m1(P, g, dr)
        phase_e(P, g, dr, 1)
        phase_c(P, g, dr, 1, dr["out"], g.b_out)
    if debug:
        dr["posd"] = nc.dram_tensor("posd", [128, NT * 2], I32, kind="ExternalOutput").ap()
        dr["wtsd"] = nc.dram_tensor("wtsd", [128, NT * 2], F32, kind="ExternalOutput").ap()
        P.begin_phase()
        P.dma("sp", dr["posd"][:, :], g.posi[:].rearrange("p a b -> p (a b)"), reads=[g.b_posi], key="dbg")
        P.dma("sp", dr["wtsd"][:, :], g.wts[:].rearrange("p a b -> p (a b)"), reads=[g.b_wts], key="dbg")
        P.end_phase()
    P.finish()
    return nc, P


def prep_inputs(inp):
    f = lambda a: np.ascontiguousarray(np.asarray(a, dtype=np.float32))
    shared = {}
    shared["ab_w_in"] = f(inp["ab_w_in"][0])
    shared["ab_w_out"] = f(inp["ab_w_out"][0])
    pv0 = np.zeros((128, 168), np.float32)
    pv0[:, 0:20] = f(inp["ab_b_in"][0]).reshape(20, 128).T
    adw = f(inp["a_dw"][0])
    pv0[:, 20:144] = adw.reshape(31, 4, 128).transpose(2, 1, 0).reshape(128, 124)
    pv0[:, 144:148] = f(inp["a_dw_b"][0]).reshape(4, 128).T
    pv0[:, 148:152] = f(inp["a_ln_g"][0]).reshape(4, 128).T
    pv0[:, 152:156] = f(inp["a_ln_b"][0]).reshape(4, 128).T
    bdw = f(inp["b_dw"][0])
    pv0[:, 156:168] = bdw.reshape(3, 4, 128).transpose(2, 1, 0).reshape(128, 12)
    shared["pv0"] = pv0
    shared["cd_w_in"] = f(inp["cd_w_in"][0])
    shared["cd_w_out"] = f(inp["cd_w_out"][0])
    cb = f(inp["cd_b_in"][0])
    pv1 = np.zeros((128, 12), np.float32)
    pv1[:, 0:4] = cb[0:512].reshape(4, 128).T
    pv1[:, 4:8] = cb[1024:1536].reshape(4, 128).T
    pv1[:, 8:12] = cb[1536:2048].reshape(4, 128).T
    shared["pv1"] = pv1
    row = np.concatenate([cb[512:1024], f(inp["c_ln_g"][0]), f(inp["c_ln_b"][0]), cb[2048:2560]])
    shared["bc1x"] = np.ascontiguousarray(np.broadcast_to(row[None, :], (128, 2048)))
    ws = f(inp["c_ws"][0])
    shared["wsT"] = np.ascontiguousarray(ws.transpose(2, 0, 1).reshape(128, 1024))
    wb = f(inp["c_ws_b"][0])
    shared["wsb"] = np.ascontiguousarray(wb.reshape(4, 2, 1, 128).repeat(64, axis=2).transpose(1, 2, 0, 3).reshape(128, 512))
    rb = f(inp["d_rel_bias"][0])
    p_ = np.arange(128)[:, None, None]
    s_ = np.arange(5)[None, :, None]
    c_ = np.arange(128)[None, None, :]
    par = c_ // 64
    i_ = c_ % 64
    delta = 64 * (8 + par - 2 * s_) + i_ - p_
    ridx = np.clip(delta, -256, 256) + 256
    cdist = 8 - 2 * s_ + par - (p_ // 64)
    valid = (cdist >= 0) & (cdist <= 8)
    relv = np.where(valid[None], rb[:, ridx], np.float32(-30000.0)).astype(np.float32)
    shared["relP"] = np.ascontiguousarray(relv.transpose(1, 0, 2, 3).reshape(128, 8 * 640))
    for L in range(2):
        row = np.concatenate([f(inp["mix_ln_g"][L]), f(inp["mix_ln_b"][L]), f(inp["moe_rg_b"][L]), f(inp["moe_re_b"][L]).reshape(-1)])
        shared["bcm%d" % L] = np.ascontiguousarray(np.broadcast_to(row[None, :], (128, row.size)))
        row = np.concatenate([f(inp["ffn_ln_g"][L]), f(inp["ffn_ln_b"][L])])
        shared["bcf%d" % L] = np.ascontiguousarray(np.broadcast_to(row[None, :], (128, row.size)))
        shared["wr%d" % L] = np.ascontiguousarray(np.concatenate(
            [f(inp["moe_rg_w"][L]), f(inp["moe_re_w"][L]).transpose(1, 0, 2).reshape(D, 32)], axis=1))
    shared["moe_w_gate"] = f(inp["moe_w_gate"])
    shared["moe_w_up"] = f(inp["moe_w_up"])
    shared["moe_w_down"] = f(inp["moe_w_down"])
    x = f(inp["x"]).reshape(NCORES, T, D)
    return shared, x


_CACHE = {}


def kernel(**inputs):
    shared, x = prep_inputs(inputs)
    if "nc" not in _CACHE:
        _CACHE["nc"] = build_program(n_layers=2)[0]
    nc = _CACHE["nc"]
    in_maps = []
    for c in range(NCORES):
        m = dict(shared)
        m["x"] = x[c]
        in_maps.append(m)
    res = run_bass_kernel_spmd(nc, in_maps, core_ids=list(range(NCORES)))
    out = np.stack([np.asarray(r["out"]) for r in res.results], 0)
    return out.reshape(16, SEQ, D).astype(np.float32)
```

```python
import numpy as np
from contextlib import ExitStack
import concourse.bass as bass
import concourse.mybir as mybir
from concourse.bass_utils import run_bass_kernel_spmd

F32 = mybir.dt.float32
BF16 = mybir.dt.bfloat16
I32 = mybir.dt.int32
AF = mybir.ActivationFunctionType
ALU = mybir.AluOpType
AX = mybir.AxisListType

NCORES = 8
D = 1024
SEQ = 2048
BPC = 2
T = BPC * SEQ
NT = T // 128
BLK = 512
NBLK = T // BLK
NE = 32
CAP = 384
NSLOT = NE * CAP
ALPHA = float(4 ** 0.25)
EPS = 1e-5
DEX = 512
import os
SKIP = set(os.environ.get("M1_SKIP", "").split(","))
ATT_T = os.environ.get("ATT_T", "1") == "1"


class Buf:
    __slots__ = ("name", "xw", "cw", "readers")

    def __init__(self, name=""):
        self.name = name
        self.xw = []
        self.cw = []
        self.readers = []

    def reset(self):
        self.xw = []
        self.cw = []
        self.readers = []


class Op:
    __slots__ = ("eng", "fn", "reads", "writes", "cwrites", "key", "deps", "signal", "seq", "kcount", "idx")

    def __init__(self, eng, fn, reads, writes, cwrites, key):
        self.eng = eng
        self.fn = fn
        self.reads = reads
        self.writes = writes
        self.cwrites = cwrites
        self.key = key
        self.deps = None
        self.signal = False
        self.seq = None
        self.kcount = None


class Ring:
    def __init__(self, items):
        self.items = items
        self.i = 0

    def next(self):
        it = self.items[self.i % len(self.items)]
        self.i += 1
        return it


class Prog:
    ENGS = ("pe", "act", "dve", "pool", "sp")

    def __init__(self, nc):
        self.nc = nc
        self.gstack = ExitStack()
        self.pstack = None
        self.ops = []
        self.bufs = []
        self.sems = {}
        self.eng_seq = {e: 0 for e in self.ENGS}
        self.key_count = {}
        self.phase_no = 0
        self.total_ops = 0
        self.uid = 0
        self.pending = None
        self._in_chain = False

    def sb(self, shape, dtype, name=None, glob=False):
        self.uid += 1
        st = self.gstack if glob else self.pstack
        return st.enter_context(self.nc.sbuf_tensor("%s_%d" % (name or "t", self.uid), list(shape), dtype))

    def ps(self, shape, dtype=F32, name=None):
        self.uid += 1
        return self.pstack.enter_context(self.nc.psum_tensor("%s_%d" % (name or "p", self.uid), list(shape), dtype))

    def buf(self, name=""):
        b = Buf(name)
        self.bufs.append(b)
        return b

    def sbb(self, shape, dtype, name=None, glob=False):
        return self.sb(shape, dtype, name, glob), self.buf(name or "")

    def psb(self, shape, dtype=F32, name=None):
        return self.ps(shape, dtype, name), self.buf(name or "")

    def sem(self, name):
        if name not in self.sems:
            self.sems[name] = self.gstack.enter_context(self.nc.semaphore(name))
        return self.sems[name]

    def op(self, eng, fn, reads=(), writes=(), cwrites=(), key=None):
        o = Op(eng, fn, tuple(reads), tuple(writes), tuple(cwrites), key)
        o.idx = len(self.ops)
        self.ops.append(o)
        if self.pending is not None and not self._in_chain and eng == "dve":
            self._in_chain = True
            try:
                next(self.pending)
            except StopIteration:
                self.pending = None
            self._in_chain = False
        return o

    def drain(self):
        if self.pending is not None:
            self._in_chain = True
            for _ in self.pending:
                pass
            self._in_chain = False
            self.pending = None

    def dma(self, eng, out, in_, reads=(), writes=(), cwrites=(), key=None, **kw):
        assert key is not None
        return self.op(eng, lambda e: e.dma_start(out=out, in_=in_, **kw), reads, writes, cwrites, key)

    def bound_reg(self, eng):
        if self._breg is None:
            self._breg = eng.to_reg(NSLOT - 1)
        return self._breg

    def begin_phase(self):
        self.pstack = ExitStack()
        self._breg = None
        self.ops = []
        for b in self.bufs:
            b.reset()

    def end_phase(self):
        self.drain()
        nc = self.nc
        ops = self.ops
        for o in ops:
            deps = set()
            for b in o.reads:
                deps.update(b.xw)
                deps.update(b.cw)
            for b in o.writes:
                deps.update(b.xw)
                deps.update(b.cw)
                deps.update(b.readers)
            for b in o.cwrites:
                deps.update(b.xw)
                deps.update(b.readers)
            deps.discard(o.idx)
            o.deps = deps
            for b in o.reads:
                b.readers.append(o.idx)
            for b in o.writes:
                b.xw = [o.idx]
                b.cw = []
                b.readers = []
            for b in o.cwrites:
                b.cw.append(o.idx)
        for o in ops:
            latest = {}
            ddeps = []
            for d in o.deps:
                p = ops[d]
                if p.key is not None:
                    ddeps.append(p)
                elif not (p.eng == "pe" and o.eng == "pe"):
                    q = latest.get(p.eng)
                    if q is None or q.idx < p.idx:
                        latest[p.eng] = p
            for p in latest.values():
                p.signal = True
            o.deps = ddeps + list(latest.values())
        last_compute = {}
        for o in ops:
            if o.key is None:
                last_compute[o.eng] = o
        for o in last_compute.values():
            o.signal = True
        for o in ops:
            if o.key is None and o.signal:
                self.eng_seq[o.eng] += 1
                o.seq = self.eng_seq[o.eng]
        waits = [None] * len(ops)
        kc = self.key_count
        keys_used = set()
        for o in ops:
            w = {}
            for p in o.deps:
                if p.key is not None:
                    nm = "k_" + p.key
                    v = 16 * kc[p.key]
                else:
                    if not p.signal:
                        continue
                    nm = "e_" + p.eng
                    v = p.seq
                if w.get(nm, 0) < v:
                    w[nm] = v
            waits[o.idx] = w
            if o.key is not None:
                kc[o.key] = kc.get(o.key, 0) + 1
                keys_used.add(o.key)
        for o in ops:
            for nm in waits[o.idx]:
                self.sem(nm)
            if o.key is not None:
                self.sem("k_" + o.key)
            elif o.signal:
                self.sem("e_" + o.eng)
        fin = {}
        for k in sorted(keys_used):
            fin["k_" + k] = 16 * kc[k]
        for e, o in last_compute.items():
            fin["e_" + e] = o.seq
        sems = self.sems
        by_eng = {e: [o for o in ops if o.eng == e] for e in self.ENGS}

        def run(ename, eng):
            waited = {}
            for o in by_eng[ename]:
                for nm, v in waits[o.idx].items():
                    if waited.get(nm, 0) >= v:
                        continue
                    waited[nm] = v
                    eng.wait_ge(sems[nm], v)
                ins = o.fn(eng)
                if o.key is not None:
                    ins.then_inc(sems["k_" + o.key], 16)
                elif o.signal:
                    ins.then_inc(sems["e_" + o.eng], 1)
            for nm, v in fin.items():
                if nm == "e_" + ename:
                    continue
                eng.wait_ge(sems[nm], v)
            if ename == "pool" and self._breg is not None:
                eng.free_register(self._breg)
                self._breg = None

        with nc.Block() as block:
            @block.tensor
            def _(e):
                run("pe", e)

            @block.scalar
            def _(e):
                run("act", e)

            @block.vector
            def _(e):
                run("dve", e)

            @block.gpsimd
            def _(e):
                run("pool", e)

            @block.sync
            def _(e):
                run("sp", e)
        self.total_ops += len(ops)
        self.pstack.close()
        self.pstack = None
        self.phase_no += 1

    def finish(self):
        self.gstack.close()


def mm(P, out, lhsT, rhs, start, stop, reads, pbuf):
    if start:
        P.op("pe", lambda e: e.matmul(out=out, lhsT=lhsT, rhs=rhs, start=True, stop=stop), reads=reads, writes=[pbuf])
    else:
        P.op("pe", lambda e: e.matmul(out=out, lhsT=lhsT, rhs=rhs, start=False, stop=stop), reads=reads, cwrites=[pbuf])


def tr(P, out, in_, ident, first, reads, pbuf):
    if first:
        P.op("pe", lambda e: e.transpose(out=out, in_=in_, identity=ident), reads=reads, writes=[pbuf])
    else:
        P.op("pe", lambda e: e.transpose(out=out, in_=in_, identity=ident), reads=reads, cwrites=[pbuf])


def act(P, out, in_, func, reads, writes, bias=None, scale=None, cwrites=(), accum_out=None):
    kw = {}
    if bias is not None:
        kw["bias"] = bias
    if scale is not None:
        kw["scale"] = scale
    if accum_out is not None:
        kw["accum_out"] = accum_out
    P.op("act", lambda e: e.activation(out=out, in_=in_, func=func, **kw), reads=reads, writes=writes, cwrites=cwrites)


def ts(P, eng, out, in0, s1, s2, op0, op1, reads, writes, cwrites=()):
    if s2 is None:
        P.op(eng, lambda e: e.tensor_scalar(out=out, in0=in0, scalar1=s1, scalar2=None, op0=op0), reads=reads, writes=writes, cwrites=cwrites)
    else:
        P.op(eng, lambda e: e.tensor_scalar(out=out, in0=in0, scalar1=s1, scalar2=s2, op0=op0, op1=op1), reads=reads, writes=writes, cwrites=cwrites)


def tt(P, eng, out, in0, in1, op, reads, writes, cwrites=()):
    P.op(eng, lambda e: e.tensor_tensor(out=out, in0=in0, in1=in1, op=op), reads=reads, writes=writes, cwrites=cwrites)


def stt(P, out, in0, scalar, in1, op0, op1, reads, writes, cwrites=()):
    P.op("dve", lambda e: e.scalar_tensor_tensor(out=out, in0=in0, scalar=scalar, in1=in1, op0=op0, op1=op1),
         reads=reads, writes=writes, cwrites=cwrites)


def cp(P, eng, out, in_, reads, writes, cwrites=()):
    if eng == "act":
        P.op("act", lambda e: e.activation(out=out, in_=in_, func=AF.Copy), reads=reads, writes=writes, cwrites=cwrites)
    else:
        P.op(eng, lambda e: e.tensor_copy(out=out, in_=in_), reads=reads, writes=writes, cwrites=cwrites)


class G:
    pass


def setup_globals(P, g):
    g.ident_f, g.b_ident_f = P.sbb([128, 128], F32, "identf", glob=True)
    g.ident_b, g.b_ident_b = P.sbb([128, 128], BF16, "identb", glob=True)
    g.U_b, g.b_U = P.sbb([128, 128], BF16, "U", glob=True)
    g.ones_b, g.b_ones = P.sbb([128, 128], BF16, "ones", glob=True)
    g.eC, g.b_eC = P.sbb([128, NE], F32, "eC", glob=True)
    g.runc, g.b_runc = P.sbb([128, NE], F32, "runc", glob=True)
    g.posi, g.b_posi = P.sbb([128, NT, 2], I32, "posi", glob=True)
    g.wts, g.b_wts = P.sbb([128, NT, 2], F32, "wts", glob=True)
    g.eCi, g.b_eCi = P.sbb([128, NE], I32, "eCi", glob=True)


def emit_const_init(P, g):
    P.op("pool", lambda e: e.memset(g.ident_f[:], 0.0), writes=[g.b_ident_f])
    P.op("pool", lambda e: e.affine_select(out=g.ident_f[:], in_=g.ident_f[:], pattern=[[-1, 128]],
                                           compare_op=ALU.not_equal, fill=1.0, base=0, channel_multiplier=1),
         writes=[g.b_ident_f])
    cp(P, "pool", g.ident_b[:], g.ident_f[:], [g.b_ident_f], [g.b_ident_b])
    P.op("pool", lambda e: e.memset(g.ones_b[:], 1.0), writes=[g.b_ones])
    P.op("pool", lambda e: e.affine_select(out=g.U_b[:], in_=g.ones_b[:], pattern=[[1, 128]],
                                           compare_op=ALU.is_gt, fill=0.0, base=0, channel_multiplier=-1),
         reads=[g.b_ones], writes=[g.b_U])
    P.op("pool", lambda e: e.iota(g.eCi[:], pattern=[[CAP, NE]], base=0, channel_multiplier=0), writes=[g.b_eCi])
    cp(P, "pool", g.eC[:], g.eCi[:], [g.b_eCi], [g.b_eC])


def emit_zero_fill(P, g, dr, t):
    zt, b_zt = t.x1b[:, 0, :], t.b_x1b[0]
    P.op("pool", lambda e: e.memset(zt, 0.0), writes=[b_zt])
    Xv = dr["Xs"].rearrange("(n p) d -> p n d", p=128)
    for n in range(NSLOT // 128):
        P.dma("sp", Xv[:, n, :], zt, reads=[b_zt], writes=[g.b_Xs] if n == 0 else (), cwrites=() if n == 0 else [g.b_Xs], key="zf")


def alloc_tail(P, g, L):
    t = G()
    t.bc, t.b_bc = P.sbb([128, 2 * D + 36], F32, "bc")
    t.wr, t.b_wr = P.sbb([128, 8, 36], F32, "wr")
    t.x1b = P.sb([128, 4, D], BF16, "x1b")
    t.b_x1b = [P.buf("x1b%d" % i) for i in range(4)]
    t.x1T = Ring([P.sbb([128, 8, 128], F32, "x1T") for _ in range(1)])
    t.lnr = Ring([make_ln_scratch(P) for _ in range(4)])
    t.sm, t.b_sm = P.sbb([128, 928], F32, "small")
    t.oh, t.b_oh = P.sbb([128, 4, NE], BF16, "oh")
    t.ptr = Ring([P.psb([128, 512], F32, "ptrT") for _ in range(2)])
    t.psm, t.b_psm = P.psb([128, 512], F32, "psm")
    return t


def load_tail_consts(P, g, t, dr, L):
    P.dma("sp", t.bc[:], dr["bcm%d" % L][:, :], writes=[t.b_bc], key="cst")
    P.dma("sp", t.wr[:], dr["wr%d" % L].rearrange("(c p) n -> p c n", p=128), writes=[t.b_wr], key="cst")


def make_ln_scratch(P):
    sc = G()
    sc.st, sc.b_st = P.sbb([128, 2, 6], F32, "bnst")
    sc.mv, sc.b_mv = P.sbb([128, 2], F32, "mv")
    sc.rstd, sc.b_rstd = P.sbb([128, 1], F32, "rstd")
    return sc


def emit_ln_rows(P, t, src, b_src, dst, b_dst, goff, boff):
    sc = t.lnr.next()
    for h in range(2):
        P.op("dve", lambda e, h=h: e.bn_stats(out=sc.st[:, h, :], in_=src[:, h * 512:(h + 1) * 512]),
             reads=[b_src], writes=[sc.b_st] if h == 0 else (), cwrites=() if h == 0 else [sc.b_st])
    P.op("dve", lambda e: e.bn_aggr(out=sc.mv[:], in_=sc.st[:].rearrange("p a b -> p (a b)")), reads=[sc.b_st], writes=[sc.b_mv])
    act(P, sc.rstd[:], sc.mv[:, 1:2], AF.Sqrt, [sc.b_mv], [sc.b_rstd], bias=EPS)
    P.op("dve", lambda e: e.reciprocal(out=sc.rstd[:], in_=sc.rstd[:]), reads=[sc.b_rstd], writes=[sc.b_rstd])
    ts(P, "dve", dst, src, sc.mv[:, 0:1], sc.rstd[:, 0:1], ALU.subtract, ALU.mult, [b_src, sc.b_mv, sc.b_rstd], [b_dst])
    tt(P, "dve", dst, dst, t.bc[:, goff:goff + D], ALU.mult, [b_dst, t.b_bc], [b_dst])
    tt(P, "pool", dst, dst, t.bc[:, boff:boff + D], ALU.add, [b_dst, t.b_bc], [b_dst])


def emit_tail_block(P, g, t, xb, b_xt, catT, b_cat, wout, b_wout, pmm, gb, dr):
    sm = t.sm
    S = [t.b_sm]
    for i in range(4):
        tile = gb * 4 + i
        xi = xb[:, i, :]
        for hf in range(2):
            pm, b_pm = pmm.next()
            for c in range(8):
                mm(P, pm[:], catT[:, c, i * 128:(i + 1) * 128], wout[:, c, hf * 512:(hf + 1) * 512], c == 0, c == 7,
                   [b_cat[c], b_wout], b_pm)
            stt(P, xb[:, i, hf * 512:(hf + 1) * 512], xb[:, i, hf * 512:(hf + 1) * 512], ALPHA, pm[:], ALU.mult, ALU.add,
                [b_xt[i], b_pm], [b_xt[i]])
        emit_ln_rows(P, t, xi, b_xt[i], xi, b_xt[i], 0, D)
        P.dma("sp", dr["X1"][tile * 128:(tile + 1) * 128, :], xi, reads=[b_xt[i]], cwrites=[g.b_X1], key="x1st%d" % i)
    P.drain()
    for i in range(4):
        cp(P, "act", t.x1b[:, i, :], xb[:, i, :], [b_xt[i]], [t.b_x1b[i]])
    for i in range(4):
        x1T, b_x1T = t.x1T.next()
        for half in range(2):
            pt, b_pt = t.ptr.next()
            for cc in range(4):
                c = half * 4 + cc
                tr(P, pt[:, cc * 128:(cc + 1) * 128], xb[:, i, c * 128:(c + 1) * 128], g.ident_f[:], cc == 0,
                   [b_xt[i], g.b_ident_f], b_pt)
            cp(P, "act" if half == 0 else "dve", x1T[:, half * 4:(half + 1) * 4, :],
               pt[:].rearrange("p (c n) -> p c n", c=4), [b_pt], [b_x1T] if half == 0 else (),
               cwrites=() if half == 0 else [b_x1T])
        for c in range(8):
            o_ = t.psm[:, i * 36:(i + 1) * 36]
            if i == 0 and c == 0:
                P.op("pe", lambda e, o_=o_, l=x1T[:, c, :], r=t.wr[:, c, :]: e.matmul(out=o_, lhsT=l, rhs=r, start=True, stop=False),
                     reads=[b_x1T, t.b_wr], writes=[t.b_psm])
            else:
                P.op("pe", lambda e, o_=o_, l=x1T[:, c, :], r=t.wr[:, c, :], st=(c == 0), sp=(c == 7):
                     e.matmul(out=o_, lhsT=l, rhs=r, start=st, stop=sp), reads=[b_x1T, t.b_wr], cwrites=[t.b_psm])
    P.pending = router_chain(P, g, t, gb, dr)


def router_chain(P, g, t, gb, dr):
    sm = t.sm
    S = [t.b_sm]
    LG = sm[:, 0:144].rearrange("p (a b) -> p a b", a=4)
    gmax = sm[:, 144:148]
    gd = sm[:, 148:164].rearrange("p (a b) -> p a b", a=4)
    gsel = sm[:, 164:180].rearrange("p (a b) -> p a b", a=4)
    gexp = sm[:, 180:196].rearrange("p (a b) -> p a b", a=4)
    gsum = sm[:, 196:200]
    gtop = sm[:, 200:204]
    pen = sm[:, 204:220].rearrange("p (a b) -> p a b", a=4)
    masked = sm[:, 220:348]
    top8 = sm[:, 348:380].rearrange("p (a b) -> p a b", a=4)
    oh1 = sm[:, 380:508].rearrange("p (a b) -> p a b", a=4)
    oh2 = sm[:, 508:636].rearrange("p (a b) -> p a b", a=4)
    dd = sm[:, 636:640]
    ee = sm[:, 640:644]
    den = sm[:, 644:648]
    wtmp = sm[:, 648:656].rearrange("p (a b) -> p a b", a=4)
    rank = sm[:, 656:784].rearrange("p (a b) -> p a b", a=4)
    tmp = sm[:, 784:912].rearrange("p (a b) -> p a b", a=4)
    posf = sm[:, 912:920].rearrange("p (a b) -> p a b", a=4)
    valid = sm[:, 920:928].rearrange("p (a b) -> p a b", a=4)
    tt(P, "dve", LG, t.psm[:, 0:144].rearrange("p (a b) -> p a b", a=4),
       t.bc[:, 2 * D:2 * D + 36].unsqueeze(1).to_broadcast([128, 4, 36]), ALU.add, [t.b_psm, t.b_bc], S)
    yield
    P.op("dve", lambda e: e.tensor_reduce(out=gmax, in_=LG[:, :, 0:4], axis=AX.X, op=ALU.max), reads=S, writes=S)
    yield
    tt(P, "dve", gd, LG[:, :, 0:4], gmax.unsqueeze(2).to_broadcast([128, 4, 4]), ALU.subtract, S, S)
    yield
    ts(P, "dve", gsel, gd, 0.0, None, ALU.is_equal, None, S, S)
    yield
    act(P, gexp, gd, AF.Exp, S, S)
    yield
    P.op("dve", lambda e: e.tensor_reduce(out=gsum, in_=gexp, axis=AX.X, op=ALU.add), reads=S, writes=S)
    yield
    P.op("dve", lambda e: e.reciprocal(out=gtop, in_=gsum), reads=S, writes=S)
    yield
    ts(P, "dve", pen, gsel, 1e30, -1e30, ALU.mult, ALU.add, S, S)
    yield
    tt(P, "dve", masked.rearrange("p (a b c) -> p a b c", a=4, b=4), LG[:, :, 4:36].rearrange("p a (b c) -> p a b c", b=4),
       pen.unsqueeze(3).to_broadcast([128, 4, 4, 8]), ALU.add, S, S)
    yield
    for i in range(4):
        P.op("dve", lambda e, i=i: e.max(out=top8[:, i, :], in_=masked[:, i * 32:(i + 1) * 32]), reads=S, writes=S)
        yield
    m3 = masked.rearrange("p (a b) -> p a b", a=4)
    tt(P, "dve", oh1, m3, top8[:, :, 0:1].to_broadcast([128, 4, 32]), ALU.is_equal, S, S)
    yield
    tt(P, "dve", oh2, m3, top8[:, :, 1:2].to_broadcast([128, 4, 32]), ALU.is_equal, S, S)
    yield
    tt(P, "dve", dd, top8[:, :, 1], top8[:, :, 0], ALU.subtract, S, S)
    yield
    act(P, ee, dd, AF.Exp, S, S)
    yield
    ts(P, "dve", den, ee, 1.0, None, ALU.add, None, S, S)
    yield
    P.op("dve", lambda e: e.reciprocal(out=den, in_=den), reads=S, writes=S)
    yield
    tt(P, "dve", wtmp[:, :, 0], den, gtop, ALU.mult, S, S)
    yield
    tt(P, "dve", wtmp[:, :, 1], wtmp[:, :, 0], ee, ALU.mult, S, S)
    yield
    tt(P, "dve", t.oh[:], oh1, oh2, ALU.add, S, [t.b_oh])
    yield
    for i in range(4):
        o_ = t.psm[:, 256 + i * 32:256 + (i + 1) * 32]
        seq = [(g.U_b, i)] + [(g.ones_b, j) for j in range(i)]
        for n_, (lt, j) in enumerate(seq):
            P.op("pe", lambda e, o_=o_, l=lt[:], r=t.oh[:, j, :], st=(n_ == 0), sp=(n_ == len(seq) - 1):
                 e.matmul(out=o_, lhsT=l, rhs=r, start=st, stop=sp), reads=[g.b_U, g.b_ones, t.b_oh], cwrites=[t.b_psm])
            yield
    for j in range(4):
        P.op("pe", lambda e, j=j: e.matmul(out=t.psm[:, 448:480], lhsT=g.ones_b[:], rhs=t.oh[:, j, :], start=(j == 0), stop=(j == 3)),
             reads=[g.b_ones, t.b_oh], cwrites=[t.b_psm])
        yield
    tt(P, "dve", rank, t.psm[:, 256:384].rearrange("p (a b) -> p a b", a=4), g.runc[:].unsqueeze(1).to_broadcast([128, 4, NE]),
       ALU.add, [t.b_psm, g.b_runc], S)
    yield
    tt(P, "dve", g.runc[:], g.runc[:], t.psm[:, 448:480], ALU.add, [t.b_psm, g.b_runc], [g.b_runc])
    yield
    ts(P, "dve", tmp, rank, float(CAP), 1.0e6, ALU.is_ge, ALU.mult, S, S)
    yield
    tt(P, "dve", rank, rank, tmp, ALU.add, S, S)
    yield
    tt(P, "dve", rank, rank, g.eC[:].unsqueeze(1).to_broadcast([128, 4, NE]), ALU.add, S + [g.b_eC], S)
    yield
    tt(P, "dve", tmp, rank, oh1, ALU.mult, S, S)
    yield
    P.op("dve", lambda e: e.tensor_reduce(out=posf[:, :, 0], in_=tmp, axis=AX.X, op=ALU.add), reads=S, writes=S)
    yield
    tt(P, "dve", tmp, rank, oh2, ALU.mult, S, S)
    yield
    P.op("dve", lambda e: e.tensor_reduce(out=posf[:, :, 1], in_=tmp, axis=AX.X, op=ALU.add), reads=S, writes=S)
    yield
    ts(P, "dve", valid, posf, float(NSLOT) - 0.5, None, ALU.is_lt, None, S, S)
    yield
    tt(P, "dve", g.wts[:, gb * 4:(gb + 1) * 4, :], wtmp, valid, ALU.mult, S, [g.b_wts])
    yield
    cp(P, "dve", g.posi[:, gb * 4:(gb + 1) * 4, :], posf, S, [g.b_posi])
    yield
    Xs = dr["Xs"]
    for i in range(4):
        tile = gb * 4 + i
        for k in range(2):
            P.op("pool", lambda e, k=k, tile=tile, i=i: e.indirect_dma_start(
                out=Xs[:, :], out_offset=bass.IndirectOffsetOnAxis(ap=g.posi[:, tile, k:k + 1], axis=0),
                in_=t.x1b[:, i, :], in_offset=None, bounds_check=P.bound_reg(e), oob_is_err=False),
                reads=[t.b_x1b[i], g.b_posi], cwrites=[g.b_Xs], key="sc%d" % i)
            yield


def load_weight_bf16(P, dst, b_dst, src_ap, n_k, ncols, key):
    first = True
    for c0 in range(0, ncols, 512):
        cw = min(512, ncols - c0)
        P.dma("pool", dst[:, :, c0:c0 + cw], src_ap.rearrange("(c p) n -> p c n", p=128)[:, :, c0:c0 + cw],
              writes=[b_dst] if first else (), cwrites=() if first else [b_dst], key=key)
        first = False


def phase_m0(P, g, dr):
    P.begin_phase()
    if not hasattr(g, "inited"):
        emit_const_init(P, g)
        g.inited = True
    P.op("pool", lambda e: e.memset(g.runc[:], 0.0), writes=[g.b_runc])
    t = alloc_tail(P, g, 0)
    load_tail_consts(P, g, t, dr, 0)
    pv, b_pv = P.sbb([128, 168], F32, "pv0")
    P.dma("sp", pv[:], dr["pv0"][:, :], writes=[b_pv], key="cst")
    win, b_win = P.sbb([128, 8, 2560], BF16, "win")
    wout, b_wout = P.sbb([128, 8, 1024], BF16, "wout")
    load_weight_bf16(P, win, b_win, dr["ab_w_in"], 8, 2560, "win")
    load_weight_bf16(P, wout, b_wout, dr["ab_w_out"], 8, 1024, "wout")
    dg, b_dg = P.sbb([128, 4 * 31, 128], BF16, "diag")
    for j in range(4 * 31):
        ts(P, "pool" if j % 2 else "dve", dg[:, j, :], g.ident_f[:], pv[:, 20 + j:21 + j], None, ALU.mult, None,
           [g.b_ident_f, b_pv], [b_dg] if j == 0 else (), cwrites=() if j == 0 else [b_dg])
    onesM, b_onesM = P.sbb([128, 128], BF16, "onesM")
    P.op("pool", lambda e: e.memset(onesM[:], 1.0 / 512.0), writes=[b_onesM])

    xring = Ring([(P.sb([128, 4, D], F32, "xblk"), [P.buf("xt%d" % i) for i in range(4)]) for _ in range(2)])
    xT, b_xT = P.sbb([128, 8, BLK], BF16, "xT")
    Ain, b_Ain = P.sbb([128, 4, 30 + BLK], BF16, "Ain")
    Bin, b_Bin = P.sbb([128, 4, 2 + BLK], F32, "Bin")
    y32, b_y32 = P.sbb([128, 4, BLK], F32, "y32")
    ybf, b_ybf = P.sbb([128, 4, BLK], BF16, "ybf")
    ysq, b_ysq = P.sbb([128, 4, BLK], BF16, "ysq")
    mean, b_mean = P.sbb([128, BLK], F32, "mean")
    rstd, b_rstd = P.sbb([128, BLK], F32, "rstdA")
    tmpA = Ring([P.sbb([128, BLK], F32, "tmpA") for _ in range(1)])
    sig = tmpA
    acc = Ring([(mean, b_mean)])
    gc = acc
    catT, b_catT = P.sbb([128, 8, BLK], BF16, "catT")
    b_cat = [P.buf("cat%d" % i) for i in range(8)]
    pmm = Ring([P.psb([128, 512], F32, "pmm") for _ in range(3)])
    pst0, b_pst0 = P.psb([128, 512], F32, "pst0")
    pst1, b_pst1 = P.psb([128, 512], F32, "pst1")
    ptr = t.ptr

    x_dr = dr["x"]

    xbf, b_xbf = P.sbb([128, 4, D], BF16, "xbf")

    def load_xbf(gbn):
        P.dma("pool", xbf[:], x_dr[gbn * BLK:(gbn + 1) * BLK, :].rearrange("(i p) d -> p i d", p=128),
              writes=[b_xbf], key="xbf")

    def load_x(gbn):
        xb_, b_ = xring.items[gbn % 2]
        P.dma("sp", xb_[:], x_dr[gbn * BLK:(gbn + 1) * BLK, :].rearrange("(i p) d -> p i d", p=128),
              writes=b_, key="xblk%d" % (gbn % 2))

    for gb in range(NBLK):
        seq_start = (gb % (SEQ // BLK)) == 0
        if gb == 0:
            load_x(0)
        if gb + 1 < NBLK:
            load_x(gb + 1)
        if gb == 0:
            emit_zero_fill(P, g, dr, t)
        xblk, b_xt = xring.items[gb % 2]
        if gb == 0:
            load_xbf(0)
        for c in range(8):
            pt, b_pt = ptr.next()
            ptv = pt[:].bitcast(BF16)
            for i in range(4):
                tr(P, ptv[:, i * 128:(i + 1) * 128], xbf[:, i, c * 128:(c + 1) * 128], g.ident_b[:], i == 0,
                   [b_xbf, g.b_ident_b], b_pt)
            cp(P, "act" if c % 2 == 0 else "dve", xT[:, c, :], ptv[:, 0:512], [b_pt], [b_xT] if c == 0 else (),
               cwrites=() if c == 0 else [b_xT])
        if gb + 1 < NBLK:
            load_xbf(gb + 1)
        if seq_start:
            P.op("pool", lambda e: e.memset(Ain[:, :, 0:30], 0.0), writes=[b_Ain])
            P.op("pool", lambda e: e.memset(Bin[:, :, 0:2], 0.0), writes=[b_Bin])

        def inproj(m):
            pm, b_pm = pmm.next()
            for c in range(8):
                mm(P, pm[:], win[:, c, m * 128:(m + 1) * 128], xT[:, c, :], c == 0, c == 7, [b_win, b_xT], b_pm)
            return pm, b_pm

        for q in range(4):
            pm, b_pm = inproj(4 + q)
            sg, b_sg = sig.next()
            act(P, sg[:], pm[:], AF.Sigmoid, [b_pm, b_pv], [b_sg], bias=pv[:, 4 + q:5 + q])
            pm2, b_pm2 = inproj(q)
            stt(P, Ain[:, q, 30:30 + BLK], pm2[:], pv[:, q:q + 1], sg[:], ALU.add, ALU.mult,
                [b_pm2, b_pv, b_sg], (), cwrites=[b_Ain])
        for q in range(4):
            pm, b_pm = pmm.next()
            for k in range(31):
                mm(P, pm[:], dg[:, q * 31 + k, :], Ain[:, q, k:k + BLK], k == 0, k == 30, [b_dg, b_Ain], b_pm)
            act(P, y32[:, q, :], pm[:], AF.Identity, [b_pm, b_pv], [b_y32] if q == 0 else (), bias=pv[:, 144 + q:145 + q],
                cwrites=() if q == 0 else [b_y32])
            act(P, ysq[:, q, :], pm[:], AF.Square, [b_pm, b_pv], [b_ysq] if q == 0 else (), bias=pv[:, 144 + q:145 + q],
                cwrites=() if q == 0 else [b_ysq])
            cp(P, "dve", ybf[:, q, :], y32[:, q, :], [b_y32], [b_ybf] if q == 0 else (), cwrites=() if q == 0 else [b_ybf])
        cp(P, "pool", Ain[:, :, 0:30], Ain[:, :, BLK:BLK + 30], [b_Ain], [b_Ain])
        for q in range(4):
            mm(P, pst0[:], onesM[:], ybf[:, q, :], q == 0, q == 3, [b_onesM, b_ybf], b_pst0)
        for q in range(4):
            mm(P, pst1[:], onesM[:], ysq[:, q, :], q == 0, q == 3, [b_onesM, b_ysq], b_pst1)
        cp(P, "act", mean[:], pst0[:], [b_pst0], [b_mean])
        tt(P, "dve", rstd[:], mean[:], mean[:], ALU.mult, [b_mean], [b_rstd])
        tt(P, "dve", rstd[:], pst1[:], rstd[:], ALU.subtract, [b_pst1, b_rstd], [b_rstd])
        ts(P, "dve", rstd[:], rstd[:], 0.0, None, ALU.max, None, [b_rstd], [b_rstd])
        act(P, rstd[:], rstd[:], AF.Sqrt, [b_rstd], [b_rstd], bias=EPS)
        P.op("dve", lambda e: e.reciprocal(out=rstd[:], in_=rstd[:]), reads=[b_rstd], writes=[b_rstd])
        for q in range(4):
            tA, b_tA = tmpA.next()
            tt(P, "dve", tA[:], y32[:, q, :], mean[:], ALU.subtract, [b_y32, b_mean], [b_tA])
            tt(P, "pool", tA[:], tA[:], rstd[:], ALU.mult, [b_tA, b_rstd], [b_tA])
            act(P, catT[:, q, :], tA[:], AF.Silu, [b_tA, b_pv], [b_cat[q]], bias=pv[:, 152 + q:153 + q], scale=pv[:, 148 + q:149 + q])
        for q in range(4):
            pm, b_pm = inproj(12 + q)
            gcq, b_gc = gc.next()
            act(P, gcq[:], pm[:], AF.Identity, [b_pm, b_pv], [b_gc], bias=pv[:, 12 + q:13 + q])
            pm2, b_pm2 = inproj(16 + q)
            stt(P, Bin[:, q, 2:2 + BLK], pm2[:], pv[:, 16 + q:17 + q], gcq[:], ALU.add, ALU.mult,
                [b_pm2, b_pv, b_gc], (), cwrites=[b_Bin])
            ac, b_ac = acc.next()
            ts(P, "dve", ac[:], Bin[:, q, 0:BLK], pv[:, 156 + q * 3:157 + q * 3], None, ALU.mult, None, [b_Bin, b_pv], [b_ac])
            stt(P, ac[:], Bin[:, q, 1:1 + BLK], pv[:, 157 + q * 3:158 + q * 3], ac[:], ALU.mult, ALU.add, [b_Bin, b_pv, b_ac], [b_ac])
            stt(P, ac[:], Bin[:, q, 2:2 + BLK], pv[:, 158 + q * 3:159 + q * 3], ac[:], ALU.mult, ALU.add, [b_Bin, b_pv, b_ac], [b_ac])
            pm3, b_pm3 = inproj(8 + q)
            stt(P, catT[:, 4 + q, :], pm3[:], pv[:, 8 + q:9 + q], ac[:], ALU.add, ALU.mult, [b_pm3, b_pv, b_ac], [b_cat[4 + q]])
        cp(P, "pool", Bin[:, :, 0:2], Bin[:, :, BLK:BLK + 2], [b_Bin], [b_Bin])
        emit_tail_block(P, g, t, xblk, b_xt, catT, b_cat, wout, b_wout, pmm, gb, dr)
    P.end_phase()


def phase_m1(P, g, dr):
    P.begin_phase()
    if not hasattr(g, "inited"):
        emit_const_init(P, g)
        g.inited = True
    P.op("pool", lambda e: e.memset(g.runc[:], 0.0), writes=[g.b_runc])
    t = alloc_tail(P, g, 1)
    load_tail_consts(P, g, t, dr, 1)
    pv, b_pv = P.sbb([128, 12], F32, "pv1")
    P.dma("sp", pv[:], dr["pv1"][:, :], writes=[b_pv], key="cst")
    bx, b_bx = P.sbb([128, 2048], F32, "bc1x")
    P.dma("sp", bx[:], dr["bc1x"][:, :], writes=[b_bx], key="cst")
    win, b_win = P.sbb([128, 8, 2560], BF16, "win")
    wout, b_wout = P.sbb([128, 8, 1024], BF16, "wout")
    load_weight_bf16(P, win, b_win, dr["cd_w_in"], 8, 2560, "win")
    load_weight_bf16(P, wout, b_wout, dr["cd_w_out"], 8, 1024, "wout")
    wsT, b_wsT = P.sbb([128, 8, 128], BF16, "wsT")
    P.dma("pool", wsT[:].rearrange("p h i -> p (h i)"), dr["wsT"][:, :], writes=[b_wsT], key="wsT")
    P.op("pool", lambda e: e.memset(wsT[64:128, :, 0:64], 0.0), writes=[b_wsT])
    wsb, b_wsb = P.sbb([128, 4, 128], F32, "wsb")
    P.dma("sp", wsb[:].rearrange("p a b -> p (a b)"), dr["wsb"][:, :], writes=[b_wsb], key="cst")
    relP, b_relP = P.sbb([128, 8, 640], F32, "relP")
    P.dma("sp", relP[:].rearrange("p a b -> p (a b)"), dr["relP"][:, :], writes=[b_relP], key="cst")

    xblk = P.sb([128, 4, D], F32, "xblk")
    b_xt = [P.buf("xt%d" % i) for i in range(4)]
    xT, b_xT = P.sbb([128, 8, BLK], BF16, "xT")
    vt, b_vt = P.sbb([128, 512], F32, "vt")
    vn, b_vn = P.sbb([128, 4, 512], BF16, "vn")
    st1, b_st1 = P.sbb([128, 6], F32, "st1")
    mv1, b_mv1 = P.sbb([128, 2], F32, "mv1")
    rs1, b_rs1 = P.sbb([128, 1], F32, "rs1")
    qz, b_qz = P.sbb([128, 8, BLK], BF16, "qz")
    P.op("pool", lambda e: e.memset(qz[:].rearrange("p a b -> p (a b)"), 0.0), writes=[b_qz])
    osb, b_osb = (None, None) if ATT_T else P.sbb([128, 264], F32, "osb")
    rden, b_rden = P.sbb([128, 512], F32, "rden")
    kT = P.sb([128, 4, SEQ], BF16, "kT")
    b_kT = [P.buf("kT%d" % j) for j in range(4)]
    Va = P.sb([128, 16, 8, 64 if ATT_T else 66], BF16, "Va")
    b_Va = [P.buf("Va%d" % j) for j in range(4)]
    gt, b_gt = P.sbb([128, 512], F32, "gt")
    catT = P.sb([128, 8, BLK], BF16, "catT")
    b_cat = [P.buf("cat%d" % i) for i in range(8)]
    sT = Ring([P.sbb([128, 640], F32, "sT") for _ in range(2)])
    pT = Ring([P.sbb([128, 640], BF16, "pT") for _ in range(2)])
    otok, b_otok = (None, None) if ATT_T else P.sbb([128, 512], F32, "otok")
    rcp, b_rcp = (None, None) if ATT_T else P.sbb([128, 8], F32, "rcp")
    pmm = Ring([P.psb([128, 512], F32, "pmm") for _ in range(3)])
    pO = [P.psb([128, 512], F32, "pO") for _ in range(2)]
    ptr = t.ptr
    spairs = Ring([(pmm.items[0], pmm.items[1]), (pmm.items[2], ptr.items[0])])
    P.op("pool", lambda e: e.memset(Va[:].rearrange("p a b c -> p (a b c)"), 1.0), writes=b_Va)

    x_dr = dr["X2"]
    for gb in range(NBLK):
        jj = gb % 4
        P.dma("sp", xblk[:], x_dr[gb * BLK:(gb + 1) * BLK, :].rearrange("(i p) d -> p i d", p=128),
              reads=[g.b_X2], writes=b_xt, key="xblk0")
        if gb == 0:
            emit_zero_fill(P, g, dr, t)
        for c in range(8):
            pt, b_pt = ptr.next()
            for i in range(4):
                tr(P, pt[:, i * 128:(i + 1) * 128], xblk[:, i, c * 128:(c + 1) * 128], g.ident_f[:], i == 0,
                   [b_xt[i], g.b_ident_f], b_pt)
            cp(P, "act" if c % 2 == 0 else "dve", xT[:, c, :], pt[:], [b_pt], [b_xT] if c == 0 else (),
               cwrites=() if c == 0 else [b_xT])

        def inproj_fm(m):
            pm, b_pm = pmm.next()
            for c in range(8):
                mm(P, pm[:], win[:, c, m * 128:(m + 1) * 128], xT[:, c, :], c == 0, c == 7, [b_win, b_xT], b_pm)
            return pm, b_pm

        def inproj_tm(i, col0):
            pm, b_pm = pmm.next()
            for c in range(8):
                mm(P, pm[:], xT[:, c, i * 128:(i + 1) * 128], win[:, c, col0:col0 + 512], c == 0, c == 7, [b_win, b_xT], b_pm)
            return pm, b_pm

        for i in range(4):
            pm, b_pm = inproj_tm(i, 512)
            tt(P, "dve", vt[:], pm[:], bx[:, 0:512], ALU.add, [b_pm, b_bx], [b_vt])
            P.op("dve", lambda e: e.bn_stats(out=st1[:], in_=vt[:]), reads=[b_vt], writes=[b_st1])
            P.op("dve", lambda e: e.bn_aggr(out=mv1[:], in_=st1[:]), reads=[b_st1], writes=[b_mv1])
            act(P, rs1[:], mv1[:, 1:2], AF.Sqrt, [b_mv1], [b_rs1], bias=EPS)
            P.op("dve", lambda e: e.reciprocal(out=rs1[:], in_=rs1[:]), reads=[b_rs1], writes=[b_rs1])
            ts(P, "dve", vt[:], vt[:], mv1[:, 0:1], rs1[:, 0:1], ALU.subtract, ALU.mult, [b_vt, b_mv1, b_rs1], [b_vt])
            tt(P, "pool", vt[:], vt[:], bx[:, 512:1024], ALU.mult, [b_vt, b_bx], [b_vt])
            tt(P, "pool", vn[:, i, :], vt[:], bx[:, 1024:1536], ALU.add, [b_vt, b_bx], [b_vn] if i == 0 else (),
               cwrites=() if i == 0 else [b_vn])
        for i in range(4):
            tl = jj * 4 + i
            pm, b_pm = inproj_tm(i, 2048)
            tt(P, "dve", Va[:, tl, :, 0:64], pm[:].rearrange("p (h c) -> p h c", h=8),
               bx[:, 1536:2048].rearrange("p (h c) -> p h c", h=8), ALU.add, [b_pm, b_bx],
               [b_Va[jj]] if i == 0 else (), cwrites=() if i == 0 else [b_Va[jj]])
        for qc in range(4):
            pm, b_pm = inproj_fm(12 + qc)
            act(P, kT[:, qc, jj * BLK:(jj + 1) * BLK], pm[:], AF.Identity, [b_pm, b_pv], [b_kT[jj]] if qc == 0 else (),
                bias=pv[:, 8 + qc:9 + qc], cwrites=() if qc == 0 else [b_kT[jj]])
        for qc in range(4):
            pm, b_pm = inproj_fm(8 + qc)
            for hh in range(2):
                ps_ = slice(hh * 64, (hh + 1) * 64)
                ts(P, "dve", qz[ps_, 2 * qc + hh, :], pm[ps_, :], pv[ps_, 4 + qc:5 + qc], 0.125, ALU.add, ALU.mult,
                   [b_pm, b_pv], [b_qz] if (qc == 0 and hh == 0) else (), cwrites=() if (qc == 0 and hh == 0) else [b_qz])
        if "sgu" in SKIP:
            for qc in range(4):
                P.op("pool", lambda e, qc=qc: e.memset(catT[:, qc, :], 0.0), writes=[b_cat[qc]])
        for qc in ([] if "sgu" in SKIP else range(4)):
            pgm, b_pgm = pmm.next()
            first = True
            for hh in range(2):
                h = 2 * qc + hh
                for i in range(4):
                    outap = pgm[hh * 64:(hh + 1) * 64, i * 128:(i + 1) * 128]
                    lhsT = vn[:, i, h * 64:(h + 1) * 64]
                    rhs = wsT[:, h, :]
                    if first:
                        P.op("pe", lambda e, o=outap, l=lhsT, r=rhs: e.matmul(out=o, lhsT=l, rhs=r, start=True, stop=True),
                             reads=[b_vn, b_wsT], writes=[b_pgm])
                        first = False
                    else:
                        P.op("pe", lambda e, o=outap, l=lhsT, r=rhs: e.matmul(out=o, lhsT=l, rhs=r, start=True, stop=True),
                             reads=[b_vn, b_wsT], cwrites=[b_pgm])
            tt(P, "dve", gt[:].rearrange("p (a b) -> p a b", a=4), pgm[:].rearrange("p (a b) -> p a b", a=4),
               wsb[:, qc, :].unsqueeze(1).to_broadcast([128, 4, 128]), ALU.add, [b_pgm, b_wsb], [b_gt])
            pm, b_pm = inproj_fm(qc)
            stt(P, catT[:, qc, :], pm[:], pv[:, qc:qc + 1], gt[:], ALU.add, ALU.mult, [b_pm, b_pv, b_gt], [b_cat[qc]])
        if "attn" in SKIP:
            for qc in range(4):
                P.op("pool", lambda e, qc=qc: e.memset(catT[:, 4 + qc, :], 0.0), writes=[b_cat[4 + qc]])
        for pr in ([] if "attn" in SKIP else range(4)):
            t0 = jj * 4 + pr
            slots = [s_ for s_ in range(5) if t0 - 4 + s_ >= 0]
            kbufs = list({b_kT[(t0 - 4 + s_) // 4] for s_ in slots})
            vbufs = list({b_Va[(t0 - 4 + s_) // 4] for s_ in slots})
            lo = slots[0]
            pend = []

            def flush_pv():
                while pend:
                    pend.pop(0)()

            for h in range(8):
                qc = h // 2
                (pa, b_pa), (pb, b_pb) = spairs.next()
                firstA = True
                for s_ in slots:
                    tk = t0 - 4 + s_
                    lhsT = kT[:, qc, tk * 128:(tk + 1) * 128]
                    rhs = qz[:, h, pr * 128:(pr + 1) * 128]
                    if s_ < 4:
                        outap, bb = pa[:, s_ * 128:(s_ + 1) * 128], b_pa
                        fw = firstA
                        firstA = False
                    else:
                        outap, bb = pb[:, 0:128], b_pb
                        fw = True
                    P.op("pe", lambda e, o=outap, l=lhsT, r=rhs: e.matmul(out=o, lhsT=l, rhs=r, start=True, stop=True),
                         reads=kbufs + [b_qz], writes=[bb] if fw else (), cwrites=() if fw else [bb])
                sTt, b_sT = sT.next()
                pTt, b_pT = pT.next()
                if lo < 4:
                    tt(P, "dve", sTt[:, lo * 128:512], pa[:, lo * 128:512], relP[:, h, lo * 128:512], ALU.add,
                       [b_pa, b_relP], [b_sT])
                    tt(P, "dve", sTt[:, 512:640], pb[:, 0:128], relP[:, h, 512:640], ALU.add, [b_pb, b_relP], (), cwrites=[b_sT])
                else:
                    tt(P, "dve", sTt[:, 512:640], pb[:, 0:128], relP[:, h, 512:640], ALU.add, [b_pb, b_relP], [b_sT])
                act(P, pTt[:, lo * 128:640], sTt[:, lo * 128:640], AF.Exp, [b_sT], [b_pT])
                if ATT_T:
                    def pv_fn(h=h, qc=qc, pTt=pTt, b_pT=b_pT):
                        po = (h % 2) * 64
                        (pN, b_pN), (pD, b_pD) = pO[0], pO[1]
                        for which, (pX, b_pX) in enumerate(((pN, b_pN), (pD, b_pD))):
                            for oi, s_ in enumerate(slots):
                                tk = t0 - 4 + s_
                                lhsT = Va[:, tk, h, 0:64] if which == 0 else g.ones_b[:, 0:64]
                                fw = (h == 0 and oi == 0)
                                P.op("pe", lambda e, o=pX[po:po + 64, qc * 128:(qc + 1) * 128], l=lhsT,
                                     r=pTt[:, s_ * 128:(s_ + 1) * 128], st=(oi == 0), sp=(oi == len(slots) - 1):
                                     e.matmul(out=o, lhsT=l, rhs=r, start=st, stop=sp),
                                     reads=(vbufs if which == 0 else [g.b_ones]) + [b_pT],
                                     writes=[b_pX] if fw else (), cwrites=() if fw else [b_pX])
                    flush_pv()
                    pend.append(pv_fn)
                    if h == 7:
                        flush_pv()
                    continue
                pOt, b_pO = pO[h // 4]
                c0 = (h % 4) * 66
                if "pv" in SKIP:
                    continue
                for oi, s_ in enumerate(slots):
                    tk = t0 - 4 + s_
                    fw = (h % 4 == 0 and oi == 0)
                    P.op("pe", lambda e, o=pOt[:, c0:c0 + 66], l=pTt[:, s_ * 128:(s_ + 1) * 128], r=Va[:, tk, h, :],
                         st=(oi == 0), sp=(oi == len(slots) - 1): e.matmul(out=o, lhsT=l, rhs=r, start=st, stop=sp),
                         reads=vbufs + [b_pT], writes=[b_pO] if fw else (), cwrites=() if fw else [b_pO])
                if h % 4 == 3 and "norm" not in SKIP:
                    hb = h // 4
                    cp(P, "act", osb[:], pOt[:, 0:264], [b_pO], [b_osb])
                    ov3 = osb[:].rearrange("p (h c) -> p h c", c=66)
                    P.op("dve", lambda e, ov3=ov3, hb=hb: e.reciprocal(out=rcp[:, hb * 4:(hb + 1) * 4], in_=ov3[:, :, 64]),
                         reads=[b_osb], writes=[b_rcp])
                    tt(P, "dve", otok[:, hb * 256:(hb + 1) * 256].rearrange("p (h c) -> p h c", c=64), ov3[:, :, 0:64],
                       rcp[:, hb * 4:(hb + 1) * 4].unsqueeze(2).to_broadcast([128, 4, 64]), ALU.mult, [b_osb, b_rcp],
                       [b_otok] if hb == 0 else (), cwrites=() if hb == 0 else [b_otok])
            if ATT_T:
                (pN, b_pN), (pD, b_pD) = pO[0], pO[1]
                P.op("dve", lambda e, pD=pD: e.reciprocal(out=rden[:], in_=pD[:]), reads=[b_pD], writes=[b_rden])
                for qc in range(4):
                    tt(P, "dve", catT[:, 4 + qc, pr * 128:(pr + 1) * 128], pN[:, qc * 128:(qc + 1) * 128],
                       rden[:, qc * 128:(qc + 1) * 128], ALU.mult, [b_pN, b_rden],
                       [b_cat[4 + qc]] if pr == 0 else (), cwrites=() if pr == 0 else [b_cat[4 + qc]])
                continue
            if "pv" in SKIP or "norm" in SKIP:
                if pr == 0:
                    for qc in range(4):
                        P.op("pool", lambda e, qc=qc: e.memset(catT[:, 4 + qc, :], 0.0), writes=[b_cat[4 + qc]])
                continue
            pt, b_pt = ptr.next()
            for qc in range(4):
                tr(P, pt[:, qc * 128:(qc + 1) * 128], otok[:, qc * 128:(qc + 1) * 128], g.ident_f[:], qc == 0,
                   [b_otok, g.b_ident_f], b_pt)
            for qc in range(4):
                cp(P, "act" if qc % 2 == 0 else "dve", catT[:, 4 + qc, pr * 128:(pr + 1) * 128], pt[:, qc * 128:(qc + 1) * 128],
                   [b_pt], [b_cat[4 + qc]] if pr == 0 else (), cwrites=() if pr == 0 else [b_cat[4 + qc]])
        emit_tail_block(P, g, t, xblk, b_xt, catT, b_cat, wout, b_wout, pmm, gb, dr)
    P.end_phase()

def phase_e(P, g, dr, L):
    P.begin_phase()
    NI = CAP // 128
    NW = 3
    sets = []
    for i in range(NW):
        sets.append((P.sbb([128, 8, DEX], BF16, "wg"), P.sbb([128, 8, DEX], BF16, "wu"), P.sbb([128, 4, D], BF16, "wd"),
                     P.sbb([128, NI, D], BF16, "xe")))
    xeT, b_xeT = P.sbb([128, 8, CAP], BF16, "xeT")
    sgt = Ring([P.sbb([128, CAP], F32, "sgt") for _ in range(2)])
    hT, b_hT = P.sbb([128, 4, CAP], BF16, "hT")
    yt = Ring([P.sbb([128, D], F32, "yt") for _ in range(3)])
    ptb = Ring([P.psb([128, 512], BF16, "ptb") for _ in range(2)])
    pg = Ring([P.psb([128, 512], F32, "pg") for _ in range(2)])
    pu = Ring([P.psb([128, 512], F32, "pu") for _ in range(2)])
    py = Ring([P.psb([128, 512], F32, "py") for _ in range(2)])
    Wg, Wu, Wd = dr["moe_w_gate"], dr["moe_w_up"], dr["moe_w_down"]
    Xs, Ys = dr["Xs"], dr["Ys"]

    def issue(ex):
        s_ = ex % NW
        (wgt, b_wg), (wut, b_wu), (wdt, b_wd), (xet, b_xe) = sets[s_]
        P.dma("sp", xet[:], Xs[ex * CAP:(ex + 1) * CAP, :].rearrange("(i p) d -> p i d", p=128),
              reads=[g.b_Xs], writes=[b_xe], key="xe%d" % s_)
        load_weight_bf16(P, wgt, b_wg, Wg[L, ex], 8, DEX, "wg%d" % s_)
        load_weight_bf16(P, wut, b_wu, Wu[L, ex], 8, DEX, "wu%d" % s_)
        load_weight_bf16(P, wdt, b_wd, Wd[L, ex], 4, D, "wd%d" % s_)

    for ex in range(min(NW - 1, NE)):
        issue(ex)
    for ex in range(NE):
        if ex + NW - 1 < NE:
            issue(ex + NW - 1)
        (wgt, b_wg), (wut, b_wu), (wdt, b_wd), (xet, b_xe) = sets[ex % NW]
        for c in range(8):
            pt, b_pt = ptb.next()
            for i in range(NI):
                tr(P, pt[:, i * 128:(i + 1) * 128], xet[:, i, c * 128:(c + 1) * 128], g.ident_b[:], i == 0,
                   [b_xe, g.b_ident_b], b_pt)
            cp(P, "act" if c % 2 == 0 else "dve", xeT[:, c, :], pt[:, 0:CAP], [b_pt], [b_xeT] if c == 0 else (),
               cwrites=() if c == 0 else [b_xeT])
        for m in range(4):
            pgt, b_pg = pg.next()
            put, b_pu = pu.next()
            for c in range(8):
                mm(P, pgt[:, 0:CAP], wgt[:, c, m * 128:(m + 1) * 128], xeT[:, c, :], c == 0, c == 7, [b_wg, b_xeT], b_pg)
            for c in range(8):
                mm(P, put[:, 0:CAP], wut[:, c, m * 128:(m + 1) * 128], xeT[:, c, :], c == 0, c == 7, [b_wu, b_xeT], b_pu)
            sg, b_sg = sgt.next()
            act(P, sg[:], pgt[:, 0:CAP], AF.Silu, [b_pg], [b_sg])
            tt(P, "dve", hT[:, m, :], sg[:], put[:, 0:CAP], ALU.mult, [b_sg, b_pu], [b_hT] if m == 0 else (),
               cwrites=() if m == 0 else [b_hT])
        for i in range(NI):
            ytile, b_yt = yt.next()
            for h in range(2):
                pyt, b_py = py.next()
                for m in range(4):
                    mm(P, pyt[:], hT[:, m, i * 128:(i + 1) * 128], wdt[:, m, h * 512:(h + 1) * 512], m == 0, m == 3,
                       [b_hT, b_wd], b_py)
                cp(P, "act" if h == 0 else "dve", ytile[:, h * 512:(h + 1) * 512], pyt[:], [b_py],
                   [b_yt] if h == 0 else (), cwrites=() if h == 0 else [b_yt])
            r0 = ex * CAP + i * 128
            P.dma("sp", Ys[r0:r0 + 128, :], ytile[:], reads=[b_yt], cwrites=[g.b_Ys], key="yst%d" % (yt.i % 3))
    P.end_phase()


def phase_c(P, g, dr, L, out_ap, b_out):
    P.begin_phase()
    t = G()
    t.bc, t.b_bc = P.sbb([128, 2 * D], F32, "bc")
    P.dma("sp", t.bc[:], dr["bcf%d" % L][:, :], writes=[t.b_bc], key="cst")
    t.lnr = Ring([make_ln_scratch(P) for _ in range(3)])
    NR = 4
    x1r = [P.sbb([128, D], F32, "x1c") for _ in range(NR)]
    y0r = [P.sbb([128, D], F32, "y0") for _ in range(NR)]
    y1r = [P.sbb([128, D], F32, "y1") for _ in range(NR)]
    rr = Ring([P.sbb([128, D], F32, "rc") for _ in range(2)])
    orr = Ring([P.sbb([128, D], F32, "oc") for _ in range(3)])
    Ys = dr["Ys"]
    for (yy, b_yy) in y0r + y1r:
        P.op("pool", lambda e, yy=yy: e.memset(yy[:], 0.0), writes=[b_yy])

    def issue_loads(tile):
        s_ = tile % NR
        x1, b_x1 = x1r[s_]
        P.dma("sp", x1[:], dr["X1"][tile * 128:(tile + 1) * 128, :], reads=[g.b_X1], writes=[b_x1], key="cx%d" % s_)
        for k, (yy, b_yy) in enumerate((y0r[s_], y1r[s_])):
            P.op("pool", lambda e, yy=yy, k=k, tile=tile: e.indirect_dma_start(
                out=yy[:], out_offset=None, in_=Ys[:, :],
                in_offset=bass.IndirectOffsetOnAxis(ap=g.posi[:, tile, k:k + 1], axis=0),
                bounds_check=P.bound_reg(e), oob_is_err=False),
                reads=[g.b_Ys, g.b_posi], writes=[b_yy], key="cy%d_%d" % (k, s_))

    NPRE = 3
    for tile in range(min(NPRE, NT)):
        issue_loads(tile)
    for tile in range(NT):
        if tile + NPRE < NT:
            issue_loads(tile + NPRE)
        s_ = tile % NR
        x1, b_x1 = x1r[s_]
        y0, b_y0 = y0r[s_]
        y1, b_y1 = y1r[s_]
        r, b_r = rr.next()
        act(P, r[:], x1[:], AF.Copy, [b_x1], [b_r], scale=ALPHA)
        stt(P, r[:], y0[:], g.wts[:, tile, 0:1], r[:], ALU.mult, ALU.add, [b_y0, g.b_wts, b_r], [b_r])
        stt(P, r[:], y1[:], g.wts[:, tile, 1:2], r[:], ALU.mult, ALU.add, [b_y1, g.b_wts, b_r], [b_r])
        o, b_o = orr.next()
        emit_ln_rows(P, t, r[:], b_r, o[:], b_o, 0, D)
        P.dma("sp", out_ap[tile * 128:(tile + 1) * 128, :], o[:], reads=[b_o], cwrites=[b_out], key="co%d" % (orr.i % 3))
    P.end_phase()


def build_program(n_layers=1, debug=False):
    nc = bass.Bass("TRN2", target_bir_lowering=False)
    dr = {}

    def din(name, shape, dt=F32):
        dr[name] = nc.dram_tensor(name, list(shape), dt, kind="ExternalInput").ap()

    din("x", [T, D])
    din("ab_w_in", [D, 2560])
    din("ab_w_out", [D, D])
    din("pv0", [128, 168])
    din("cd_w_in", [D, 2560])
    din("cd_w_out", [D, D])
    din("pv1", [128, 12])
    din("bc1x", [128, 2048])
    din("wsT", [128, 1024])
    din("wsb", [128, 512])
    din("relP", [128, 8 * 640])
    for L in range(2):
        din("bcm%d" % L, [128, 2 * D + 36])
        din("bcf%d" % L, [128, 2 * D])
        din("wr%d" % L, [D, 36])
    din("moe_w_gate", [2, NE, D, DEX])
    din("moe_w_up", [2, NE, D, DEX])
    din("moe_w_down", [2, NE, DEX, D])
    dr["out"] = nc.dram_tensor("out", [T, D], F32, kind="ExternalOutput").ap()
    kind = "ExternalOutput" if debug else "Internal"
    dr["X1"] = nc.dram_tensor("X1", [T, D], F32, kind=kind).ap()
    dr["Xs"] = nc.dram_tensor("Xs", [NSLOT, D], BF16, kind=kind).ap()
    dr["Ys"] = nc.dram_tensor("Ys", [NSLOT, D], F32, kind=kind).ap()
    dr["X2"] = nc.dram_tensor("X2", [T, D], F32, kind="Internal").ap()
    P = Prog(nc)
    g = G()
    setup_globals(P, g)
    g.b_X1 = P.buf("X1")
    g.b_Xs = P.buf("Xs")
    g.b_Ys = P.buf("Ys")
    g.b_X2 = P.buf("X2")
    g.b_out = P.buf("out")
    phase_m0(P, g, dr)
    phase_e(P, g, dr, 0)
    if n_layers == 1:
        phase_c(P, g, dr, 0, dr["out"], g.b_out)
    else:
        phase_c(P, g, dr, 0, dr["X2"], g.b_X2)
        phase_m1(P, g, dr)
        phase_e(P, g, dr, 1)
        phase_c(P, g, dr, 1, dr["out"], g.b_out)
    if debug:
        dr["posd"] = nc.dram_tensor("posd", [128, NT * 2], I32, kind="ExternalOutput").ap()
        dr["wtsd"] = nc.dram_tensor("wtsd", [128, NT * 2], F32, kind="ExternalOutput").ap()
        P.begin_phase()
        P.dma("sp", dr["posd"][:, :], g.posi[:].rearrange("p a b -> p (a b)"), reads=[g.b_posi], key="dbg")
        P.dma("sp", dr["wtsd"][:, :], g.wts[:].rearrange("p a b -> p (a b)"), reads=[g.b_wts], key="dbg")
        P.end_phase()
    P.finish()
    return nc, P


def prep_inputs(inp):
    f = lambda a: np.ascontiguousarray(np.asarray(a, dtype=np.float32))
    shared = {}
    shared["ab_w_in"] = f(inp["ab_w_in"][0])
    shared["ab_w_out"] = f(inp["ab_w_out"][0])
    pv0 = np.zeros((128, 168), np.float32)
    pv0[:, 0:20] = f(inp["ab_b_in"][0]).reshape(20, 128).T
    adw = f(inp["a_dw"][0])
    pv0[:, 20:144] = adw.reshape(31, 4, 128).transpose(2, 1, 0).reshape(128, 124)
    pv0[:, 144:148] = f(inp["a_dw_b"][0]).reshape(4, 128).T
    pv0[:, 148:152] = f(inp["a_ln_g"][0]).reshape(4, 128).T
    pv0[:, 152:156] = f(inp["a_ln_b"][0]).reshape(4, 128).T
    bdw = f(inp["b_dw"][0])
    pv0[:, 156:168] = bdw.reshape(3, 4, 128).transpose(2, 1, 0).reshape(128, 12)
    shared["pv0"] = pv0
    shared["cd_w_in"] = f(inp["cd_w_in"][0])
    shared["cd_w_out"] = f(inp["cd_w_out"][0])
    cb = f(inp["cd_b_in"][0])
    pv1 = np.zeros((128, 12), np.float32)
    pv1[:, 0:4] = cb[0:512].reshape(4, 128).T
    pv1[:, 4:8] = cb[1024:1536].reshape(4, 128).T
    pv1[:, 8:12] = cb[1536:2048].reshape(4, 128).T
    shared["pv1"] = pv1
    row = np.concatenate([cb[512:1024], f(inp["c_ln_g"][0]), f(inp["c_ln_b"][0]), cb[2048:2560]])
    shared["bc1x"] = np.ascontiguousarray(np.broadcast_to(row[None, :], (128, 2048)))
    ws = f(inp["c_ws"][0])
    shared["wsT"] = np.ascontiguousarray(ws.transpose(2, 0, 1).reshape(128, 1024))
    wb = f(inp["c_ws_b"][0])
    shared["wsb"] = np.ascontiguousarray(wb.reshape(4, 2, 1, 128).repeat(64, axis=2).transpose(1, 2, 0, 3).reshape(128, 512))
    rb = f(inp["d_rel_bias"][0])
    p_ = np.arange(128)[:, None, None]
    s_ = np.arange(5)[None, :, None]
    c_ = np.arange(128)[None, None, :]
    par = c_ // 64
    i_ = c_ % 64
    delta = 64 * (8 + par - 2 * s_) + i_ - p_
    ridx = np.clip(delta, -256, 256) + 256
    cdist = 8 - 2 * s_ + par - (p_ // 64)
    valid = (cdist >= 0) & (cdist <= 8)
    relv = np.where(valid[None], rb[:, ridx], np.float32(-30000.0)).astype(np.float32)
    shared["relP"] = np.ascontiguousarray(relv.transpose(1, 0, 2, 3).reshape(128, 8 * 640))
    for L in range(2):
        row = np.concatenate([f(inp["mix_ln_g"][L]), f(inp["mix_ln_b"][L]), f(inp["moe_rg_b"][L]), f(inp["moe_re_b"][L]).reshape(-1)])
        shared["bcm%d" % L] = np.ascontiguousarray(np.broadcast_to(row[None, :], (128, row.size)))
        row = np.concatenate([f(inp["ffn_ln_g"][L]), f(inp["ffn_ln_b"][L])])
        shared["bcf%d" % L] = np.ascontiguousarray(np.broadcast_to(row[None, :], (128, row.size)))
        shared["wr%d" % L] = np.ascontiguousarray(np.concatenate(
            [f(inp["moe_rg_w"][L]), f(inp["moe_re_w"][L]).transpose(1, 0, 2).reshape(D, 32)], axis=1))
    shared["moe_w_gate"] = f(inp["moe_w_gate"])
    shared["moe_w_up"] = f(inp["moe_w_up"])
    shared["moe_w_down"] = f(inp["moe_w_down"])
    x = f(inp["x"]).reshape(NCORES, T, D)
    return shared, x


_CACHE = {}


def kernel(**inputs):
    shared, x = prep_inputs(inputs)
    if "nc" not in _CACHE:
        _CACHE["nc"] = build_program(n_layers=2)[0]
    nc = _CACHE["nc"]
    in_maps = []
    for c in range(NCORES):
        m = dict(shared)
        m["x"] = x[c]
        in_maps.append(m)
    res = run_bass_kernel_spmd(nc, in_maps, core_ids=list(range(NCORES)))
    out = np.stack([np.asarray(r["out"]) for r in res.results], 0)
    return out.reshape(16, SEQ, D).astype(np.float32)
```

```python
import numpy as np
from contextlib import ExitStack
import concourse.bass as bass
import concourse.mybir as mybir
from concourse.bass_utils import run_bass_kernel_spmd

F32 = mybir.dt.float32
BF16 = mybir.dt.bfloat16
I32 = mybir.dt.int32
AF = mybir.ActivationFunctionType
ALU = mybir.AluOpType
AX = mybir.AxisListType

NCORES = 8
D = 1024
SEQ = 2048
BPC = 2
T = BPC * SEQ
NT = T // 128
BLK = 512
NBLK = T // BLK
NE = 32
CAP = 384
NSLOT = NE * CAP
ALPHA = float(4 ** 0.25)
EPS = 1e-5
DEX = 512
import os
SKIP = set(os.environ.get("M1_SKIP", "").split(","))
ATT_T = os.environ.get("ATT_T", "1") == "1"


class Buf:
    __slots__ = ("name", "xw", "cw", "readers")

    def __init__(self, name=""):
        self.name = name
        self.xw = []
        self.cw = []
        self.readers = []

    def reset(self):
        self.xw = []
        self.cw = []
        self.readers = []


class Op:
    __slots__ = ("eng", "fn", "reads", "writes", "cwrites", "key", "deps", "signal", "seq", "kcount", "idx")

    def __init__(self, eng, fn, reads, writes, cwrites, key):
        self.eng = eng
        self.fn = fn
        self.reads = reads
        self.writes = writes
        self.cwrites = cwrites
        self.key = key
        self.deps = None
        self.signal = False
        self.seq = None
        self.kcount = None


class Ring:
    def __init__(self, items):
        self.items = items
        self.i = 0

    def next(self):
        it = self.items[self.i % len(self.items)]
        self.i += 1
        return it


class Prog:
    ENGS = ("pe", "act", "dve", "pool", "sp")

    def __init__(self, nc):
        self.nc = nc
        self.gstack = ExitStack()
        self.pstack = None
        self.ops = []
        self.bufs = []
        self.sems = {}
        self.eng_seq = {e: 0 for e in self.ENGS}
        self.key_count = {}
        self.phase_no = 0
        self.total_ops = 0
        self.uid = 0
        self.pending = None
        self._in_chain = False

    def sb(self, shape, dtype, name=None, glob=False):
        self.uid += 1
        st = self.gstack if glob else self.pstack
        return st.enter_context(self.nc.sbuf_tensor("%s_%d" % (name or "t", self.uid), list(shape), dtype))

    def ps(self, shape, dtype=F32, name=None):
        self.uid += 1
        return self.pstack.enter_context(self.nc.psum_tensor("%s_%d" % (name or "p", self.uid), list(shape), dtype))

    def buf(self, name=""):
        b = Buf(name)
        self.bufs.append(b)
        return b

    def sbb(self, shape, dtype, name=None, glob=False):
        return self.sb(shape, dtype, name, glob), self.buf(name or "")

    def psb(self, shape, dtype=F32, name=None):
        return self.ps(shape, dtype, name), self.buf(name or "")

    def sem(self, name):
        if name not in self.sems:
            self.sems[name] = self.gstack.enter_context(self.nc.semaphore(name))
        return self.sems[name]

    def op(self, eng, fn, reads=(), writes=(), cwrites=(), key=None):
        o = Op(eng, fn, tuple(reads), tuple(writes), tuple(cwrites), key)
        o.idx = len(self.ops)
        self.ops.append(o)
        if self.pending is not None and not self._in_chain and eng == "dve":
            self._in_chain = True
            try:
                next(self.pending)
            except StopIteration:
                self.pending = None
            self._in_chain = False
        return o

    def drain(self):
        if self.pending is not None:
            self._in_chain = True
            for _ in self.pending:
                pass
            self._in_chain = False
            self.pending = None

    def dma(self, eng, out, in_, reads=(), writes=(), cwrites=(), key=None, **kw):
        assert key is not None
        return self.op(eng, lambda e: e.dma_start(out=out, in_=in_, **kw), reads, writes, cwrites, key)

    def bound_reg(self, eng):
        if self._breg is None:
            self._breg = eng.to_reg(NSLOT - 1)
        return self._breg

    def begin_phase(self):
        self.pstack = ExitStack()
        self._breg = None
        self.ops = []
        for b in self.bufs:
            b.reset()

    def end_phase(self):
        self.drain()
        nc = self.nc
        ops = self.ops
        for o in ops:
            deps = set()
            for b in o.reads:
                deps.update(b.xw)
                deps.update(b.cw)
            for b in o.writes:
                deps.update(b.xw)
                deps.update(b.cw)
                deps.update(b.readers)
            for b in o.cwrites:
                deps.update(b.xw)
                deps.update(b.readers)
            deps.discard(o.idx)
            o.deps = deps
            for b in o.reads:
                b.readers.append(o.idx)
            for b in o.writes:
                b.xw = [o.idx]
                b.cw = []
                b.readers = []
            for b in o.cwrites:
                b.cw.append(o.idx)
        for o in ops:
            latest = {}
            ddeps = []
            for d in o.deps:
                p = ops[d]
                if p.key is not None:
                    ddeps.append(p)
                elif not (p.eng == "pe" and o.eng == "pe"):
                    q = latest.get(p.eng)
                    if q is None or q.idx < p.idx:
                        latest[p.eng] = p
            for p in latest.values():
                p.signal = True
            o.deps = ddeps + list(latest.values())
        last_compute = {}
        for o in ops:
            if o.key is None:
                last_compute[o.eng] = o
        for o in last_compute.values():
            o.signal = True
        for o in ops:
            if o.key is None and o.signal:
                self.eng_seq[o.eng] += 1
                o.seq = self.eng_seq[o.eng]
        waits = [None] * len(ops)
        kc = self.key_count
        keys_used = set()
        for o in ops:
            w = {}
            for p in o.deps:
                if p.key is not None:
                    nm = "k_" + p.key
                    v = 16 * kc[p.key]
                else:
                    if not p.signal:
                        continue
                    nm = "e_" + p.eng
                    v = p.seq
                if w.get(nm, 0) < v:
                    w[nm] = v
            waits[o.idx] = w
            if o.key is not None:
                kc[o.key] = kc.get(o.key, 0) + 1
                keys_used.add(o.key)
        for o in ops:
            for nm in waits[o.idx]:
                self.sem(nm)
            if o.key is not None:
                self.sem("k_" + o.key)
            elif o.signal:
                self.sem("e_" + o.eng)
        fin = {}
        for k in sorted(keys_used):
            fin["k_" + k] = 16 * kc[k]
        for e, o in last_compute.items():
            fin["e_" + e] = o.seq
        sems = self.sems
        by_eng = {e: [o for o in ops if o.eng == e] for e in self.ENGS}

        def run(ename, eng):
            waited = {}
            for o in by_eng[ename]:
                for nm, v in waits[o.idx].items():
                    if waited.get(nm, 0) >= v:
                        continue
                    waited[nm] = v
                    eng.wait_ge(sems[nm], v)
                ins = o.fn(eng)
                if o.key is not None:
                    ins.then_inc(sems["k_" + o.key], 16)
                elif o.signal:
                    ins.then_inc(sems["e_" + o.eng], 1)
            for nm, v in fin.items():
                if nm == "e_" + ename:
                    continue
                eng.wait_ge(sems[nm], v)
            if ename == "pool" and self._breg is not None:
                eng.free_register(self._breg)
                self._breg = None

        with nc.Block() as block:
            @block.tensor
            def _(e):
                run("pe", e)

            @block.scalar
            def _(e):
                run("act", e)

            @block.vector
            def _(e):
                run("dve", e)

            @block.gpsimd
            def _(e):
                run("pool", e)

            @block.sync
            def _(e):
                run("sp", e)
        self.total_ops += len(ops)
        self.pstack.close()
        self.pstack = None
        self.phase_no += 1

    def finish(self):
        self.gstack.close()


def mm(P, out, lhsT, rhs, start, stop, reads, pbuf):
    if start:
        P.op("pe", lambda e: e.matmul(out=out, lhsT=lhsT, rhs=rhs, start=True, stop=stop), reads=reads, writes=[pbuf])
    else:
        P.op("pe", lambda e: e.matmul(out=out, lhsT=lhsT, rhs=rhs, start=False, stop=stop), reads=reads, cwrites=[pbuf])


def tr(P, out, in_, ident, first, reads, pbuf):
    if first:
        P.op("pe", lambda e: e.transpose(out=out, in_=in_, identity=ident), reads=reads, writes=[pbuf])
    else:
        P.op("pe", lambda e: e.transpose(out=out, in_=in_, identity=ident), reads=reads, cwrites=[pbuf])


def act(P, out, in_, func, reads, writes, bias=None, scale=None, cwrites=(), accum_out=None):
    kw = {}
    if bias is not None:
        kw["bias"] = bias
    if scale is not None:
        kw["scale"] = scale
    if accum_out is not None:
        kw["accum_out"] = accum_out
    P.op("act", lambda e: e.activation(out=out, in_=in_, func=func, **kw), reads=reads, writes=writes, cwrites=cwrites)


def ts(P, eng, out, in0, s1, s2, op0, op1, reads, writes, cwrites=()):
    if s2 is None:
        P.op(eng, lambda e: e.tensor_scalar(out=out, in0=in0, scalar1=s1, scalar2=None, op0=op0), reads=reads, writes=writes, cwrites=cwrites)
    else:
        P.op(eng, lambda e: e.tensor_scalar(out=out, in0=in0, scalar1=s1, scalar2=s2, op0=op0, op1=op1), reads=reads, writes=writes, cwrites=cwrites)


def tt(P, eng, out, in0, in1, op, reads, writes, cwrites=()):
    P.op(eng, lambda e: e.tensor_tensor(out=out, in0=in0, in1=in1, op=op), reads=reads, writes=writes, cwrites=cwrites)


def stt(P, out, in0, scalar, in1, op0, op1, reads, writes, cwrites=()):
    P.op("dve", lambda e: e.scalar_tensor_tensor(out=out, in0=in0, scalar=scalar, in1=in1, op0=op0, op1=op1),
         reads=reads, writes=writes, cwrites=cwrites)


def cp(P, eng, out, in_, reads, writes, cwrites=()):
    if eng == "act":
        P.op("act", lambda e: e.activation(out=out, in_=in_, func=AF.Copy), reads=reads, writes=writes, cwrites=cwrites)
    else:
        P.op(eng, lambda e: e.tensor_copy(out=out, in_=in_), reads=reads, writes=writes, cwrites=cwrites)


class G:
    pass


def setup_globals(P, g):
    g.ident_f, g.b_ident_f = P.sbb([128, 128], F32, "identf", glob=True)
    g.ident_b, g.b_ident_b = P.sbb([128, 128], BF16, "identb", glob=True)
    g.U_b, g.b_U = P.sbb([128, 128], BF16, "U", glob=True)
    g.ones_b, g.b_ones = P.sbb([128, 128], BF16, "ones", glob=True)
    g.eC, g.b_eC = P.sbb([128, NE], F32, "eC", glob=True)
    g.runc, g.b_runc = P.sbb([128, NE], F32, "runc", glob=True)
    g.posi, g.b_posi = P.sbb([128, NT, 2], I32, "posi", glob=True)
    g.wts, g.b_wts = P.sbb([128, NT, 2], F32, "wts", glob=True)
    g.eCi, g.b_eCi = P.sbb([128, NE], I32, "eCi", glob=True)


def emit_const_init(P, g):
    P.op("pool", lambda e: e.memset(g.ident_f[:], 0.0), writes=[g.b_ident_f])
    P.op("pool", lambda e: e.affine_select(out=g.ident_f[:], in_=g.ident_f[:], pattern=[[-1, 128]],
                                           compare_op=ALU.not_equal, fill=1.0, base=0, channel_multiplier=1),
         writes=[g.b_ident_f])
    cp(P, "pool", g.ident_b[:], g.ident_f[:], [g.b_ident_f], [g.b_ident_b])
    P.op("pool", lambda e: e.memset(g.ones_b[:], 1.0), writes=[g.b_ones])
    P.op("pool", lambda e: e.affine_select(out=g.U_b[:], in_=g.ones_b[:], pattern=[[1, 128]],
                                           compare_op=ALU.is_gt, fill=0.0, base=0, channel_multiplier=-1),
         reads=[g.b_ones], writes=[g.b_U])
    P.op("pool", lambda e: e.iota(g.eCi[:], pattern=[[CAP, NE]], base=0, channel_multiplier=0), writes=[g.b_eCi])
    cp(P, "pool", g.eC[:], g.eCi[:], [g.b_eCi], [g.b_eC])


def emit_zero_fill(P, g, dr, t):
    zt, b_zt = t.x1b[:, 0, :], t.b_x1b[0]
    P.op("pool", lambda e: e.memset(zt, 0.0), writes=[b_zt])
    Xv = dr["Xs"].rearrange("(n p) d -> p n d", p=128)
    for n in range(NSLOT // 128):
        P.dma("sp", Xv[:, n, :], zt, reads=[b_zt], writes=[g.b_Xs] if n == 0 else (), cwrites=() if n == 0 else [g.b_Xs], key="zf")


def alloc_tail(P, g, L):
    t = G()
    t.bc, t.b_bc = P.sbb([128, 2 * D + 36], F32, "bc")
    t.wr, t.b_wr = P.sbb([128, 8, 36], F32, "wr")
    t.x1b = P.sb([128, 4, D], BF16, "x1b")
    t.b_x1b = [P.buf("x1b%d" % i) for i in range(4)]
    t.x1T = Ring([P.sbb([128, 8, 128], F32, "x1T") for _ in range(1)])
    t.lnr = Ring([make_ln_scratch(P) for _ in range(4)])
    t.sm, t.b_sm = P.sbb([128, 928], F32, "small")
    t.oh, t.b_oh = P.sbb([128, 4, NE], BF16, "oh")
    t.ptr = Ring([P.psb([128, 512], F32, "ptrT") for _ in range(2)])
    t.psm, t.b_psm = P.psb([128, 512], F32, "psm")
    return t


def load_tail_consts(P, g, t, dr, L):
    P.dma("sp", t.bc[:], dr["bcm%d" % L][:, :], writes=[t.b_bc], key="cst")
    P.dma("sp", t.wr[:], dr["wr%d" % L].rearrange("(c p) n -> p c n", p=128), writes=[t.b_wr], key="cst")


def make_ln_scratch(P):
    sc = G()
    sc.st, sc.b_st = P.sbb([128, 2, 6], F32, "bnst")
    sc.mv, sc.b_mv = P.sbb([128, 2], F32, "mv")
    sc.rstd, sc.b_rstd = P.sbb([128, 1], F32, "rstd")
    return sc


def emit_ln_rows(P, t, src, b_src, dst, b_dst, goff, boff):
    sc = t.lnr.next()
    for h in range(2):
        P.op("dve", lambda e, h=h: e.bn_stats(out=sc.st[:, h, :], in_=src[:, h * 512:(h + 1) * 512]),
             reads=[b_src], writes=[sc.b_st] if h == 0 else (), cwrites=() if h == 0 else [sc.b_st])
    P.op("dve", lambda e: e.bn_aggr(out=sc.mv[:], in_=sc.st[:].rearrange("p a b -> p (a b)")), reads=[sc.b_st], writes=[sc.b_mv])
    act(P, sc.rstd[:], sc.mv[:, 1:2], AF.Sqrt, [sc.b_mv], [sc.b_rstd], bias=EPS)
    P.op("dve", lambda e: e.reciprocal(out=sc.rstd[:], in_=sc.rstd[:]), reads=[sc.b_rstd], writes=[sc.b_rstd])
    ts(P, "dve", dst, src, sc.mv[:, 0:1], sc.rstd[:, 0:1], ALU.subtract, ALU.mult, [b_src, sc.b_mv, sc.b_rstd], [b_dst])
    tt(P, "dve", dst, dst, t.bc[:, goff:goff + D], ALU.mult, [b_dst, t.b_bc], [b_dst])
    tt(P, "pool", dst, dst, t.bc[:, boff:boff + D], ALU.add, [b_dst, t.b_bc], [b_dst])


def emit_tail_block(P, g, t, xb, b_xt, catT, b_cat, wout, b_wout, pmm, gb, dr):
    sm = t.sm
    S = [t.b_sm]
    for i in range(4):
        tile = gb * 4 + i
        xi = xb[:, i, :]
        for hf in range(2):
            pm, b_pm = pmm.next()
            for c in range(8):
                mm(P, pm[:], catT[:, c, i * 128:(i + 1) * 128], wout[:, c, hf * 512:(hf + 1) * 512], c == 0, c == 7,
                   [b_cat[c], b_wout], b_pm)
            stt(P, xb[:, i, hf * 512:(hf + 1) * 512], xb[:, i, hf * 512:(hf + 1) * 512], ALPHA, pm[:], ALU.mult, ALU.add,
                [b_xt[i], b_pm], [b_xt[i]])
        emit_ln_rows(P, t, xi, b_xt[i], xi, b_xt[i], 0, D)
        P.dma("sp", dr["X1"][tile * 128:(tile + 1) * 128, :], xi, reads=[b_xt[i]], cwrites=[g.b_X1], key="x1st%d" % i)
    P.drain()
    for i in range(4):
        cp(P, "act", t.x1b[:, i, :], xb[:, i, :], [b_xt[i]], [t.b_x1b[i]])
    for i in range(4):
        x1T, b_x1T = t.x1T.next()
        for half in range(2):
            pt, b_pt = t.ptr.next()
            for cc in range(4):
                c = half * 4 + cc
                tr(P, pt[:, cc * 128:(cc + 1) * 128], xb[:, i, c * 128:(c + 1) * 128], g.ident_f[:], cc == 0,
                   [b_xt[i], g.b_ident_f], b_pt)
            cp(P, "act" if half == 0 else "dve", x1T[:, half * 4:(half + 1) * 4, :],
               pt[:].rearrange("p (c n) -> p c n", c=4), [b_pt], [b_x1T] if half == 0 else (),
               cwrites=() if half == 0 else [b_x1T])
        for c in range(8):
            o_ = t.psm[:, i * 36:(i + 1) * 36]
            if i == 0 and c == 0:
                P.op("pe", lambda e, o_=o_, l=x1T[:, c, :], r=t.wr[:, c, :]: e.matmul(out=o_, lhsT=l, rhs=r, start=True, stop=False),
                     reads=[b_x1T, t.b_wr], writes=[t.b_psm])
            else:
                P.op("pe", lambda e, o_=o_, l=x1T[:, c, :], r=t.wr[:, c, :], st=(c == 0), sp=(c == 7):
                     e.matmul(out=o_, lhsT=l, rhs=r, start=st, stop=sp), reads=[b_x1T, t.b_wr], cwrites=[t.b_psm])
    P.pending = router_chain(P, g, t, gb, dr)


def router_chain(P, g, t, gb, dr):
    sm = t.sm
    S = [t.b_sm]
    LG = sm[:, 0:144].rearrange("p (a b) -> p a b", a=4)
    gmax = sm[:, 144:148]
    gd = sm[:, 148:164].rearrange("p (a b) -> p a b", a=4)
    gsel = sm[:, 164:180].rearrange("p (a b) -> p a b", a=4)
    gexp = sm[:, 180:196].rearrange("p (a b) -> p a b", a=4)
    gsum = sm[:, 196:200]
    gtop = sm[:, 200:204]
    pen = sm[:, 204:220].rearrange("p (a b) -> p a b", a=4)
    masked = sm[:, 220:348]
    top8 = sm[:, 348:380].rearrange("p (a b) -> p a b", a=4)
    oh1 = sm[:, 380:508].rearrange("p (a b) -> p a b", a=4)
    oh2 = sm[:, 508:636].rearrange("p (a b) -> p a b", a=4)
    dd = sm[:, 636:640]
    ee = sm[:, 640:644]
    den = sm[:, 644:648]
    wtmp = sm[:, 648:656].rearrange("p (a b) -> p a b", a=4)
    rank = sm[:, 656:784].rearrange("p (a b) -> p a b", a=4)
    tmp = sm[:, 784:912].rearrange("p (a b) -> p a b", a=4)
    posf = sm[:, 912:920].rearrange("p (a b) -> p a b", a=4)
    valid = sm[:, 920:928].rearrange("p (a b) -> p a b", a=4)
    tt(P, "dve", LG, t.psm[:, 0:144].rearrange("p (a b) -> p a b", a=4),
       t.bc[:, 2 * D:2 * D + 36].unsqueeze(1).to_broadcast([128, 4, 36]), ALU.add, [t.b_psm, t.b_bc], S)
    yield
    P.op("dve", lambda e: e.tensor_reduce(out=gmax, in_=LG[:, :, 0:4], axis=AX.X, op=ALU.max), reads=S, writes=S)
    yield
    tt(P, "dve", gd, LG[:, :, 0:4], gmax.unsqueeze(2).to_broadcast([128, 4, 4]), ALU.subtract, S, S)
    yield
    ts(P, "dve", gsel, gd, 0.0, None, ALU.is_equal, None, S, S)
    yield
    act(P, gexp, gd, AF.Exp, S, S)
    yield
    P.op("dve", lambda e: e.tensor_reduce(out=gsum, in_=gexp, axis=AX.X, op=ALU.add), reads=S, writes=S)
    yield
    P.op("dve", lambda e: e.reciprocal(out=gtop, in_=gsum), reads=S, writes=S)
    yield
    ts(P, "dve", pen, gsel, 1e30, -1e30, ALU.mult, ALU.add, S, S)
    yield
    tt(P, "dve", masked.rearrange("p (a b c) -> p a b c", a=4, b=4), LG[:, :, 4:36].rearrange("p a (b c) -> p a b c", b=4),
       pen.unsqueeze(3).to_broadcast([128, 4, 4, 8]), ALU.add, S, S)
    yield
    for i in range(4):
        P.op("dve", lambda e, i=i: e.max(out=top8[:, i, :], in_=masked[:, i * 32:(i + 1) * 32]), reads=S, writes=S)
        yield
    m3 = masked.rearrange("p (a b) -> p a b", a=4)
    tt(P, "dve", oh1, m3, top8[:, :, 0:1].to_broadcast([128, 4, 32]), ALU.is_equal, S, S)
    yield
    tt(P, "dve", oh2, m3, top8[:, :, 1:2].to_broadcast([128, 4, 32]), ALU.is_equal, S, S)
    yield
    tt(P, "dve", dd, top8[:, :, 1], top8[:, :, 0], ALU.subtract, S, S)
    yield
    act(P, ee, dd, AF.Exp, S, S)
    yield
    ts(P, "dve", den, ee, 1.0, None, ALU.add, None, S, S)
    yield
    P.op("dve", lambda e: e.reciprocal(out=den, in_=den), reads=S, writes=S)
    yield
    tt(P, "dve", wtmp[:, :, 0], den, gtop, ALU.mult, S, S)
    yield
    tt(P, "dve", wtmp[:, :, 1], wtmp[:, :, 0], ee, ALU.mult, S, S)
    yield
    tt(P, "dve", t.oh[:], oh1, oh2, ALU.add, S, [t.b_oh])
    yield
    for i in range(4):
        o_ = t.psm[:, 256 + i * 32:256 + (i + 1) * 32]
        seq = [(g.U_b, i)] + [(g.ones_b, j) for j in range(i)]
        for n_, (lt, j) in enumerate(seq):
            P.op("pe", lambda e, o_=o_, l=lt[:], r=t.oh[:, j, :], st=(n_ == 0), sp=(n_ == len(seq) - 1):
                 e.matmul(out=o_, lhsT=l, rhs=r, start=st, stop=sp), reads=[g.b_U, g.b_ones, t.b_oh], cwrites=[t.b_psm])
            yield
    for j in range(4):
        P.op("pe", lambda e, j=j: e.matmul(out=t.psm[:, 448:480], lhsT=g.ones_b[:], rhs=t.oh[:, j, :], start=(j == 0), stop=(j == 3)),
             reads=[g.b_ones, t.b_oh], cwrites=[t.b_psm])
        yield
    tt(P, "dve", rank, t.psm[:, 256:384].rearrange("p (a b) -> p a b", a=4), g.runc[:].unsqueeze(1).to_broadcast([128, 4, NE]),
       ALU.add, [t.b_psm, g.b_runc], S)
    yield
    tt(P, "dve", g.runc[:], g.runc[:], t.psm[:, 448:480], ALU.add, [t.b_psm, g.b_runc], [g.b_runc])
    yield
    ts(P, "dve", tmp, rank, float(CAP), 1.0e6, ALU.is_ge, ALU.mult, S, S)
    yield
    tt(P, "dve", rank, rank, tmp, ALU.add, S, S)
    yield
    tt(P, "dve", rank, rank, g.eC[:].unsqueeze(1).to_broadcast([128, 4, NE]), ALU.add, S + [g.b_eC], S)
    yield
    tt(P, "dve", tmp, rank, oh1, ALU.mult, S, S)
    yield
    P.op("dve", lambda e: e.tensor_reduce(out=posf[:, :, 0], in_=tmp, axis=AX.X, op=ALU.add), reads=S, writes=S)
    yield
    tt(P, "dve", tmp, rank, oh2, ALU.mult, S, S)
    yield
    P.op("dve", lambda e: e.tensor_reduce(out=posf[:, :, 1], in_=tmp, axis=AX.X, op=ALU.add), reads=S, writes=S)
    yield
    ts(P, "dve", valid, posf, float(NSLOT) - 0.5, None, ALU.is_lt, None, S, S)
    yield
    tt(P, "dve", g.wts[:, gb * 4:(gb + 1) * 4, :], wtmp, valid, ALU.mult, S, [g.b_wts])
    yield
    cp(P, "dve", g.posi[:, gb * 4:(gb + 1) * 4, :], posf, S, [g.b_posi])
    yield
    Xs = dr["Xs"]
    for i in range(4):
        tile = gb * 4 + i
        for k in range(2):
            P.op("pool", lambda e, k=k, tile=tile, i=i: e.indirect_dma_start(
                out=Xs[:, :], out_offset=bass.IndirectOffsetOnAxis(ap=g.posi[:, tile, k:k + 1], axis=0),
                in_=t.x1b[:, i, :], in_offset=None, bounds_check=P.bound_reg(e), oob_is_err=False),
                reads=[t.b_x1b[i], g.b_posi], cwrites=[g.b_Xs], key="sc%d" % i)
            yield


def load_weight_bf16(P, dst, b_dst, src_ap, n_k, ncols, key):
    first = True
    for c0 in range(0, ncols, 512):
        cw = min(512, ncols - c0)
        P.dma("pool", dst[:, :, c0:c0 + cw], src_ap.rearrange("(c p) n -> p c n", p=128)[:, :, c0:c0 + cw],
              writes=[b_dst] if first else (), cwrites=() if first else [b_dst], key=key)
        first = False


def phase_m0(P, g, dr):
    P.begin_phase()
    if not hasattr(g, "inited"):
        emit_const_init(P, g)
        g.inited = True
    P.op("pool", lambda e: e.memset(g.runc[:], 0.0), writes=[g.b_runc])
    t = alloc_tail(P, g, 0)
    load_tail_consts(P, g, t, dr, 0)
    pv, b_pv = P.sbb([128, 168], F32, "pv0")
    P.dma("sp", pv[:], dr["pv0"][:, :], writes=[b_pv], key="cst")
    win, b_win = P.sbb([128, 8, 2560], BF16, "win")
    wout, b_wout = P.sbb([128, 8, 1024], BF16, "wout")
    load_weight_bf16(P, win, b_win, dr["ab_w_in"], 8, 2560, "win")
    load_weight_bf16(P, wout, b_wout, dr["ab_w_out"], 8, 1024, "wout")
    dg, b_dg = P.sbb([128, 4 * 31, 128], BF16, "diag")
    for j in range(4 * 31):
        ts(P, "pool" if j % 2 else "dve", dg[:, j, :], g.ident_f[:], pv[:, 20 + j:21 + j], None, ALU.mult, None,
           [g.b_ident_f, b_pv], [b_dg] if j == 0 else (), cwrites=() if j == 0 else [b_dg])
    onesM, b_onesM = P.sbb([128, 128], BF16, "onesM")
    P.op("pool", lambda e: e.memset(onesM[:], 1.0 / 512.0), writes=[b_onesM])

    xring = Ring([(P.sb([128, 4, D], F32, "xblk"), [P.buf("xt%d" % i) for i in range(4)]) for _ in range(2)])
    xT, b_xT = P.sbb([128, 8, BLK], BF16, "xT")
    Ain, b_Ain = P.sbb([128, 4, 30 + BLK], BF16, "Ain")
    Bin, b_Bin = P.sbb([128, 4, 2 + BLK], F32, "Bin")
    y32, b_y32 = P.sbb([128, 4, BLK], F32, "y32")
    ybf, b_ybf = P.sbb([128, 4, BLK], BF16, "ybf")
    ysq, b_ysq = P.sbb([128, 4, BLK], BF16, "ysq")
    mean, b_mean = P.sbb([128, BLK], F32, "mean")
    rstd, b_rstd = P.sbb([128, BLK], F32, "rstdA")
    tmpA = Ring([P.sbb([128, BLK], F32, "tmpA") for _ in range(1)])
    sig = tmpA
    acc = Ring([(mean, b_mean)])
    gc = acc
    catT, b_catT = P.sbb([128, 8, BLK], BF16, "catT")
    b_cat = [P.buf("cat%d" % i) for i in range(8)]
    pmm = Ring([P.psb([128, 512], F32, "pmm") for _ in range(3)])
    pst0, b_pst0 = P.psb([128, 512], F32, "pst0")
    pst1, b_pst1 = P.psb([128, 512], F32, "pst1")
    ptr = t.ptr

    x_dr = dr["x"]

    xbf, b_xbf = P.sbb([128, 4, D], BF16, "xbf")

    def load_xbf(gbn):
        P.dma("pool", xbf[:], x_dr[gbn * BLK:(gbn + 1) * BLK, :].rearrange("(i p) d -> p i d", p=128),
              writes=[b_xbf], key="xbf")

    def load_x(gbn):
        xb_, b_ = xring.items[gbn % 2]
        P.dma("sp", xb_[:], x_dr[gbn * BLK:(gbn + 1) * BLK, :].rearrange("(i p) d -> p i d", p=128),
              writes=b_, key="xblk%d" % (gbn % 2))

    for gb in range(NBLK):
        seq_start = (gb % (SEQ // BLK)) == 0
        if gb == 0:
            load_x(0)
        if gb + 1 < NBLK:
            load_x(gb + 1)
        if gb == 0:
            emit_zero_fill(P, g, dr, t)
        xblk, b_xt = xring.items[gb % 2]
        if gb == 0:
            load_xbf(0)
        for c in range(8):
            pt, b_pt = ptr.next()
            ptv = pt[:].bitcast(BF16)
            for i in range(4):
                tr(P, ptv[:, i * 128:(i + 1) * 128], xbf[:, i, c * 128:(c + 1) * 128], g.ident_b[:], i == 0,
                   [b_xbf, g.b_ident_b], b_pt)
            cp(P, "act" if c % 2 == 0 else "dve", xT[:, c, :], ptv[:, 0:512], [b_pt], [b_xT] if c == 0 else (),
               cwrites=() if c == 0 else [b_xT])
        if gb + 1 < NBLK:
            load_xbf(gb + 1)
        if seq_start:
            P.op("pool", lambda e: e.memset(Ain[:, :, 0:30], 0.0), writes=[b_Ain])
            P.op("pool", lambda e: e.memset(Bin[:, :, 0:2], 0.0), writes=[b_Bin])

        def inproj(m):
            pm, b_pm = pmm.next()
            for c in range(8):
                mm(P, pm[:], win[:, c, m * 128:(m + 1) * 128], xT[:, c, :], c == 0, c == 7, [b_win, b_xT], b_pm)
            return pm, b_pm

        for q in range(4):
            pm, b_pm = inproj(4 + q)
            sg, b_sg = sig.next()
            act(P, sg[:], pm[:], AF.Sigmoid, [b_pm, b_pv], [b_sg], bias=pv[:, 4 + q:5 + q])
            pm2, b_pm2 = inproj(q)
            stt(P, Ain[:, q, 30:30 + BLK], pm2[:], pv[:, q:q + 1], sg[:], ALU.add, ALU.mult,
                [b_pm2, b_pv, b_sg], (), cwrites=[b_Ain])
        for q in range(4):
            pm, b_pm = pmm.next()
            for k in range(31):
                mm(P, pm[:], dg[:, q * 31 + k, :], Ain[:, q, k:k + BLK], k == 0, k == 30, [b_dg, b_Ain], b_pm)
            act(P, y32[:, q, :], pm[:], AF.Identity, [b_pm, b_pv], [b_y32] if q == 0 else (), bias=pv[:, 144 + q:145 + q],
                cwrites=() if q == 0 else [b_y32])
            act(P, ysq[:, q, :], pm[:], AF.Square, [b_pm, b_pv], [b_ysq] if q == 0 else (), bias=pv[:, 144 + q:145 + q],
                cwrites=() if q == 0 else [b_ysq])
            cp(P, "dve", ybf[:, q, :], y32[:, q, :], [b_y32], [b_ybf] if q == 0 else (), cwrites=() if q == 0 else [b_ybf])
        cp(P, "pool", Ain[:, :, 0:30], Ain[:, :, BLK:BLK + 30], [b_Ain], [b_Ain])
        for q in range(4):
            mm(P, pst0[:], onesM[:], ybf[:, q, :], q == 0, q == 3, [b_onesM, b_ybf], b_pst0)
        for q in range(4):
            mm(P, pst1[:], onesM[:], ysq[:, q, :], q == 0, q == 3, [b_onesM, b_ysq], b_pst1)
        cp(P, "act", mean[:], pst0[:], [b_pst0], [b_mean])
        tt(P, "dve", rstd[:], mean[:], mean[:], ALU.mult, [b_mean], [b_rstd])
        tt(P, "dve", rstd[:], pst1[:], rstd[:], ALU.subtract, [b_pst1, b_rstd], [b_rstd])
        ts(P, "dve", rstd[:], rstd[:], 0.0, None, ALU.max, None, [b_rstd], [b_rstd])
        act(P, rstd[:], rstd[:], AF.Sqrt, [b_rstd], [b_rstd], bias=EPS)
        P.op("dve", lambda e: e.reciprocal(out=rstd[:], in_=rstd[:]), reads=[b_rstd], writes=[b_rstd])
        for q in range(4):
            tA, b_tA = tmpA.next()
            tt(P, "dve", tA[:], y32[:, q, :], mean[:], ALU.subtract, [b_y32, b_mean], [b_tA])
            tt(P, "pool", tA[:], tA[:], rstd[:], ALU.mult, [b_tA, b_rstd], [b_tA])
            act(P, catT[:, q, :], tA[:], AF.Silu, [b_tA, b_pv], [b_cat[q]], bias=pv[:, 152 + q:153 + q], scale=pv[:, 148 + q:149 + q])
        for q in range(4):
            pm, b_pm = inproj(12 + q)
            gcq, b_gc = gc.next()
            act(P, gcq[:], pm[:], AF.Identity, [b_pm, b_pv], [b_gc], bias=pv[:, 12 + q:13 + q])
            pm2, b_pm2 = inproj(16 + q)
            stt(P, Bin[:, q, 2:2 + BLK], pm2[:], pv[:, 16 + q:17 + q], gcq[:], ALU.add, ALU.mult,
                [b_pm2, b_pv, b_gc], (), cwrites=[b_Bin])
            ac, b_ac = acc.next()
            ts(P, "dve", ac[:], Bin[:, q, 0:BLK], pv[:, 156 + q * 3:157 + q * 3], None, ALU.mult, None, [b_Bin, b_pv], [b_ac])
            stt(P, ac[:], Bin[:, q, 1:1 + BLK], pv[:, 157 + q * 3:158 + q * 3], ac[:], ALU.mult, ALU.add, [b_Bin, b_pv, b_ac], [b_ac])
            stt(P, ac[:], Bin[:, q, 2:2 + BLK], pv[:, 158 + q * 3:159 + q * 3], ac[:], ALU.mult, ALU.add, [b_Bin, b_pv, b_ac], [b_ac])
            pm3, b_pm3 = inproj(8 + q)
            stt(P, catT[:, 4 + q, :], pm3[:], pv[:, 8 + q:9 + q], ac[:], ALU.add, ALU.mult, [b_pm3, b_pv, b_ac], [b_cat[4 + q]])
        cp(P, "pool", Bin[:, :, 0:2], Bin[:, :, BLK:BLK + 2], [b_Bin], [b_Bin])
        emit_tail_block(P, g, t, xblk, b_xt, catT, b_cat, wout, b_wout, pmm, gb, dr)
    P.end_phase()


def phase_m1(P, g, dr):
    P.begin_phase()
    if not hasattr(g, "inited"):
        emit_const_init(P, g)
        g.inited = True
    P.op("pool", lambda e: e.memset(g.runc[:], 0.0), writes=[g.b_runc])
    t = alloc_tail(P, g, 1)
    load_tail_consts(P, g, t, dr, 1)
    pv, b_pv = P.sbb([128, 12], F32, "pv1")
    P.dma("sp", pv[:], dr["pv1"][:, :], writes=[b_pv], key="cst")
    bx, b_bx = P.sbb([128, 2048], F32, "bc1x")
    P.dma("sp", bx[:], dr["bc1x"][:, :], writes=[b_bx], key="cst")
    win, b_win = P.sbb([128, 8, 2560], BF16, "win")
    wout, b_wout = P.sbb([128, 8, 1024], BF16, "wout")
    load_weight_bf16(P, win, b_win, dr["cd_w_in"], 8, 2560, "win")
    load_weight_bf16(P, wout, b_wout, dr["cd_w_out"], 8, 1024, "wout")
    wsT, b_wsT = P.sbb([128, 8, 128], BF16, "wsT")
    P.dma("pool", wsT[:].rearrange("p h i -> p (h i)"), dr["wsT"][:, :], writes=[b_wsT], key="wsT")
    P.op("pool", lambda e: e.memset(wsT[64:128, :, 0:64], 0.0), writes=[b_wsT])
    wsb, b_wsb = P.sbb([128, 4, 128], F32, "wsb")
    P.dma("sp", wsb[:].rearrange("p a b -> p (a b)"), dr["wsb"][:, :], writes=[b_wsb], key="cst")
    relP, b_relP = P.sbb([128, 8, 640], BF16, "relP")
    for hh_ in range(8):
        P.dma("pool", relP[:, hh_, :], dr["relP"][:, hh_ * 640:(hh_ + 1) * 640], writes=[b_relP] if hh_ == 0 else (),
              cwrites=() if hh_ == 0 else [b_relP], key="relP")

    xblk = P.sb([128, 4, D], F32, "xblk")
    b_xt = [P.buf("xt%d" % i) for i in range(4)]
    xT, b_xT = P.sbb([128, 8, BLK], BF16, "xT")
    vt, b_vt = P.sbb([128, 512], F32, "vt")
    vn, b_vn = P.sbb([128, 4, 512], BF16, "vn")
    st1, b_st1 = P.sbb([128, 6], F32, "st1")
    mv1, b_mv1 = P.sbb([128, 2], F32, "mv1")
    rs1, b_rs1 = P.sbb([128, 1], F32, "rs1")
    qz, b_qz = P.sbb([128, 8, BLK], BF16, "qz")
    P.op("pool", lambda e: e.memset(qz[:].rearrange("p a b -> p (a b)"), 0.0), writes=[b_qz])
    osb, b_osb = (None, None) if ATT_T else P.sbb([128, 264], F32, "osb")
    rden, b_rden = P.sbb([128, 512], F32, "rden")
    kT = P.sb([128, 4, SEQ], BF16, "kT")
    b_kT = [P.buf("kT%d" % j) for j in range(4)]
    Va = P.sb([128, 16, 8, 64 if ATT_T else 66], BF16, "Va")
    b_Va = [P.buf("Va%d" % j) for j in range(4)]
    gt, b_gt = P.sbb([128, 512], F32, "gt")
    catT = P.sb([128, 8, BLK], BF16, "catT")
    b_cat = [P.buf("cat%d" % i) for i in range(8)]
    pT = Ring([P.sbb([128, 640], BF16, "pT") for _ in range(2)])
    otok, b_otok = (None, None) if ATT_T else P.sbb([128, 512], F32, "otok")
    rcp, b_rcp = (None, None) if ATT_T else P.sbb([128, 8], F32, "rcp")
    pmm = Ring([P.psb([128, 512], F32, "pmm") for _ in range(3)])
    pO = [P.psb([128, 512], F32, "pO") for _ in range(2)]
    ptr = t.ptr
    spairs = Ring([(pmm.items[0], pmm.items[1]), (pmm.items[2], ptr.items[0])])
    P.op("pool", lambda e: e.memset(Va[:].rearrange("p a b c -> p (a b c)"), 1.0), writes=b_Va)

    x_dr = dr["X2"]
    for gb in range(NBLK):
        jj = gb % 4
        P.dma("sp", xblk[:], x_dr[gb * BLK:(gb + 1) * BLK, :].rearrange("(i p) d -> p i d", p=128),
              reads=[g.b_X2], writes=b_xt, key="xblk0")
        if gb == 0:
            emit_zero_fill(P, g, dr, t)
        for c in range(8):
            pt, b_pt = ptr.next()
            for i in range(4):
                tr(P, pt[:, i * 128:(i + 1) * 128], xblk[:, i, c * 128:(c + 1) * 128], g.ident_f[:], i == 0,
                   [b_xt[i], g.b_ident_f], b_pt)
            cp(P, "act" if c % 2 == 0 else "dve", xT[:, c, :], pt[:], [b_pt], [b_xT] if c == 0 else (),
               cwrites=() if c == 0 else [b_xT])

        def inproj_fm(m):
            pm, b_pm = pmm.next()
            for c in range(8):
                mm(P, pm[:], win[:, c, m * 128:(m + 1) * 128], xT[:, c, :], c == 0, c == 7, [b_win, b_xT], b_pm)
            return pm, b_pm

        def inproj_tm(i, col0):
            pm, b_pm = pmm.next()
            for c in range(8):
                mm(P, pm[:], xT[:, c, i * 128:(i + 1) * 128], win[:, c, col0:col0 + 512], c == 0, c == 7, [b_win, b_xT], b_pm)
            return pm, b_pm

        for i in range(4):
            pm, b_pm = inproj_tm(i, 512)
            tt(P, "dve", vt[:], pm[:], bx[:, 0:512], ALU.add, [b_pm, b_bx], [b_vt])
            P.op("dve", lambda e: e.bn_stats(out=st1[:], in_=vt[:]), reads=[b_vt], writes=[b_st1])
            P.op("dve", lambda e: e.bn_aggr(out=mv1[:], in_=st1[:]), reads=[b_st1], writes=[b_mv1])
            act(P, rs1[:], mv1[:, 1:2], AF.Sqrt, [b_mv1], [b_rs1], bias=EPS)
            P.op("dve", lambda e: e.reciprocal(out=rs1[:], in_=rs1[:]), reads=[b_rs1], writes=[b_rs1])
            ts(P, "dve", vt[:], vt[:], mv1[:, 0:1], rs1[:, 0:1], ALU.subtract, ALU.mult, [b_vt, b_mv1, b_rs1], [b_vt])
            tt(P, "pool", vt[:], vt[:], bx[:, 512:1024], ALU.mult, [b_vt, b_bx], [b_vt])
            tt(P, "pool", vn[:, i, :], vt[:], bx[:, 1024:1536], ALU.add, [b_vt, b_bx], [b_vn] if i == 0 else (),
               cwrites=() if i == 0 else [b_vn])
        for i in range(4):
            tl = jj * 4 + i
            pm, b_pm = inproj_tm(i, 2048)
            tt(P, "dve", Va[:, tl, :, 0:64], pm[:].rearrange("p (h c) -> p h c", h=8),
               bx[:, 1536:2048].rearrange("p (h c) -> p h c", h=8), ALU.add, [b_pm, b_bx],
               [b_Va[jj]] if i == 0 else (), cwrites=() if i == 0 else [b_Va[jj]])
        for qc in range(4):
            pm, b_pm = inproj_fm(12 + qc)
            act(P, kT[:, qc, jj * BLK:(jj + 1) * BLK], pm[:], AF.Identity, [b_pm, b_pv], [b_kT[jj]] if qc == 0 else (),
                bias=pv[:, 8 + qc:9 + qc], cwrites=() if qc == 0 else [b_kT[jj]])
        for qc in range(4):
            pm, b_pm = inproj_fm(8 + qc)
            for hh in range(2):
                ps_ = slice(hh * 64, (hh + 1) * 64)
                ts(P, "dve", qz[ps_, 2 * qc + hh, :], pm[ps_, :], pv[ps_, 4 + qc:5 + qc], 0.125, ALU.add, ALU.mult,
                   [b_pm, b_pv], [b_qz] if (qc == 0 and hh == 0) else (), cwrites=() if (qc == 0 and hh == 0) else [b_qz])
        if "sgu" in SKIP:
            for qc in range(4):
                P.op("pool", lambda e, qc=qc: e.memset(catT[:, qc, :], 0.0), writes=[b_cat[qc]])
        for qc in ([] if "sgu" in SKIP else range(4)):
            pgm, b_pgm = pmm.next()
            first = True
            for hh in range(2):
                h = 2 * qc + hh
                for i in range(4):
                    outap = pgm[hh * 64:(hh + 1) * 64, i * 128:(i + 1) * 128]
                    lhsT = vn[:, i, h * 64:(h + 1) * 64]
                    rhs = wsT[:, h, :]
                    if first:
                        P.op("pe", lambda e, o=outap, l=lhsT, r=rhs: e.matmul(out=o, lhsT=l, rhs=r, start=True, stop=True),
                             reads=[b_vn, b_wsT], writes=[b_pgm])
                        first = False
                    else:
                        P.op("pe", lambda e, o=outap, l=lhsT, r=rhs: e.matmul(out=o, lhsT=l, rhs=r, start=True, stop=True),
                             reads=[b_vn, b_wsT], cwrites=[b_pgm])
            tt(P, "dve", gt[:].rearrange("p (a b) -> p a b", a=4), pgm[:].rearrange("p (a b) -> p a b", a=4),
               wsb[:, qc, :].unsqueeze(1).to_broadcast([128, 4, 128]), ALU.add, [b_pgm, b_wsb], [b_gt])
            pm, b_pm = inproj_fm(qc)
            stt(P, catT[:, qc, :], pm[:], pv[:, qc:qc + 1], gt[:], ALU.add, ALU.mult, [b_pm, b_pv, b_gt], [b_cat[qc]])
        if "attn" in SKIP:
            for qc in range(4):
                P.op("pool", lambda e, qc=qc: e.memset(catT[:, 4 + qc, :], 0.0), writes=[b_cat[4 + qc]])
        for pr in ([] if "attn" in SKIP else range(4)):
            t0 = jj * 4 + pr
            slots = [s_ for s_ in range(5) if t0 - 4 + s_ >= 0]
            kbufs = list({b_kT[(t0 - 4 + s_) // 4] for s_ in slots})
            vbufs = list({b_Va[(t0 - 4 + s_) // 4] for s_ in slots})
            lo = slots[0]
            pend = []

            def flush_pv():
                while pend:
                    pend.pop(0)()

            for h in range(8):
                qc = h // 2
                (pa, b_pa), (pb, b_pb) = spairs.next()
                firstA = True
                for s_ in slots:
                    tk = t0 - 4 + s_
                    lhsT = kT[:, qc, tk * 128:(tk + 1) * 128]
                    rhs = qz[:, h, pr * 128:(pr + 1) * 128]
                    if s_ < 4:
                        outap, bb = pa[:, s_ * 128:(s_ + 1) * 128], b_pa
                        fw = firstA
                        firstA = False
                    else:
                        outap, bb = pb[:, 0:128], b_pb
                        fw = True
                    P.op("pe", lambda e, o=outap, l=lhsT, r=rhs: e.matmul(out=o, lhsT=l, rhs=r, start=True, stop=False),
                         reads=kbufs + [b_qz], writes=[bb] if fw else (), cwrites=() if fw else [bb])
                    P.op("pe", lambda e, o=outap, r=relP[:, h, s_ * 128:(s_ + 1) * 128]:
                         e.matmul(out=o, lhsT=g.ident_b[:], rhs=r, start=False, stop=True),
                         reads=[g.b_ident_b, b_relP], cwrites=[bb])
                pTt, b_pT = pT.next()
                if lo < 4:
                    act(P, pTt[:, lo * 128:512], pa[:, lo * 128:512], AF.Exp, [b_pa], [b_pT])
                    act(P, pTt[:, 512:640], pb[:, 0:128], AF.Exp, [b_pb], (), cwrites=[b_pT])
                else:
                    act(P, pTt[:, 512:640], pb[:, 0:128], AF.Exp, [b_pb], [b_pT])
                if ATT_T:
                    def pv_fn(h=h, qc=qc, pTt=pTt, b_pT=b_pT):
                        po = (h % 2) * 64
                        (pN, b_pN), (pD, b_pD) = pO[0], pO[1]
                        for which, (pX, b_pX) in enumerate(((pN, b_pN), (pD, b_pD))):
                            for oi, s_ in enumerate(slots):
                                tk = t0 - 4 + s_
                                lhsT = Va[:, tk, h, 0:64] if which == 0 else g.ones_b[:, 0:64]
                                fw = (h == 0 and oi == 0)
                                P.op("pe", lambda e, o=pX[po:po + 64, qc * 128:(qc + 1) * 128], l=lhsT,
                                     r=pTt[:, s_ * 128:(s_ + 1) * 128], st=(oi == 0), sp=(oi == len(slots) - 1):
                                     e.matmul(out=o, lhsT=l, rhs=r, start=st, stop=sp),
                                     reads=(vbufs if which == 0 else [g.b_ones]) + [b_pT],
                                     writes=[b_pX] if fw else (), cwrites=() if fw else [b_pX])
                    flush_pv()
                    pend.append(pv_fn)
                    if h == 7:
                        flush_pv()
                    continue
                pOt, b_pO = pO[h // 4]
                c0 = (h % 4) * 66
                if "pv" in SKIP:
                    continue
                for oi, s_ in enumerate(slots):
                    tk = t0 - 4 + s_
                    fw = (h % 4 == 0 and oi == 0)
                    P.op("pe", lambda e, o=pOt[:, c0:c0 + 66], l=pTt[:, s_ * 128:(s_ + 1) * 128], r=Va[:, tk, h, :],
                         st=(oi == 0), sp=(oi == len(slots) - 1): e.matmul(out=o, lhsT=l, rhs=r, start=st, stop=sp),
                         reads=vbufs + [b_pT], writes=[b_pO] if fw else (), cwrites=() if fw else [b_pO])
                if h % 4 == 3 and "norm" not in SKIP:
                    hb = h // 4
                    cp(P, "act", osb[:], pOt[:, 0:264], [b_pO], [b_osb])
                    ov3 = osb[:].rearrange("p (h c) -> p h c", c=66)
                    P.op("dve", lambda e, ov3=ov3, hb=hb: e.reciprocal(out=rcp[:, hb * 4:(hb + 1) * 4], in_=ov3[:, :, 64]),
                         reads=[b_osb], writes=[b_rcp])
                    tt(P, "dve", otok[:, hb * 256:(hb + 1) * 256].rearrange("p (h c) -> p h c", c=64), ov3[:, :, 0:64],
                       rcp[:, hb * 4:(hb + 1) * 4].unsqueeze(2).to_broadcast([128, 4, 64]), ALU.mult, [b_osb, b_rcp],
                       [b_otok] if hb == 0 else (), cwrites=() if hb == 0 else [b_otok])
            if ATT_T:
                (pN, b_pN), (pD, b_pD) = pO[0], pO[1]
                P.op("dve", lambda e, pD=pD: e.reciprocal(out=rden[:], in_=pD[:]), reads=[b_pD], writes=[b_rden])
                for qc in range(4):
                    tt(P, "dve", catT[:, 4 + qc, pr * 128:(pr + 1) * 128], pN[:, qc * 128:(qc + 1) * 128],
                       rden[:, qc * 128:(qc + 1) * 128], ALU.mult, [b_pN, b_rden],
                       [b_cat[4 + qc]] if pr == 0 else (), cwrites=() if pr == 0 else [b_cat[4 + qc]])
                continue
            if "pv" in SKIP or "norm" in SKIP:
                if pr == 0:
                    for qc in range(4):
                        P.op("pool", lambda e, qc=qc: e.memset(catT[:, 4 + qc, :], 0.0), writes=[b_cat[4 + qc]])
                continue
            pt, b_pt = ptr.next()
            for qc in range(4):
                tr(P, pt[:, qc * 128:(qc + 1) * 128], otok[:, qc * 128:(qc + 1) * 128], g.ident_f[:], qc == 0,
                   [b_otok, g.b_ident_f], b_pt)
            for qc in range(4):
                cp(P, "act" if qc % 2 == 0 else "dve", catT[:, 4 + qc, pr * 128:(pr + 1) * 128], pt[:, qc * 128:(qc + 1) * 128],
                   [b_pt], [b_cat[4 + qc]] if pr == 0 else (), cwrites=() if pr == 0 else [b_cat[4 + qc]])
        emit_tail_block(P, g, t, xblk, b_xt, catT, b_cat, wout, b_wout, pmm, gb, dr)
    P.end_phase()

def phase_e(P, g, dr, L):
    P.begin_phase()
    NI = CAP // 128
    NW = 3
    sets = []
    for i in range(NW):
        sets.append((P.sbb([128, 8, DEX], BF16, "wg"), P.sbb([128, 8, DEX], BF16, "wu"), P.sbb([128, 4, D], BF16, "wd"),
                     P.sbb([128, NI, D], BF16, "xe")))
    xeT, b_xeT = P.sbb([128, 8, CAP], BF16, "xeT")
    sgt = Ring([P.sbb([128, CAP], F32, "sgt") for _ in range(2)])
    hT, b_hT = P.sbb([128, 4, CAP], BF16, "hT")
    yt = Ring([P.sbb([128, D], F32, "yt") for _ in range(3)])
    ptb = Ring([P.psb([128, 512], BF16, "ptb") for _ in range(2)])
    pg = Ring([P.psb([128, 512], F32, "pg") for _ in range(2)])
    pu = Ring([P.psb([128, 512], F32, "pu") for _ in range(2)])
    py = Ring([P.psb([128, 512], F32, "py") for _ in range(2)])
    Wg, Wu, Wd = dr["moe_w_gate"], dr["moe_w_up"], dr["moe_w_down"]
    Xs, Ys = dr["Xs"], dr["Ys"]

    def issue(ex):
        s_ = ex % NW
        (wgt, b_wg), (wut, b_wu), (wdt, b_wd), (xet, b_xe) = sets[s_]
        P.dma("sp", xet[:], Xs[ex * CAP:(ex + 1) * CAP, :].rearrange("(i p) d -> p i d", p=128),
              reads=[g.b_Xs], writes=[b_xe], key="xe%d" % s_)
        load_weight_bf16(P, wgt, b_wg, Wg[L, ex], 8, DEX, "wg%d" % s_)
        load_weight_bf16(P, wut, b_wu, Wu[L, ex], 8, DEX, "wu%d" % s_)
        load_weight_bf16(P, wdt, b_wd, Wd[L, ex], 4, D, "wd%d" % s_)

    for ex in range(min(NW - 1, NE)):
        issue(ex)
    for ex in range(NE):
        if ex + NW - 1 < NE:
            issue(ex + NW - 1)
        (wgt, b_wg), (wut, b_wu), (wdt, b_wd), (xet, b_xe) = sets[ex % NW]
        for c in range(8):
            pt, b_pt = ptb.next()
            for i in range(NI):
                tr(P, pt[:, i * 128:(i + 1) * 128], xet[:, i, c * 128:(c + 1) * 128], g.ident_b[:], i == 0,
                   [b_xe, g.b_ident_b], b_pt)
            cp(P, "act" if c % 2 == 0 else "dve", xeT[:, c, :], pt[:, 0:CAP], [b_pt], [b_xeT] if c == 0 else (),
               cwrites=() if c == 0 else [b_xeT])
        for m in range(4):
            pgt, b_pg = pg.next()
            put, b_pu = pu.next()
            for c in range(8):
                mm(P, pgt[:, 0:CAP], wgt[:, c, m * 128:(m + 1) * 128], xeT[:, c, :], c == 0, c == 7, [b_wg, b_xeT], b_pg)
            for c in range(8):
                mm(P, put[:, 0:CAP], wut[:, c, m * 128:(m + 1) * 128], xeT[:, c, :], c == 0, c == 7, [b_wu, b_xeT], b_pu)
            sg, b_sg = sgt.next()
            act(P, sg[:], pgt[:, 0:CAP], AF.Silu, [b_pg], [b_sg])
            tt(P, "dve", hT[:, m, :], sg[:], put[:, 0:CAP], ALU.mult, [b_sg, b_pu], [b_hT] if m == 0 else (),
               cwrites=() if m == 0 else [b_hT])
        for i in range(NI):
            ytile, b_yt = yt.next()
            for h in range(2):
                pyt, b_py = py.next()
                for m in range(4):
                    mm(P, pyt[:], hT[:, m, i * 128:(i + 1) * 128], wdt[:, m, h * 512:(h + 1) * 512], m == 0, m == 3,
                       [b_hT, b_wd], b_py)
                cp(P, "act" if h == 0 else "dve", ytile[:, h * 512:(h + 1) * 512], pyt[:], [b_py],
                   [b_yt] if h == 0 else (), cwrites=() if h == 0 else [b_yt])
            r0 = ex * CAP + i * 128
            P.dma("sp", Ys[r0:r0 + 128, :], ytile[:], reads=[b_yt], cwrites=[g.b_Ys], key="yst%d" % (yt.i % 3))
    P.end_phase()


def phase_c(P, g, dr, L, out_ap, b_out):
    P.begin_phase()
    t = G()
    t.bc, t.b_bc = P.sbb([128, 2 * D], F32, "bc")
    P.dma("sp", t.bc[:], dr["bcf%d" % L][:, :], writes=[t.b_bc], key="cst")
    t.lnr = Ring([make_ln_scratch(P) for _ in range(3)])
    NR = 4
    x1r = [P.sbb([128, D], F32, "x1c") for _ in range(NR)]
    y0r = [P.sbb([128, D], F32, "y0") for _ in range(NR)]
    y1r = [P.sbb([128, D], F32, "y1") for _ in range(NR)]
    rr = Ring([P.sbb([128, D], F32, "rc") for _ in range(2)])
    orr = Ring([P.sbb([128, D], F32, "oc") for _ in range(3)])
    Ys = dr["Ys"]
    for (yy, b_yy) in y0r + y1r:
        P.op("pool", lambda e, yy=yy: e.memset(yy[:], 0.0), writes=[b_yy])

    def issue_loads(tile):
        s_ = tile % NR
        x1, b_x1 = x1r[s_]
        P.dma("sp", x1[:], dr["X1"][tile * 128:(tile + 1) * 128, :], reads=[g.b_X1], writes=[b_x1], key="cx%d" % s_)
        for k, (yy, b_yy) in enumerate((y0r[s_], y1r[s_])):
            P.op("pool", lambda e, yy=yy, k=k, tile=tile: e.indirect_dma_start(
                out=yy[:], out_offset=None, in_=Ys[:, :],
                in_offset=bass.IndirectOffsetOnAxis(ap=g.posi[:, tile, k:k + 1], axis=0),
                bounds_check=P.bound_reg(e), oob_is_err=False),
                reads=[g.b_Ys, g.b_posi], writes=[b_yy], key="cy%d_%d" % (k, s_))

    NPRE = 3
    for tile in range(min(NPRE, NT)):
        issue_loads(tile)
    for tile in range(NT):
        if tile + NPRE < NT:
            issue_loads(tile + NPRE)
        s_ = tile % NR
        x1, b_x1 = x1r[s_]
        y0, b_y0 = y0r[s_]
        y1, b_y1 = y1r[s_]
        r, b_r = rr.next()
        act(P, r[:], x1[:], AF.Copy, [b_x1], [b_r], scale=ALPHA)
        stt(P, r[:], y0[:], g.wts[:, tile, 0:1], r[:], ALU.mult, ALU.add, [b_y0, g.b_wts, b_r], [b_r])
        stt(P, r[:], y1[:], g.wts[:, tile, 1:2], r[:], ALU.mult, ALU.add, [b_y1, g.b_wts, b_r], [b_r])
        o, b_o = orr.next()
        emit_ln_rows(P, t, r[:], b_r, o[:], b_o, 0, D)
        P.dma("sp", out_ap[tile * 128:(tile + 1) * 128, :], o[:], reads=[b_o], cwrites=[b_out], key="co%d" % (orr.i % 3))
    P.end_phase()


def build_program(n_layers=1, debug=False):
    nc = bass.Bass("TRN2", target_bir_lowering=False)
    dr = {}

    def din(name, shape, dt=F32):
        dr[name] = nc.dram_tensor(name, list(shape), dt, kind="ExternalInput").ap()

    din("x", [T, D])
    din("ab_w_in", [D, 2560])
    din("ab_w_out", [D, D])
    din("pv0", [128, 168])
    din("cd_w_in", [D, 2560])
    din("cd_w_out", [D, D])
    din("pv1", [128, 12])
    din("bc1x", [128, 2048])
    din("wsT", [128, 1024])
    din("wsb", [128, 512])
    din("relP", [128, 8 * 640])
    for L in range(2):
        din("bcm%d" % L, [128, 2 * D + 36])
        din("bcf%d" % L, [128, 2 * D])
        din("wr%d" % L, [D, 36])
    din("moe_w_gate", [2, NE, D, DEX])
    din("moe_w_up", [2, NE, D, DEX])
    din("moe_w_down", [2, NE, DEX, D])
    dr["out"] = nc.dram_tensor("out", [T, D], F32, kind="ExternalOutput").ap()
    kind = "ExternalOutput" if debug else "Internal"
    dr["X1"] = nc.dram_tensor("X1", [T, D], F32, kind=kind).ap()
    dr["Xs"] = nc.dram_tensor("Xs", [NSLOT, D], BF16, kind=kind).ap()
    dr["Ys"] = nc.dram_tensor("Ys", [NSLOT, D], F32, kind=kind).ap()
    dr["X2"] = nc.dram_tensor("X2", [T, D], F32, kind="Internal").ap()
    P = Prog(nc)
    g = G()
    setup_globals(P, g)
    g.b_X1 = P.buf("X1")
    g.b_Xs = P.buf("Xs")
    g.b_Ys = P.buf("Ys")
    g.b_X2 = P.buf("X2")
    g.b_out = P.buf("out")
    phase_m0(P, g, dr)
    phase_e(P, g, dr, 0)
    if n_layers == 1:
        phase_c(P, g, dr, 0, dr["out"], g.b_out)
    else:
        phase_c(P, g, dr, 0, dr["X2"], g.b_X2)
        phase_m1(P, g, dr)
        phase_e(P, g, dr, 1)
        phase_c(P, g, dr, 1, dr["out"], g.b_out)
    if debug:
        dr["posd"] = nc.dram_tensor("posd", [128, NT * 2], I32, kind="ExternalOutput").ap()
        dr["wtsd"] = nc.dram_tensor("wtsd", [128, NT * 2], F32, kind="ExternalOutput").ap()
        P.begin_phase()
        P.dma("sp", dr["posd"][:, :], g.posi[:].rearrange("p a b -> p (a b)"), reads=[g.b_posi], key="dbg")
        P.dma("sp", dr["wtsd"][:, :], g.wts[:].rearrange("p a b -> p (a b)"), reads=[g.b_wts], key="dbg")
        P.end_phase()
    P.finish()
    return nc, P


def prep_inputs(inp):
    f = lambda a: np.ascontiguousarray(np.asarray(a, dtype=np.float32))
    shared = {}
    shared["ab_w_in"] = f(inp["ab_w_in"][0])
    shared["ab_w_out"] = f(inp["ab_w_out"][0])
    pv0 = np.zeros((128, 168), np.float32)
    pv0[:, 0:20] = f(inp["ab_b_in"][0]).reshape(20, 128).T
    adw = f(inp["a_dw"][0])
    pv0[:, 20:144] = adw.reshape(31, 4, 128).transpose(2, 1, 0).reshape(128, 124)
    pv0[:, 144:148] = f(inp["a_dw_b"][0]).reshape(4, 128).T
    pv0[:, 148:152] = f(inp["a_ln_g"][0]).reshape(4, 128).T
    pv0[:, 152:156] = f(inp["a_ln_b"][0]).reshape(4, 128).T
    bdw = f(inp["b_dw"][0])
    pv0[:, 156:168] = bdw.reshape(3, 4, 128).transpose(2, 1, 0).reshape(128, 12)
    shared["pv0"] = pv0
    shared["cd_w_in"] = f(inp["cd_w_in"][0])
    shared["cd_w_out"] = f(inp["cd_w_out"][0])
    cb = f(inp["cd_b_in"][0])
    pv1 = np.zeros((128, 12), np.float32)
    pv1[:, 0:4] = cb[0:512].reshape(4, 128).T
    pv1[:, 4:8] = cb[1024:1536].reshape(4, 128).T
    pv1[:, 8:12] = cb[1536:2048].reshape(4, 128).T
    shared["pv1"] = pv1
    row = np.concatenate([cb[512:1024], f(inp["c_ln_g"][0]), f(inp["c_ln_b"][0]), cb[2048:2560]])
    shared["bc1x"] = np.ascontiguousarray(np.broadcast_to(row[None, :], (128, 2048)))
    ws = f(inp["c_ws"][0])
    shared["wsT"] = np.ascontiguousarray(ws.transpose(2, 0, 1).reshape(128, 1024))
    wb = f(inp["c_ws_b"][0])
    shared["wsb"] = np.ascontiguousarray(wb.reshape(4, 2, 1, 128).repeat(64, axis=2).transpose(1, 2, 0, 3).reshape(128, 512))
    rb = f(inp["d_rel_bias"][0])
    p_ = np.arange(128)[:, None, None]
    s_ = np.arange(5)[None, :, None]
    c_ = np.arange(128)[None, None, :]
    par = c_ // 64
    i_ = c_ % 64
    delta = 64 * (8 + par - 2 * s_) + i_ - p_
    ridx = np.clip(delta, -256, 256) + 256
    cdist = 8 - 2 * s_ + par - (p_ // 64)
    valid = (cdist >= 0) & (cdist <= 8)
    relv = np.where(valid[None], rb[:, ridx], np.float32(-30000.0)).astype(np.float32)
    shared["relP"] = np.ascontiguousarray(relv.transpose(1, 0, 2, 3).reshape(128, 8 * 640))
    for L in range(2):
        row = np.concatenate([f(inp["mix_ln_g"][L]), f(inp["mix_ln_b"][L]), f(inp["moe_rg_b"][L]), f(inp["moe_re_b"][L]).reshape(-1)])
        shared["bcm%d" % L] = np.ascontiguousarray(np.broadcast_to(row[None, :], (128, row.size)))
        row = np.concatenate([f(inp["ffn_ln_g"][L]), f(inp["ffn_ln_b"][L])])
        shared["bcf%d" % L] = np.ascontiguousarray(np.broadcast_to(row[None, :], (128, row.size)))
        shared["wr%d" % L] = np.ascontiguousarray(np.concatenate(
            [f(inp["moe_rg_w"][L]), f(inp["moe_re_w"][L]).transpose(1, 0, 2).reshape(D, 32)], axis=1))
    shared["moe_w_gate"] = f(inp["moe_w_gate"])
    shared["moe_w_up"] = f(inp["moe_w_up"])
    shared["moe_w_down"] = f(inp["moe_w_down"])
    x = f(inp["x"]).reshape(NCORES, T, D)
    return shared, x


_CACHE = {}


def kernel(**inputs):
    shared, x = prep_inputs(inputs)
    if "nc" not in _CACHE:
        _CACHE["nc"] = build_program(n_layers=2)[0]
    nc = _CACHE["nc"]
    in_maps = []
    for c in range(NCORES):
        m = dict(shared)
        m["x"] = x[c]
        in_maps.append(m)
    res = run_bass_kernel_spmd(nc, in_maps, core_ids=list(range(NCORES)))
    out = np.stack([np.asarray(r["out"]) for r in res.results], 0)
    return out.reshape(16, SEQ, D).astype(np.float32)
```

```python
import numpy as np
from contextlib import ExitStack
import concourse.bass as bass
import concourse.mybir as mybir
from concourse.bass_utils import run_bass_kernel_spmd

F32 = mybir.dt.float32
BF16 = mybir.dt.bfloat16
I32 = mybir.dt.int32
AF = mybir.ActivationFunctionType
ALU = mybir.AluOpType
AX = mybir.AxisListType

NCORES = 8
D = 1024
SEQ = 2048
BPC = 2
T = BPC * SEQ
NT = T // 128
BLK = 512
NBLK = T // BLK
NE = 32
CAP = 384
NSLOT = NE * CAP
ALPHA = float(4 ** 0.25)
EPS = 1e-5
DEX = 512
import os
SKIP = set(os.environ.get("M1_SKIP", "").split(","))
ATT_T = os.environ.get("ATT_T", "1") == "1"


class Buf:
    __slots__ = ("name", "xw", "cw", "readers")

    def __init__(self, name=""):
        self.name = name
        self.xw = []
        self.cw = []
        self.readers = []

    def reset(self):
        self.xw = []
        self.cw = []
        self.readers = []


class Op:
    __slots__ = ("eng", "fn", "reads", "writes", "cwrites", "key", "deps", "signal", "seq", "kcount", "idx")

    def __init__(self, eng, fn, reads, writes, cwrites, key):
        self.eng = eng
        self.fn = fn
        self.reads = reads
        self.writes = writes
        self.cwrites = cwrites
        self.key = key
        self.deps = None
        self.signal = False
        self.seq = None
        self.kcount = None


class Ring:
    def __init__(self, items):
        self.items = items
        self.i = 0

    def next(self):
        it = self.items[self.i % len(self.items)]
        self.i += 1
        return it


class Prog:
    ENGS = ("pe", "act", "dve", "pool", "sp")

    def __init__(self, nc):
        self.nc = nc
        self.gstack = ExitStack()
        self.pstack = None
        self.ops = []
        self.bufs = []
        self.sems = {}
        self.eng_seq = {e: 0 for e in self.ENGS}
        self.key_count = {}
        self.phase_no = 0
        self.total_ops = 0
        self.uid = 0
        self.pending = None
        self._in_chain = False

    def sb(self, shape, dtype, name=None, glob=False):
        self.uid += 1
        st = self.gstack if glob else self.pstack
        return st.enter_context(self.nc.sbuf_tensor("%s_%d" % (name or "t", self.uid), list(shape), dtype))

    def ps(self, shape, dtype=F32, name=None):
        self.uid += 1
        return self.pstack.enter_context(self.nc.psum_tensor("%s_%d" % (name or "p", self.uid), list(shape), dtype))

    def buf(self, name=""):
        b = Buf(name)
        self.bufs.append(b)
        return b

    def sbb(self, shape, dtype, name=None, glob=False):
        return self.sb(shape, dtype, name, glob), self.buf(name or "")

    def psb(self, shape, dtype=F32, name=None):
        return self.ps(shape, dtype, name), self.buf(name or "")

    def sem(self, name):
        if name not in self.sems:
            self.sems[name] = self.gstack.enter_context(self.nc.semaphore(name))
        return self.sems[name]

    def op(self, eng, fn, reads=(), writes=(), cwrites=(), key=None):
        o = Op(eng, fn, tuple(reads), tuple(writes), tuple(cwrites), key)
        o.idx = len(self.ops)
        self.ops.append(o)
        if self.pending is not None and not self._in_chain and eng == "dve":
            self._in_chain = True
            try:
                next(self.pending)
            except StopIteration:
                self.pending = None
            self._in_chain = False
        return o

    def drain(self):
        if self.pending is not None:
            self._in_chain = True
            for _ in self.pending:
                pass
            self._in_chain = False
            self.pending = None

    def dma(self, eng, out, in_, reads=(), writes=(), cwrites=(), key=None, **kw):
        assert key is not None
        return self.op(eng, lambda e: e.dma_start(out=out, in_=in_, **kw), reads, writes, cwrites, key)

    def bound_reg(self, eng):
        if self._breg is None:
            self._breg = eng.to_reg(NSLOT - 1)
        return self._breg

    def begin_phase(self):
        self.pstack = ExitStack()
        self._breg = None
        self.ops = []
        for b in self.bufs:
            b.reset()

    def end_phase(self):
        self.drain()
        nc = self.nc
        ops = self.ops
        for o in ops:
            deps = set()
            for b in o.reads:
                deps.update(b.xw)
                deps.update(b.cw)
            for b in o.writes:
                deps.update(b.xw)
                deps.update(b.cw)
                deps.update(b.readers)
            for b in o.cwrites:
                deps.update(b.xw)
                deps.update(b.readers)
            deps.discard(o.idx)
            o.deps = deps
            for b in o.reads:
                b.readers.append(o.idx)
            for b in o.writes:
                b.xw = [o.idx]
                b.cw = []
                b.readers = []
            for b in o.cwrites:
                b.cw.append(o.idx)
        for o in ops:
            latest = {}
            ddeps = []
            for d in o.deps:
                p = ops[d]
                if p.key is not None:
                    ddeps.append(p)
                elif not (p.eng == "pe" and o.eng == "pe"):
                    q = latest.get(p.eng)
                    if q is None or q.idx < p.idx:
                        latest[p.eng] = p
            for p in latest.values():
                p.signal = True
            o.deps = ddeps + list(latest.values())
        last_compute = {}
        for o in ops:
            if o.key is None:
                last_compute[o.eng] = o
        for o in last_compute.values():
            o.signal = True
        for o in ops:
            if o.key is None and o.signal:
                self.eng_seq[o.eng] += 1
                o.seq = self.eng_seq[o.eng]
        waits = [None] * len(ops)
        kc = self.key_count
        keys_used = set()
        for o in ops:
            w = {}
            for p in o.deps:
                if p.key is not None:
                    nm = "k_" + p.key
                    v = 16 * kc[p.key]
                else:
                    if not p.signal:
                        continue
                    nm = "e_" + p.eng
                    v = p.seq
                if w.get(nm, 0) < v:
                    w[nm] = v
            waits[o.idx] = w
            if o.key is not None:
                kc[o.key] = kc.get(o.key, 0) + 1
                keys_used.add(o.key)
        for o in ops:
            for nm in waits[o.idx]:
                self.sem(nm)
            if o.key is not None:
                self.sem("k_" + o.key)
            elif o.signal:
                self.sem("e_" + o.eng)
        fin = {}
        for k in sorted(keys_used):
            fin["k_" + k] = 16 * kc[k]
        for e, o in last_compute.items():
            fin["e_" + e] = o.seq
        sems = self.sems
        by_eng = {e: [o for o in ops if o.eng == e] for e in self.ENGS}

        def run(ename, eng):
            waited = {}
            for o in by_eng[ename]:
                for nm, v in waits[o.idx].items():
                    if waited.get(nm, 0) >= v:
                        continue
                    waited[nm] = v
                    eng.wait_ge(sems[nm], v)
                ins = o.fn(eng)
                if o.key is not None:
                    ins.then_inc(sems["k_" + o.key], 16)
                elif o.signal:
                    ins.then_inc(sems["e_" + o.eng], 1)
            for nm, v in fin.items():
                if nm == "e_" + ename:
                    continue
                eng.wait_ge(sems[nm], v)
            if ename == "pool" and self._breg is not None:
                eng.free_register(self._breg)
                self._breg = None

        with nc.Block() as block:
            @block.tensor
            def _(e):
                run("pe", e)

            @block.scalar
            def _(e):
                run("act", e)

            @block.vector
            def _(e):
                run("dve", e)

            @block.gpsimd
            def _(e):
                run("pool", e)

            @block.sync
            def _(e):
                run("sp", e)
        self.total_ops += len(ops)
        self.pstack.close()
        self.pstack = None
        self.phase_no += 1

    def finish(self):
        self.gstack.close()


def mm(P, out, lhsT, rhs, start, stop, reads, pbuf):
    if start:
        P.op("pe", lambda e: e.matmul(out=out, lhsT=lhsT, rhs=rhs, start=True, stop=stop), reads=reads, writes=[pbuf])
    else:
        P.op("pe", lambda e: e.matmul(out=out, lhsT=lhsT, rhs=rhs, start=False, stop=stop), reads=reads, cwrites=[pbuf])


def tr(P, out, in_, ident, first, reads, pbuf):
    if first:
        P.op("pe", lambda e: e.transpose(out=out, in_=in_, identity=ident), reads=reads, writes=[pbuf])
    else:
        P.op("pe", lambda e: e.transpose(out=out, in_=in_, identity=ident), reads=reads, cwrites=[pbuf])


def act(P, out, in_, func, reads, writes, bias=None, scale=None, cwrites=(), accum_out=None):
    kw = {}
    if bias is not None:
        kw["bias"] = bias
    if scale is not None:
        kw["scale"] = scale
    if accum_out is not None:
        kw["accum_out"] = accum_out
    P.op("act", lambda e: e.activation(out=out, in_=in_, func=func, **kw), reads=reads, writes=writes, cwrites=cwrites)


def ts(P, eng, out, in0, s1, s2, op0, op1, reads, writes, cwrites=()):
    if s2 is None:
        P.op(eng, lambda e: e.tensor_scalar(out=out, in0=in0, scalar1=s1, scalar2=None, op0=op0), reads=reads, writes=writes, cwrites=cwrites)
    else:
        P.op(eng, lambda e: e.tensor_scalar(out=out, in0=in0, scalar1=s1, scalar2=s2, op0=op0, op1=op1), reads=reads, writes=writes, cwrites=cwrites)


def tt(P, eng, out, in0, in1, op, reads, writes, cwrites=()):
    P.op(eng, lambda e: e.tensor_tensor(out=out, in0=in0, in1=in1, op=op), reads=reads, writes=writes, cwrites=cwrites)


def stt(P, out, in0, scalar, in1, op0, op1, reads, writes, cwrites=()):
    P.op("dve", lambda e: e.scalar_tensor_tensor(out=out, in0=in0, scalar=scalar, in1=in1, op0=op0, op1=op1),
         reads=reads, writes=writes, cwrites=cwrites)


def cp(P, eng, out, in_, reads, writes, cwrites=()):
    if eng == "act":
        P.op("act", lambda e: e.activation(out=out, in_=in_, func=AF.Copy), reads=reads, writes=writes, cwrites=cwrites)
    else:
        P.op(eng, lambda e: e.tensor_copy(out=out, in_=in_), reads=reads, writes=writes, cwrites=cwrites)


class G:
    pass


def setup_globals(P, g):
    g.ident_f, g.b_ident_f = P.sbb([128, 128], F32, "identf", glob=True)
    g.ident_b, g.b_ident_b = P.sbb([128, 128], BF16, "identb", glob=True)
    g.U_b, g.b_U = P.sbb([128, 128], BF16, "U", glob=True)
    g.ones_b, g.b_ones = P.sbb([128, 128], BF16, "ones", glob=True)
    g.eC, g.b_eC = P.sbb([128, NE], F32, "eC", glob=True)
    g.runc, g.b_runc = P.sbb([128, NE], F32, "runc", glob=True)
    g.posi, g.b_posi = P.sbb([128, NT, 2], I32, "posi", glob=True)
    g.wts, g.b_wts = P.sbb([128, NT, 2], F32, "wts", glob=True)
    g.eCi, g.b_eCi = P.sbb([128, NE], I32, "eCi", glob=True)


def emit_const_init(P, g):
    P.op("pool", lambda e: e.memset(g.ident_f[:], 0.0), writes=[g.b_ident_f])
    P.op("pool", lambda e: e.affine_select(out=g.ident_f[:], in_=g.ident_f[:], pattern=[[-1, 128]],
                                           compare_op=ALU.not_equal, fill=1.0, base=0, channel_multiplier=1),
         writes=[g.b_ident_f])
    cp(P, "pool", g.ident_b[:], g.ident_f[:], [g.b_ident_f], [g.b_ident_b])
    P.op("pool", lambda e: e.memset(g.ones_b[:], 1.0), writes=[g.b_ones])
    P.op("pool", lambda e: e.affine_select(out=g.U_b[:], in_=g.ones_b[:], pattern=[[1, 128]],
                                           compare_op=ALU.is_gt, fill=0.0, base=0, channel_multiplier=-1),
         reads=[g.b_ones], writes=[g.b_U])
    P.op("pool", lambda e: e.iota(g.eCi[:], pattern=[[CAP, NE]], base=0, channel_multiplier=0), writes=[g.b_eCi])
    cp(P, "pool", g.eC[:], g.eCi[:], [g.b_eCi], [g.b_eC])


def emit_zero_fill(P, g, dr):
    zt, b_zt = P.sbb([128, D], BF16, "zt")
    P.op("pool", lambda e: e.memset(zt[:], 0.0), writes=[b_zt])
    Xv = dr["Xs"].rearrange("(n p) d -> p n d", p=128)
    for n in range(NSLOT // 128):
        P.dma("sp", Xv[:, n, :], zt[:], reads=[b_zt], writes=[g.b_Xs] if n == 0 else (), cwrites=() if n == 0 else [g.b_Xs], key="zf")


def alloc_tail(P, g, L):
    t = G()
    t.bc, t.b_bc = P.sbb([128, 2 * D + 36], F32, "bc")
    t.wr, t.b_wr = P.sbb([128, 8, 36], F32, "wr")
    t.x1b = P.sb([128, 4, D], BF16, "x1b")
    t.b_x1b = [P.buf("x1b%d" % i) for i in range(4)]
    t.x1T = Ring([P.sbb([128, 8, 128], F32, "x1T") for _ in range(1)])
    t.lnr = Ring([make_ln_scratch(P) for _ in range(4)])
    t.sm, t.b_sm = P.sbb([128, 928], F32, "small")
    t.oh, t.b_oh = P.sbb([128, 4, NE], BF16, "oh")
    t.ptr = Ring([P.psb([128, 512], F32, "ptrT") for _ in range(2)])
    t.psm, t.b_psm = P.psb([128, 512], F32, "psm")
    return t


def load_tail_consts(P, g, t, dr, L):
    P.dma("sp", t.bc[:], dr["bcm%d" % L][:, :], writes=[t.b_bc], key="cst")
    P.dma("sp", t.wr[:], dr["wr%d" % L].rearrange("(c p) n -> p c n", p=128), writes=[t.b_wr], key="cst")


def make_ln_scratch(P):
    sc = G()
    sc.st, sc.b_st = P.sbb([128, 2, 6], F32, "bnst")
    sc.mv, sc.b_mv = P.sbb([128, 2], F32, "mv")
    sc.rstd, sc.b_rstd = P.sbb([128, 1], F32, "rstd")
    return sc


def emit_ln_rows(P, t, src, b_src, dst, b_dst, goff, boff):
    sc = t.lnr.next()
    for h in range(2):
        P.op("dve", lambda e, h=h: e.bn_stats(out=sc.st[:, h, :], in_=src[:, h * 512:(h + 1) * 512]),
             reads=[b_src], writes=[sc.b_st] if h == 0 else (), cwrites=() if h == 0 else [sc.b_st])
    P.op("dve", lambda e: e.bn_aggr(out=sc.mv[:], in_=sc.st[:].rearrange("p a b -> p (a b)")), reads=[sc.b_st], writes=[sc.b_mv])
    act(P, sc.rstd[:], sc.mv[:, 1:2], AF.Sqrt, [sc.b_mv], [sc.b_rstd], bias=EPS)
    P.op("dve", lambda e: e.reciprocal(out=sc.rstd[:], in_=sc.rstd[:]), reads=[sc.b_rstd], writes=[sc.b_rstd])
    ts(P, "dve", dst, src, sc.mv[:, 0:1], sc.rstd[:, 0:1], ALU.subtract, ALU.mult, [b_src, sc.b_mv, sc.b_rstd], [b_dst])
    tt(P, "dve", dst, dst, t.bc[:, goff:goff + D], ALU.mult, [b_dst, t.b_bc], [b_dst])
    tt(P, "pool", dst, dst, t.bc[:, boff:boff + D], ALU.add, [b_dst, t.b_bc], [b_dst])


def emit_tail_block(P, g, t, xb, b_xt, catT, b_cat, wout, b_wout, pmm, gb, dr):
    sm = t.sm
    S = [t.b_sm]
    for i in range(4):
        tile = gb * 4 + i
        xi = xb[:, i, :]
        for hf in range(2):
            pm, b_pm = pmm.next()
            for c in range(8):
                mm(P, pm[:], catT[:, c, i * 128:(i + 1) * 128], wout[:, c, hf * 512:(hf + 1) * 512], c == 0, c == 7,
                   [b_cat[c], b_wout], b_pm)
            stt(P, xb[:, i, hf * 512:(hf + 1) * 512], xb[:, i, hf * 512:(hf + 1) * 512], ALPHA, pm[:], ALU.mult, ALU.add,
                [b_xt[i], b_pm], [b_xt[i]])
        emit_ln_rows(P, t, xi, b_xt[i], xi, b_xt[i], 0, D)
        P.dma("sp", dr["X1"][tile * 128:(tile + 1) * 128, :], xi, reads=[b_xt[i]], cwrites=[g.b_X1], key="x1st%d" % i)
    P.drain()
    for i in range(4):
        cp(P, "act", t.x1b[:, i, :], xb[:, i, :], [b_xt[i]], [t.b_x1b[i]])
    for i in range(4):
        x1T, b_x1T = t.x1T.next()
        for half in range(2):
            pt, b_pt = t.ptr.next()
            for cc in range(4):
                c = half * 4 + cc
                tr(P, pt[:, cc * 128:(cc + 1) * 128], xb[:, i, c * 128:(c + 1) * 128], g.ident_f[:], cc == 0,
                   [b_xt[i], g.b_ident_f], b_pt)
            cp(P, "act" if half == 0 else "dve", x1T[:, half * 4:(half + 1) * 4, :],
               pt[:].rearrange("p (c n) -> p c n", c=4), [b_pt], [b_x1T] if half == 0 else (),
               cwrites=() if half == 0 else [b_x1T])
        for c in range(8):
            o_ = t.psm[:, i * 36:(i + 1) * 36]
            if i == 0 and c == 0:
                P.op("pe", lambda e, o_=o_, l=x1T[:, c, :], r=t.wr[:, c, :]: e.matmul(out=o_, lhsT=l, rhs=r, start=True, stop=False),
                     reads=[b_x1T, t.b_wr], writes=[t.b_psm])
            else:
                P.op("pe", lambda e, o_=o_, l=x1T[:, c, :], r=t.wr[:, c, :], st=(c == 0), sp=(c == 7):
                     e.matmul(out=o_, lhsT=l, rhs=r, start=st, stop=sp), reads=[b_x1T, t.b_wr], cwrites=[t.b_psm])
    P.pending = router_chain(P, g, t, gb, dr)


def router_chain(P, g, t, gb, dr):
    sm = t.sm
    S = [t.b_sm]
    LG = sm[:, 0:144].rearrange("p (a b) -> p a b", a=4)
    gmax = sm[:, 144:148]
    gd = sm[:, 148:164].rearrange("p (a b) -> p a b", a=4)
    gsel = sm[:, 164:180].rearrange("p (a b) -> p a b", a=4)
    gexp = sm[:, 180:196].rearrange("p (a b) -> p a b", a=4)
    gsum = sm[:, 196:200]
    gtop = sm[:, 200:204]
    pen = sm[:, 204:220].rearrange("p (a b) -> p a b", a=4)
    masked = sm[:, 220:348]
    top8 = sm[:, 348:380].rearrange("p (a b) -> p a b", a=4)
    oh1 = sm[:, 380:508].rearrange("p (a b) -> p a b", a=4)
    oh2 = sm[:, 508:636].rearrange("p (a b) -> p a b", a=4)
    dd = sm[:, 636:640]
    ee = sm[:, 640:644]
    den = sm[:, 644:648]
    wtmp = sm[:, 648:656].rearrange("p (a b) -> p a b", a=4)
    rank = sm[:, 656:784].rearrange("p (a b) -> p a b", a=4)
    tmp = sm[:, 784:912].rearrange("p (a b) -> p a b", a=4)
    posf = sm[:, 912:920].rearrange("p (a b) -> p a b", a=4)
    valid = sm[:, 920:928].rearrange("p (a b) -> p a b", a=4)
    tt(P, "dve", LG, t.psm[:, 0:144].rearrange("p (a b) -> p a b", a=4),
       t.bc[:, 2 * D:2 * D + 36].unsqueeze(1).to_broadcast([128, 4, 36]), ALU.add, [t.b_psm, t.b_bc], S)
    yield
    P.op("dve", lambda e: e.tensor_reduce(out=gmax, in_=LG[:, :, 0:4], axis=AX.X, op=ALU.max), reads=S, writes=S)
    yield
    tt(P, "dve", gd, LG[:, :, 0:4], gmax.unsqueeze(2).to_broadcast([128, 4, 4]), ALU.subtract, S, S)
    yield
    ts(P, "dve", gsel, gd, 0.0, None, ALU.is_equal, None, S, S)
    yield
    act(P, gexp, gd, AF.Exp, S, S)
    yield
    P.op("dve", lambda e: e.tensor_reduce(out=gsum, in_=gexp, axis=AX.X, op=ALU.add), reads=S, writes=S)
    yield
    P.op("dve", lambda e: e.reciprocal(out=gtop, in_=gsum), reads=S, writes=S)
    yield
    ts(P, "dve", pen, gsel, 1e30, -1e30, ALU.mult, ALU.add, S, S)
    yield
    tt(P, "dve", masked.rearrange("p (a b c) -> p a b c", a=4, b=4), LG[:, :, 4:36].rearrange("p a (b c) -> p a b c", b=4),
       pen.unsqueeze(3).to_broadcast([128, 4, 4, 8]), ALU.add, S, S)
    yield
    for i in range(4):
        P.op("dve", lambda e, i=i: e.max(out=top8[:, i, :], in_=masked[:, i * 32:(i + 1) * 32]), reads=S, writes=S)
        yield
    m3 = masked.rearrange("p (a b) -> p a b", a=4)
    tt(P, "dve", oh1, m3, top8[:, :, 0:1].to_broadcast([128, 4, 32]), ALU.is_equal, S, S)
    yield
    tt(P, "dve", oh2, m3, top8[:, :, 1:2].to_broadcast([128, 4, 32]), ALU.is_equal, S, S)
    yield
    tt(P, "dve", dd, top8[:, :, 1], top8[:, :, 0], ALU.subtract, S, S)
    yield
    act(P, ee, dd, AF.Exp, S, S)
    yield
    ts(P, "dve", den, ee, 1.0, None, ALU.add, None, S, S)
    yield
    P.op("dve", lambda e: e.reciprocal(out=den, in_=den), reads=S, writes=S)
    yield
    tt(P, "dve", wtmp[:, :, 0], den, gtop, ALU.mult, S, S)
    yield
    tt(P, "dve", wtmp[:, :, 1], wtmp[:, :, 0], ee, ALU.mult, S, S)
    yield
    tt(P, "dve", t.oh[:], oh1, oh2, ALU.add, S, [t.b_oh])
    yield
    for i in range(4):
        o_ = t.psm[:, 256 + i * 32:256 + (i + 1) * 32]
        seq = [(g.U_b, i)] + [(g.ones_b, j) for j in range(i)]
        for n_, (lt, j) in enumerate(seq):
            P.op("pe", lambda e, o_=o_, l=lt[:], r=t.oh[:, j, :], st=(n_ == 0), sp=(n_ == len(seq) - 1):
                 e.matmul(out=o_, lhsT=l, rhs=r, start=st, stop=sp), reads=[g.b_U, g.b_ones, t.b_oh], cwrites=[t.b_psm])
            yield
    for j in range(4):
        P.op("pe", lambda e, j=j: e.matmul(out=t.psm[:, 448:480], lhsT=g.ones_b[:], rhs=t.oh[:, j, :], start=(j == 0), stop=(j == 3)),
             reads=[g.b_ones, t.b_oh], cwrites=[t.b_psm])
        yield
    tt(P, "dve", rank, t.psm[:, 256:384].rearrange("p (a b) -> p a b", a=4), g.runc[:].unsqueeze(1).to_broadcast([128, 4, NE]),
       ALU.add, [t.b_psm, g.b_runc], S)
    yield
    tt(P, "dve", g.runc[:], g.runc[:], t.psm[:, 448:480], ALU.add, [t.b_psm, g.b_runc], [g.b_runc])
    yield
    ts(P, "dve", tmp, rank, float(CAP), 1.0e6, ALU.is_ge, ALU.mult, S, S)
    yield
    tt(P, "dve", rank, rank, tmp, ALU.add, S, S)
    yield
    tt(P, "dve", rank, rank, g.eC[:].unsqueeze(1).to_broadcast([128, 4, NE]), ALU.add, S + [g.b_eC], S)
    yield
    tt(P, "dve", tmp, rank, oh1, ALU.mult, S, S)
    yield
    P.op("dve", lambda e: e.tensor_reduce(out=posf[:, :, 0], in_=tmp, axis=AX.X, op=ALU.add), reads=S, writes=S)
    yield
    tt(P, "dve", tmp, rank, oh2, ALU.mult, S, S)
    yield
    P.op("dve", lambda e: e.tensor_reduce(out=posf[:, :, 1], in_=tmp, axis=AX.X, op=ALU.add), reads=S, writes=S)
    yield
    ts(P, "dve", valid, posf, float(NSLOT) - 0.5, None, ALU.is_lt, None, S, S)
    yield
    tt(P, "dve", g.wts[:, gb * 4:(gb + 1) * 4, :], wtmp, valid, ALU.mult, S, [g.b_wts])
    yield
    cp(P, "dve", g.posi[:, gb * 4:(gb + 1) * 4, :], posf, S, [g.b_posi])
    yield
    Xs = dr["Xs"]
    for i in range(4):
        tile = gb * 4 + i
        for k in range(2):
            P.op("pool", lambda e, k=k, tile=tile, i=i: e.indirect_dma_start(
                out=Xs[:, :], out_offset=bass.IndirectOffsetOnAxis(ap=g.posi[:, tile, k:k + 1], axis=0),
                in_=t.x1b[:, i, :], in_offset=None, bounds_check=P.bound_reg(e), oob_is_err=False),
                reads=[t.b_x1b[i], g.b_posi], cwrites=[g.b_Xs], key="sc%d" % i)
            yield


def load_weight_bf16(P, dst, b_dst, src_ap, n_k, ncols, key):
    first = True
    for c0 in range(0, ncols, 512):
        cw = min(512, ncols - c0)
        P.dma("pool", dst[:, :, c0:c0 + cw], src_ap.rearrange("(c p) n -> p c n", p=128)[:, :, c0:c0 + cw],
              writes=[b_dst] if first else (), cwrites=() if first else [b_dst], key=key)
        first = False


def phase_m0(P, g, dr):
    P.begin_phase()
    if not hasattr(g, "inited"):
        emit_const_init(P, g)
        g.inited = True
    P.op("pool", lambda e: e.memset(g.runc[:], 0.0), writes=[g.b_runc])
    t = alloc_tail(P, g, 0)
    load_tail_consts(P, g, t, dr, 0)
    pv, b_pv = P.sbb([128, 168], F32, "pv0")
    P.dma("sp", pv[:], dr["pv0"][:, :], writes=[b_pv], key="cst")
    win, b_win = P.sbb([128, 8, 2560], BF16, "win")
    wout, b_wout = P.sbb([128, 8, 1024], BF16, "wout")
    load_weight_bf16(P, win, b_win, dr["ab_w_in"], 8, 2560, "win")
    load_weight_bf16(P, wout, b_wout, dr["ab_w_out"], 8, 1024, "wout")
    dg, b_dg = P.sbb([128, 4 * 31, 128], BF16, "diag")
    for j in range(4 * 31):
        ts(P, "pool" if j % 2 else "dve", dg[:, j, :], g.ident_f[:], pv[:, 20 + j:21 + j], None, ALU.mult, None,
           [g.b_ident_f, b_pv], [b_dg] if j == 0 else (), cwrites=() if j == 0 else [b_dg])
    onesM, b_onesM = P.sbb([128, 128], BF16, "onesM")
    P.op("pool", lambda e: e.memset(onesM[:], 1.0 / 512.0), writes=[b_onesM])

    xring = Ring([(P.sb([128, 4, D], F32, "xblk"), [P.buf("xt%d" % i) for i in range(4)]) for _ in range(2)])
    xT, b_xT = P.sbb([128, 8, BLK], BF16, "xT")
    Ain, b_Ain = P.sbb([128, 4, 30 + BLK], BF16, "Ain")
    Bin, b_Bin = P.sbb([128, 4, 2 + BLK], F32, "Bin")
    sig = Ring([P.sbb([128, BLK], F32, "sig") for _ in range(1)])
    y32, b_y32 = P.sbb([128, 4, BLK], F32, "y32")
    ybf, b_ybf = P.sbb([128, 4, BLK], BF16, "ybf")
    ysq, b_ysq = P.sbb([128, 4, BLK], BF16, "ysq")
    mean, b_mean = P.sbb([128, BLK], F32, "mean")
    rstd, b_rstd = P.sbb([128, BLK], F32, "rstdA")
    tmpA = Ring([P.sbb([128, BLK], F32, "tmpA") for _ in range(1)])
    catT, b_catT = P.sbb([128, 8, BLK], BF16, "catT")
    b_cat = [P.buf("cat%d" % i) for i in range(8)]
    gc = Ring([P.sbb([128, BLK], F32, "gc") for _ in range(1)])
    acc = Ring([P.sbb([128, BLK], F32, "acc") for _ in range(1)])
    pmm = Ring([P.psb([128, 512], F32, "pmm") for _ in range(3)])
    pst0, b_pst0 = P.psb([128, 512], F32, "pst0")
    pst1, b_pst1 = P.psb([128, 512], F32, "pst1")
    ptr = t.ptr

    x_dr = dr["x"]

    def load_x(gbn):
        xb_, b_ = xring.items[gbn % 2]
        P.dma("sp", xb_[:], x_dr[gbn * BLK:(gbn + 1) * BLK, :].rearrange("(i p) d -> p i d", p=128),
              writes=b_, key="xblk%d" % (gbn % 2))

    for gb in range(NBLK):
        seq_start = (gb % (SEQ // BLK)) == 0
        if gb == 0:
            load_x(0)
        if gb + 1 < NBLK:
            load_x(gb + 1)
        if gb == 0:
            emit_zero_fill(P, g, dr)
        xblk, b_xt = xring.items[gb % 2]
        for c in range(8):
            pt, b_pt = ptr.next()
            for i in range(4):
                tr(P, pt[:, i * 128:(i + 1) * 128], xblk[:, i, c * 128:(c + 1) * 128], g.ident_f[:], i == 0,
                   [b_xt[i], g.b_ident_f], b_pt)
            cp(P, "act" if c % 2 == 0 else "dve", xT[:, c, :], pt[:], [b_pt], [b_xT] if c == 0 else (),
               cwrites=() if c == 0 else [b_xT])
        if seq_start:
            P.op("pool", lambda e: e.memset(Ain[:, :, 0:30], 0.0), writes=[b_Ain])
            P.op("pool", lambda e: e.memset(Bin[:, :, 0:2], 0.0), writes=[b_Bin])

        def inproj(m):
            pm, b_pm = pmm.next()
            for c in range(8):
                mm(P, pm[:], win[:, c, m * 128:(m + 1) * 128], xT[:, c, :], c == 0, c == 7, [b_win, b_xT], b_pm)
            return pm, b_pm

        for q in range(4):
            pm, b_pm = inproj(4 + q)
            sg, b_sg = sig.next()
            act(P, sg[:], pm[:], AF.Sigmoid, [b_pm, b_pv], [b_sg], bias=pv[:, 4 + q:5 + q])
            pm2, b_pm2 = inproj(q)
            stt(P, Ain[:, q, 30:30 + BLK], pm2[:], pv[:, q:q + 1], sg[:], ALU.add, ALU.mult,
                [b_pm2, b_pv, b_sg], (), cwrites=[b_Ain])
        for q in range(4):
            pm, b_pm = pmm.next()
            for k in range(31):
                mm(P, pm[:], dg[:, q * 31 + k, :], Ain[:, q, k:k + BLK], k == 0, k == 30, [b_dg, b_Ain], b_pm)
            act(P, y32[:, q, :], pm[:], AF.Identity, [b_pm, b_pv], [b_y32] if q == 0 else (), bias=pv[:, 144 + q:145 + q],
                cwrites=() if q == 0 else [b_y32])
            act(P, ysq[:, q, :], pm[:], AF.Square, [b_pm, b_pv], [b_ysq] if q == 0 else (), bias=pv[:, 144 + q:145 + q],
                cwrites=() if q == 0 else [b_ysq])
            cp(P, "dve", ybf[:, q, :], y32[:, q, :], [b_y32], [b_ybf] if q == 0 else (), cwrites=() if q == 0 else [b_ybf])
        cp(P, "pool", Ain[:, :, 0:30], Ain[:, :, BLK:BLK + 30], [b_Ain], [b_Ain])
        for q in range(4):
            mm(P, pst0[:], onesM[:], ybf[:, q, :], q == 0, q == 3, [b_onesM, b_ybf], b_pst0)
        for q in range(4):
            mm(P, pst1[:], onesM[:], ysq[:, q, :], q == 0, q == 3, [b_onesM, b_ysq], b_pst1)
        cp(P, "act", mean[:], pst0[:], [b_pst0], [b_mean])
        tt(P, "dve", rstd[:], mean[:], mean[:], ALU.mult, [b_mean], [b_rstd])
        tt(P, "dve", rstd[:], pst1[:], rstd[:], ALU.subtract, [b_pst1, b_rstd], [b_rstd])
        ts(P, "dve", rstd[:], rstd[:], 0.0, None, ALU.max, None, [b_rstd], [b_rstd])
        act(P, rstd[:], rstd[:], AF.Sqrt, [b_rstd], [b_rstd], bias=EPS)
        P.op("dve", lambda e: e.reciprocal(out=rstd[:], in_=rstd[:]), reads=[b_rstd], writes=[b_rstd])
        for q in range(4):
            tA, b_tA = tmpA.next()
            tt(P, "dve", tA[:], y32[:, q, :], mean[:], ALU.subtract, [b_y32, b_mean], [b_tA])
            tt(P, "pool", tA[:], tA[:], rstd[:], ALU.mult, [b_tA, b_rstd], [b_tA])
            act(P, catT[:, q, :], tA[:], AF.Silu, [b_tA, b_pv], [b_cat[q]], bias=pv[:, 152 + q:153 + q], scale=pv[:, 148 + q:149 + q])
        for q in range(4):
            pm, b_pm = inproj(12 + q)
            gcq, b_gc = gc.next()
            act(P, gcq[:], pm[:], AF.Identity, [b_pm, b_pv], [b_gc], bias=pv[:, 12 + q:13 + q])
            pm2, b_pm2 = inproj(16 + q)
            stt(P, Bin[:, q, 2:2 + BLK], pm2[:], pv[:, 16 + q:17 + q], gcq[:], ALU.add, ALU.mult,
                [b_pm2, b_pv, b_gc], (), cwrites=[b_Bin])
            ac, b_ac = acc.next()
            ts(P, "dve", ac[:], Bin[:, q, 0:BLK], pv[:, 156 + q * 3:157 + q * 3], None, ALU.mult, None, [b_Bin, b_pv], [b_ac])
            stt(P, ac[:], Bin[:, q, 1:1 + BLK], pv[:, 157 + q * 3:158 + q * 3], ac[:], ALU.mult, ALU.add, [b_Bin, b_pv, b_ac], [b_ac])
            stt(P, ac[:], Bin[:, q, 2:2 + BLK], pv[:, 158 + q * 3:159 + q * 3], ac[:], ALU.mult, ALU.add, [b_Bin, b_pv, b_ac], [b_ac])
            pm3, b_pm3 = inproj(8 + q)
            stt(P, catT[:, 4 + q, :], pm3[:], pv[:, 8 + q:9 + q], ac[:], ALU.add, ALU.mult, [b_pm3, b_pv, b_ac], [b_cat[4 + q]])
        cp(P, "pool", Bin[:, :, 0:2], Bin[:, :, BLK:BLK + 2], [b_Bin], [b_Bin])
        emit_tail_block(P, g, t, xblk, b_xt, catT, b_cat, wout, b_wout, pmm, gb, dr)
    P.end_phase()


def phase_m1(P, g, dr):
    P.begin_phase()
    if not hasattr(g, "inited"):
        emit_const_init(P, g)
        g.inited = True
    P.op("pool", lambda e: e.memset(g.runc[:], 0.0), writes=[g.b_runc])
    t = alloc_tail(P, g, 1)
    load_tail_consts(P, g, t, dr, 1)
    pv, b_pv = P.sbb([128, 12], F32, "pv1")
    P.dma("sp", pv[:], dr["pv1"][:, :], writes=[b_pv], key="cst")
    bx, b_bx = P.sbb([128, 2048], F32, "bc1x")
    P.dma("sp", bx[:], dr["bc1x"][:, :], writes=[b_bx], key="cst")
    win, b_win = P.sbb([128, 8, 2560], BF16, "win")
    wout, b_wout = P.sbb([128, 8, 1024], BF16, "wout")
    load_weight_bf16(P, win, b_win, dr["cd_w_in"], 8, 2560, "win")
    load_weight_bf16(P, wout, b_wout, dr["cd_w_out"], 8, 1024, "wout")
    wsT, b_wsT = P.sbb([128, 8, 128], BF16, "wsT")
    P.dma("pool", wsT[:].rearrange("p h i -> p (h i)"), dr["wsT"][:, :], writes=[b_wsT], key="wsT")
    P.op("pool", lambda e: e.memset(wsT[64:128, :, 0:64], 0.0), writes=[b_wsT])
    wsb, b_wsb = P.sbb([128, 4, 128], F32, "wsb")
    P.dma("sp", wsb[:].rearrange("p a b -> p (a b)"), dr["wsb"][:, :], writes=[b_wsb], key="cst")
    relP, b_relP = P.sbb([128, 8, 640], BF16, "relP")
    for hh_ in range(8):
        P.dma("pool", relP[:, hh_, :], dr["relP"][:, hh_ * 640:(hh_ + 1) * 640], writes=[b_relP] if hh_ == 0 else (),
              cwrites=() if hh_ == 0 else [b_relP], key="relP")

    xblk = P.sb([128, 4, D], F32, "xblk")
    b_xt = [P.buf("xt%d" % i) for i in range(4)]
    xT, b_xT = P.sbb([128, 8, BLK], BF16, "xT")
    vt, b_vt = P.sbb([128, 512], F32, "vt")
    vn, b_vn = P.sbb([128, 4, 512], BF16, "vn")
    st1, b_st1 = P.sbb([128, 6], F32, "st1")
    mv1, b_mv1 = P.sbb([128, 2], F32, "mv1")
    rs1, b_rs1 = P.sbb([128, 1], F32, "rs1")
    qz, b_qz = P.sbb([128, 8, BLK], BF16, "qz")
    P.op("pool", lambda e: e.memset(qz[:].rearrange("p a b -> p (a b)"), 0.0), writes=[b_qz])
    osb, b_osb = (None, None) if ATT_T else P.sbb([128, 264], F32, "osb")
    rden, b_rden = P.sbb([128, 512], F32, "rden")
    kT = P.sb([128, 4, SEQ], BF16, "kT")
    b_kT = [P.buf("kT%d" % j) for j in range(4)]
    Va = P.sb([128, 16, 8, 64 if ATT_T else 66], BF16, "Va")
    b_Va = [P.buf("Va%d" % j) for j in range(4)]
    gt, b_gt = P.sbb([128, 512], F32, "gt")
    catT = P.sb([128, 8, BLK], BF16, "catT")
    b_cat = [P.buf("cat%d" % i) for i in range(8)]
    pT = Ring([P.sbb([128, 640], BF16, "pT") for _ in range(2)])
    otok, b_otok = (None, None) if ATT_T else P.sbb([128, 512], F32, "otok")
    rcp, b_rcp = (None, None) if ATT_T else P.sbb([128, 8], F32, "rcp")
    pmm = Ring([P.psb([128, 512], F32, "pmm") for _ in range(3)])
    pO = [P.psb([128, 512], F32, "pO") for _ in range(2)]
    ptr = t.ptr
    spairs = Ring([(pmm.items[0], pmm.items[1]), (pmm.items[2], ptr.items[0])])
    P.op("pool", lambda e: e.memset(Va[:].rearrange("p a b c -> p (a b c)"), 1.0), writes=b_Va)

    x_dr = dr["X2"]
    for gb in range(NBLK):
        jj = gb % 4
        P.dma("sp", xblk[:], x_dr[gb * BLK:(gb + 1) * BLK, :].rearrange("(i p) d -> p i d", p=128),
              reads=[g.b_X2], writes=b_xt, key="xblk0")
        if gb == 0:
            emit_zero_fill(P, g, dr)
        for c in range(8):
            pt, b_pt = ptr.next()
            for i in range(4):
                tr(P, pt[:, i * 128:(i + 1) * 128], xblk[:, i, c * 128:(c + 1) * 128], g.ident_f[:], i == 0,
                   [b_xt[i], g.b_ident_f], b_pt)
            cp(P, "act" if c % 2 == 0 else "dve", xT[:, c, :], pt[:], [b_pt], [b_xT] if c == 0 else (),
               cwrites=() if c == 0 else [b_xT])

        def inproj_fm(m):
            pm, b_pm = pmm.next()
            for c in range(8):
                mm(P, pm[:], win[:, c, m * 128:(m + 1) * 128], xT[:, c, :], c == 0, c == 7, [b_win, b_xT], b_pm)
            return pm, b_pm

        def inproj_tm(i, col0):
            pm, b_pm = pmm.next()
            for c in range(8):
                mm(P, pm[:], xT[:, c, i * 128:(i + 1) * 128], win[:, c, col0:col0 + 512], c == 0, c == 7, [b_win, b_xT], b_pm)
            return pm, b_pm

        for i in range(4):
            pm, b_pm = inproj_tm(i, 512)
            tt(P, "dve", vt[:], pm[:], bx[:, 0:512], ALU.add, [b_pm, b_bx], [b_vt])
            P.op("dve", lambda e: e.bn_stats(out=st1[:], in_=vt[:]), reads=[b_vt], writes=[b_st1])
            P.op("dve", lambda e: e.bn_aggr(out=mv1[:], in_=st1[:]), reads=[b_st1], writes=[b_mv1])
            act(P, rs1[:], mv1[:, 1:2], AF.Sqrt, [b_mv1], [b_rs1], bias=EPS)
            P.op("dve", lambda e: e.reciprocal(out=rs1[:], in_=rs1[:]), reads=[b_rs1], writes=[b_rs1])
            ts(P, "dve", vt[:], vt[:], mv1[:, 0:1], rs1[:, 0:1], ALU.subtract, ALU.mult, [b_vt, b_mv1, b_rs1], [b_vt])
            tt(P, "pool", vt[:], vt[:], bx[:, 512:1024], ALU.mult, [b_vt, b_bx], [b_vt])
            tt(P, "pool", vn[:, i, :], vt[:], bx[:, 1024:1536], ALU.add, [b_vt, b_bx], [b_vn] if i == 0 else (),
               cwrites=() if i == 0 else [b_vn])
        for i in range(4):
            tl = jj * 4 + i
            pm, b_pm = inproj_tm(i, 2048)
            tt(P, "dve", Va[:, tl, :, 0:64], pm[:].rearrange("p (h c) -> p h c", h=8),
               bx[:, 1536:2048].rearrange("p (h c) -> p h c", h=8), ALU.add, [b_pm, b_bx],
               [b_Va[jj]] if i == 0 else (), cwrites=() if i == 0 else [b_Va[jj]])
        for qc in range(4):
            pm, b_pm = inproj_fm(12 + qc)
            act(P, kT[:, qc, jj * BLK:(jj + 1) * BLK], pm[:], AF.Identity, [b_pm, b_pv], [b_kT[jj]] if qc == 0 else (),
                bias=pv[:, 8 + qc:9 + qc], cwrites=() if qc == 0 else [b_kT[jj]])
        for qc in range(4):
            pm, b_pm = inproj_fm(8 + qc)
            for hh in range(2):
                ps_ = slice(hh * 64, (hh + 1) * 64)
                ts(P, "dve", qz[ps_, 2 * qc + hh, :], pm[ps_, :], pv[ps_, 4 + qc:5 + qc], 0.125, ALU.add, ALU.mult,
                   [b_pm, b_pv], [b_qz] if (qc == 0 and hh == 0) else (), cwrites=() if (qc == 0 and hh == 0) else [b_qz])
        if "sgu" in SKIP:
            for qc in range(4):
                P.op("pool", lambda e, qc=qc: e.memset(catT[:, qc, :], 0.0), writes=[b_cat[qc]])
        for qc in ([] if "sgu" in SKIP else range(4)):
            pgm, b_pgm = pmm.next()
            first = True
            for hh in range(2):
                h = 2 * qc + hh
                for i in range(4):
                    outap = pgm[hh * 64:(hh + 1) * 64, i * 128:(i + 1) * 128]
                    lhsT = vn[:, i, h * 64:(h + 1) * 64]
                    rhs = wsT[:, h, :]
                    if first:
                        P.op("pe", lambda e, o=outap, l=lhsT, r=rhs: e.matmul(out=o, lhsT=l, rhs=r, start=True, stop=True),
                             reads=[b_vn, b_wsT], writes=[b_pgm])
                        first = False
                    else:
                        P.op("pe", lambda e, o=outap, l=lhsT, r=rhs: e.matmul(out=o, lhsT=l, rhs=r, start=True, stop=True),
                             reads=[b_vn, b_wsT], cwrites=[b_pgm])
            tt(P, "dve", gt[:].rearrange("p (a b) -> p a b", a=4), pgm[:].rearrange("p (a b) -> p a b", a=4),
               wsb[:, qc, :].unsqueeze(1).to_broadcast([128, 4, 128]), ALU.add, [b_pgm, b_wsb], [b_gt])
            pm, b_pm = inproj_fm(qc)
            stt(P, catT[:, qc, :], pm[:], pv[:, qc:qc + 1], gt[:], ALU.add, ALU.mult, [b_pm, b_pv, b_gt], [b_cat[qc]])
        if "attn" in SKIP:
            for qc in range(4):
                P.op("pool", lambda e, qc=qc: e.memset(catT[:, 4 + qc, :], 0.0), writes=[b_cat[4 + qc]])
        for pr in ([] if "attn" in SKIP else range(4)):
            t0 = jj * 4 + pr
            slots = [s_ for s_ in range(5) if t0 - 4 + s_ >= 0]
            kbufs = list({b_kT[(t0 - 4 + s_) // 4] for s_ in slots})
            vbufs = list({b_Va[(t0 - 4 + s_) // 4] for s_ in slots})
            lo = slots[0]
            pend = []

            def flush_pv():
                while pend:
                    pend.pop(0)()

            for h in range(8):
                qc = h // 2
                (pa, b_pa), (pb, b_pb) = spairs.next()
                firstA = True
                for s_ in slots:
                    tk = t0 - 4 + s_
                    lhsT = kT[:, qc, tk * 128:(tk + 1) * 128]
                    rhs = qz[:, h, pr * 128:(pr + 1) * 128]
                    if s_ < 4:
                        outap, bb = pa[:, s_ * 128:(s_ + 1) * 128], b_pa
                        fw = firstA
                        firstA = False
                    else:
                        outap, bb = pb[:, 0:128], b_pb
                        fw = True
                    P.op("pe", lambda e, o=outap, l=lhsT, r=rhs: e.matmul(out=o, lhsT=l, rhs=r, start=True, stop=False),
                         reads=kbufs + [b_qz], writes=[bb] if fw else (), cwrites=() if fw else [bb])
                    P.op("pe", lambda e, o=outap, r=relP[:, h, s_ * 128:(s_ + 1) * 128]:
                         e.matmul(out=o, lhsT=g.ident_b[:], rhs=r, start=False, stop=True),
                         reads=[g.b_ident_b, b_relP], cwrites=[bb])
                pTt, b_pT = pT.next()
                if lo < 4:
                    act(P, pTt[:, lo * 128:512], pa[:, lo * 128:512], AF.Exp, [b_pa], [b_pT])
                    act(P, pTt[:, 512:640], pb[:, 0:128], AF.Exp, [b_pb], (), cwrites=[b_pT])
                else:
                    act(P, pTt[:, 512:640], pb[:, 0:128], AF.Exp, [b_pb], [b_pT])
                if ATT_T:
                    def pv_fn(h=h, qc=qc, pTt=pTt, b_pT=b_pT):
                        po = (h % 2) * 64
                        (pN, b_pN), (pD, b_pD) = pO[0], pO[1]
                        for which, (pX, b_pX) in enumerate(((pN, b_pN), (pD, b_pD))):
                            for oi, s_ in enumerate(slots):
                                tk = t0 - 4 + s_
                                lhsT = Va[:, tk, h, 0:64] if which == 0 else g.ones_b[:, 0:64]
                                fw = (h == 0 and oi == 0)
                                P.op("pe", lambda e, o=pX[po:po + 64, qc * 128:(qc + 1) * 128], l=lhsT,
                                     r=pTt[:, s_ * 128:(s_ + 1) * 128], st=(oi == 0), sp=(oi == len(slots) - 1):
                                     e.matmul(out=o, lhsT=l, rhs=r, start=st, stop=sp),
                                     reads=(vbufs if which == 0 else [g.b_ones]) + [b_pT],
                                     writes=[b_pX] if fw else (), cwrites=() if fw else [b_pX])
                    flush_pv()
                    pend.append(pv_fn)
                    if h == 7:
                        flush_pv()
                    continue
                pOt, b_pO = pO[h // 4]
                c0 = (h % 4) * 66
                if "pv" in SKIP:
                    continue
                for oi, s_ in enumerate(slots):
                    tk = t0 - 4 + s_
                    fw = (h % 4 == 0 and oi == 0)
                    P.op("pe", lambda e, o=pOt[:, c0:c0 + 66], l=pTt[:, s_ * 128:(s_ + 1) * 128], r=Va[:, tk, h, :],
                         st=(oi == 0), sp=(oi == len(slots) - 1): e.matmul(out=o, lhsT=l, rhs=r, start=st, stop=sp),
                         reads=vbufs + [b_pT], writes=[b_pO] if fw else (), cwrites=() if fw else [b_pO])
                if h % 4 == 3 and "norm" not in SKIP:
                    hb = h // 4
                    cp(P, "act", osb[:], pOt[:, 0:264], [b_pO], [b_osb])
                    ov3 = osb[:].rearrange("p (h c) -> p h c", c=66)
                    P.op("dve", lambda e, ov3=ov3, hb=hb: e.reciprocal(out=rcp[:, hb * 4:(hb + 1) * 4], in_=ov3[:, :, 64]),
                         reads=[b_osb], writes=[b_rcp])
                    tt(P, "dve", otok[:, hb * 256:(hb + 1) * 256].rearrange("p (h c) -> p h c", c=64), ov3[:, :, 0:64],
                       rcp[:, hb * 4:(hb + 1) * 4].unsqueeze(2).to_broadcast([128, 4, 64]), ALU.mult, [b_osb, b_rcp],
                       [b_otok] if hb == 0 else (), cwrites=() if hb == 0 else [b_otok])
            if ATT_T:
                (pN, b_pN), (pD, b_pD) = pO[0], pO[1]
                P.op("dve", lambda e, pD=pD: e.reciprocal(out=rden[:], in_=pD[:]), reads=[b_pD], writes=[b_rden])
                for qc in range(4):
                    tt(P, "dve", catT[:, 4 + qc, pr * 128:(pr + 1) * 128], pN[:, qc * 128:(qc + 1) * 128],
                       rden[:, qc * 128:(qc + 1) * 128], ALU.mult, [b_pN, b_rden],
                       [b_cat[4 + qc]] if pr == 0 else (), cwrites=() if pr == 0 else [b_cat[4 + qc]])
                continue
            if "pv" in SKIP or "norm" in SKIP:
                if pr == 0:
                    for qc in range(4):
                        P.op("pool", lambda e, qc=qc: e.memset(catT[:, 4 + qc, :], 0.0), writes=[b_cat[4 + qc]])
                continue
            pt, b_pt = ptr.next()
            for qc in range(4):
                tr(P, pt[:, qc * 128:(qc + 1) * 128], otok[:, qc * 128:(qc + 1) * 128], g.ident_f[:], qc == 0,
                   [b_otok, g.b_ident_f], b_pt)
            for qc in range(4):
                cp(P, "act" if qc % 2 == 0 else "dve", catT[:, 4 + qc, pr * 128:(pr + 1) * 128], pt[:, qc * 128:(qc + 1) * 128],
                   [b_pt], [b_cat[4 + qc]] if pr == 0 else (), cwrites=() if pr == 0 else [b_cat[4 + qc]])
        emit_tail_block(P, g, t, xblk, b_xt, catT, b_cat, wout, b_wout, pmm, gb, dr)
    P.end_phase()

def phase_e(P, g, dr, L):
    P.begin_phase()
    NI = CAP // 128
    NW = 3
    sets = []
    for i in range(NW):
        sets.append((P.sbb([128, 8, DEX], BF16, "wg"), P.sbb([128, 8, DEX], BF16, "wu"), P.sbb([128, 4, D], BF16, "wd"),
                     P.sbb([128, NI, D], BF16, "xe")))
    xeT, b_xeT = P.sbb([128, 8, CAP], BF16, "xeT")
    sgt = Ring([P.sbb([128, CAP], F32, "sgt") for _ in range(2)])
    hT, b_hT = P.sbb([128, 4, CAP], BF16, "hT")
    yt = Ring([P.sbb([128, D], F32, "yt") for _ in range(3)])
    ptb = Ring([P.psb([128, 512], BF16, "ptb") for _ in range(2)])
    pg = Ring([P.psb([128, 512], F32, "pg") for _ in range(2)])
    pu = Ring([P.psb([128, 512], F32, "pu") for _ in range(2)])
    py = Ring([P.psb([128, 512], F32, "py") for _ in range(2)])
    Wg, Wu, Wd = dr["moe_w_gate"], dr["moe_w_up"], dr["moe_w_down"]
    Xs, Ys = dr["Xs"], dr["Ys"]

    def issue(ex):
        s_ = ex % NW
        (wgt, b_wg), (wut, b_wu), (wdt, b_wd), (xet, b_xe) = sets[s_]
        P.dma("sp", xet[:], Xs[ex * CAP:(ex + 1) * CAP, :].rearrange("(i p) d -> p i d", p=128),
              reads=[g.b_Xs], writes=[b_xe], key="xe%d" % s_)
        load_weight_bf16(P, wgt, b_wg, Wg[L, ex], 8, DEX, "wg%d" % s_)
        load_weight_bf16(P, wut, b_wu, Wu[L, ex], 8, DEX, "wu%d" % s_)
        load_weight_bf16(P, wdt, b_wd, Wd[L, ex], 4, D, "wd%d" % s_)

    for ex in range(min(NW - 1, NE)):
        issue(ex)
    for ex in range(NE):
        if ex + NW - 1 < NE:
            issue(ex + NW - 1)
        (wgt, b_wg), (wut, b_wu), (wdt, b_wd), (xet, b_xe) = sets[ex % NW]
        for c in range(8):
            pt, b_pt = ptb.next()
            for i in range(NI):
                tr(P, pt[:, i * 128:(i + 1) * 128], xet[:, i, c * 128:(c + 1) * 128], g.ident_b[:], i == 0,
                   [b_xe, g.b_ident_b], b_pt)
            cp(P, "act" if c % 2 == 0 else "dve", xeT[:, c, :], pt[:, 0:CAP], [b_pt], [b_xeT] if c == 0 else (),
               cwrites=() if c == 0 else [b_xeT])
        for m in range(4):
            pgt, b_pg = pg.next()
            put, b_pu = pu.next()
            for c in range(8):
                mm(P, pgt[:, 0:CAP], wgt[:, c, m * 128:(m + 1) * 128], xeT[:, c, :], c == 0, c == 7, [b_wg, b_xeT], b_pg)
            for c in range(8):
                mm(P, put[:, 0:CAP], wut[:, c, m * 128:(m + 1) * 128], xeT[:, c, :], c == 0, c == 7, [b_wu, b_xeT], b_pu)
            sg, b_sg = sgt.next()
            act(P, sg[:], pgt[:, 0:CAP], AF.Silu, [b_pg], [b_sg])
            tt(P, "dve", hT[:, m, :], sg[:], put[:, 0:CAP], ALU.mult, [b_sg, b_pu], [b_hT] if m == 0 else (),
               cwrites=() if m == 0 else [b_hT])
        for i in range(NI):
            ytile, b_yt = yt.next()
            for h in range(2):
                pyt, b_py = py.next()
                for m in range(4):
                    mm(P, pyt[:], hT[:, m, i * 128:(i + 1) * 128], wdt[:, m, h * 512:(h + 1) * 512], m == 0, m == 3,
                       [b_hT, b_wd], b_py)
                cp(P, "act" if h == 0 else "dve", ytile[:, h * 512:(h + 1) * 512], pyt[:], [b_py],
                   [b_yt] if h == 0 else (), cwrites=() if h == 0 else [b_yt])
            r0 = ex * CAP + i * 128
            P.dma("sp", Ys[r0:r0 + 128, :], ytile[:], reads=[b_yt], cwrites=[g.b_Ys], key="yst%d" % (yt.i % 3))
    P.end_phase()


def phase_c(P, g, dr, L, out_ap, b_out):
    P.begin_phase()
    t = G()
    t.bc, t.b_bc = P.sbb([128, 2 * D], F32, "bc")
    P.dma("sp", t.bc[:], dr["bcf%d" % L][:, :], writes=[t.b_bc], key="cst")
    t.lnr = Ring([make_ln_scratch(P) for _ in range(3)])
    NR = 4
    x1r = [P.sbb([128, D], F32, "x1c") for _ in range(NR)]
    y0r = [P.sbb([128, D], F32, "y0") for _ in range(NR)]
    y1r = [P.sbb([128, D], F32, "y1") for _ in range(NR)]
    rr = Ring([P.sbb([128, D], F32, "rc") for _ in range(2)])
    orr = Ring([P.sbb([128, D], F32, "oc") for _ in range(3)])
    Ys = dr["Ys"]
    for (yy, b_yy) in y0r + y1r:
        P.op("pool", lambda e, yy=yy: e.memset(yy[:], 0.0), writes=[b_yy])

    def issue_loads(tile):
        s_ = tile % NR
        x1, b_x1 = x1r[s_]
        P.dma("sp", x1[:], dr["X1"][tile * 128:(tile + 1) * 128, :], reads=[g.b_X1], writes=[b_x1], key="cx%d" % s_)
        for k, (yy, b_yy) in enumerate((y0r[s_], y1r[s_])):
            P.op("pool", lambda e, yy=yy, k=k, tile=tile: e.indirect_dma_start(
                out=yy[:], out_offset=None, in_=Ys[:, :],
                in_offset=bass.IndirectOffsetOnAxis(ap=g.posi[:, tile, k:k + 1], axis=0),
                bounds_check=P.bound_reg(e), oob_is_err=False),
                reads=[g.b_Ys, g.b_posi], writes=[b_yy], key="cy%d_%d" % (k, s_))

    NPRE = 3
    for tile in range(min(NPRE, NT)):
        issue_loads(tile)
    for tile in range(NT):
        if tile + NPRE < NT:
            issue_loads(tile + NPRE)
        s_ = tile % NR
        x1, b_x1 = x1r[s_]
        y0, b_y0 = y0r[s_]
        y1, b_y1 = y1r[s_]
        r, b_r = rr.next()
        act(P, r[:], x1[:], AF.Copy, [b_x1], [b_r], scale=ALPHA)
        stt(P, r[:], y0[:], g.wts[:, tile, 0:1], r[:], ALU.mult, ALU.add, [b_y0, g.b_wts, b_r], [b_r])
        stt(P, r[:], y1[:], g.wts[:, tile, 1:2], r[:], ALU.mult, ALU.add, [b_y1, g.b_wts, b_r], [b_r])
        o, b_o = orr.next()
        emit_ln_rows(P, t, r[:], b_r, o[:], b_o, 0, D)
        P.dma("sp", out_ap[tile * 128:(tile + 1) * 128, :], o[:], reads=[b_o], cwrites=[b_out], key="co%d" % (orr.i % 3))
    P.end_phase()


def build_program(n_layers=1, debug=False):
    nc = bass.Bass("TRN2", target_bir_lowering=False)
    dr = {}

    def din(name, shape, dt=F32):
        dr[name] = nc.dram_tensor(name, list(shape), dt, kind="ExternalInput").ap()

    din("x", [T, D])
    din("ab_w_in", [D, 2560])
    din("ab_w_out", [D, D])
    din("pv0", [128, 168])
    din("cd_w_in", [D, 2560])
    din("cd_w_out", [D, D])
    din("pv1", [128, 12])
    din("bc1x", [128, 2048])
    din("wsT", [128, 1024])
    din("wsb", [128, 512])
    din("relP", [128, 8 * 640])
    for L in range(2):
        din("bcm%d" % L, [128, 2 * D + 36])
        din("bcf%d" % L, [128, 2 * D])
        din("wr%d" % L, [D, 36])
    din("moe_w_gate", [2, NE, D, DEX])
    din("moe_w_up", [2, NE, D, DEX])
    din("moe_w_down", [2, NE, DEX, D])
    dr["out"] = nc.dram_tensor("out", [T, D], F32, kind="ExternalOutput").ap()
    kind = "ExternalOutput" if debug else "Internal"
    dr["X1"] = nc.dram_tensor("X1", [T, D], F32, kind=kind).ap()
    dr["Xs"] = nc.dram_tensor("Xs", [NSLOT, D], BF16, kind=kind).ap()
    dr["Ys"] = nc.dram_tensor("Ys", [NSLOT, D], F32, kind=kind).ap()
    dr["X2"] = nc.dram_tensor("X2", [T, D], F32, kind="Internal").ap()
    P = Prog(nc)
    g = G()
    setup_globals(P, g)
    g.b_X1 = P.buf("X1")
    g.b_Xs = P.buf("Xs")
    g.b_Ys = P.buf("Ys")
    g.b_X2 = P.buf("X2")
    g.b_out = P.buf("out")
    phase_m0(P, g, dr)
    phase_e(P, g, dr, 0)
    if n_layers == 1:
        phase_c(P, g, dr, 0, dr["out"], g.b_out)
    else:
        phase_c(P, g, dr, 0, dr["X2"], g.b_X2)
        phase_m1(P, g, dr)
        phase_e(P, g, dr, 1)
        phase_c(P, g, dr, 1, dr["out"], g.b_out)
    if debug:
        dr["posd"] = nc.dram_tensor("posd", [128, NT * 2], I32, kind="ExternalOutput").ap()
        dr["wtsd"] = nc.dram_tensor("wtsd", [128, NT * 2], F32, kind="ExternalOutput").ap()
        P.begin_phase()
        P.dma("sp", dr["posd"][:, :], g.posi[:].rearrange("p a b -> p (a b)"), reads=[g.b_posi], key="dbg")
        P.dma("sp", dr["wtsd"][:, :], g.wts[:].rearrange("p a b -> p (a b)"), reads=[g.b_wts], key="dbg")
        P.end_phase()
    P.finish()
    return nc, P


def prep_inputs(inp):
    f = lambda a: np.ascontiguousarray(np.asarray(a, dtype=np.float32))
    shared = {}
    shared["ab_w_in"] = f(inp["ab_w_in"][0])
    shared["ab_w_out"] = f(inp["ab_w_out"][0])
    pv0 = np.zeros((128, 168), np.float32)
    pv0[:, 0:20] = f(inp["ab_b_in"][0]).reshape(20, 128).T
    adw = f(inp["a_dw"][0])
    pv0[:, 20:144] = adw.reshape(31, 4, 128).transpose(2, 1, 0).reshape(128, 124)
    pv0[:, 144:148] = f(inp["a_dw_b"][0]).reshape(4, 128).T
    pv0[:, 148:152] = f(inp["a_ln_g"][0]).reshape(4, 128).T
    pv0[:, 152:156] = f(inp["a_ln_b"][0]).reshape(4, 128).T
    bdw = f(inp["b_dw"][0])
    pv0[:, 156:168] = bdw.reshape(3, 4, 128).transpose(2, 1, 0).reshape(128, 12)
    shared["pv0"] = pv0
    shared["cd_w_in"] = f(inp["cd_w_in"][0])
    shared["cd_w_out"] = f(inp["cd_w_out"][0])
    cb = f(inp["cd_b_in"][0])
    pv1 = np.zeros((128, 12), np.float32)
    pv1[:, 0:4] = cb[0:512].reshape(4, 128).T
    pv1[:, 4:8] = cb[1024:1536].reshape(4, 128).T
    pv1[:, 8:12] = cb[1536:2048].reshape(4, 128).T
    shared["pv1"] = pv1
    row = np.concatenate([cb[512:1024], f(inp["c_ln_g"][0]), f(inp["c_ln_b"][0]), cb[2048:2560]])
    shared["bc1x"] = np.ascontiguousarray(np.broadcast_to(row[None, :], (128, 2048)))
    ws = f(inp["c_ws"][0])
    shared["wsT"] = np.ascontiguousarray(ws.transpose(2, 0, 1).reshape(128, 1024))
    wb = f(inp["c_ws_b"][0])
    shared["wsb"] = np.ascontiguousarray(wb.reshape(4, 2, 1, 128).repeat(64, axis=2).transpose(1, 2, 0, 3).reshape(128, 512))
    rb = f(inp["d_rel_bias"][0])
    p_ = np.arange(128)[:, None, None]
    s_ = np.arange(5)[None, :, None]
    c_ = np.arange(128)[None, None, :]
    par = c_ // 64
    i_ = c_ % 64
    delta = 64 * (8 + par - 2 * s_) + i_ - p_
    ridx = np.clip(delta, -256, 256) + 256
    cdist = 8 - 2 * s_ + par - (p_ // 64)
    valid = (cdist >= 0) & (cdist <= 8)
    relv = np.where(valid[None], rb[:, ridx], np.float32(-30000.0)).astype(np.float32)
    shared["relP"] = np.ascontiguousarray(relv.transpose(1, 0, 2, 3).reshape(128, 8 * 640))
    for L in range(2):
        row = np.concatenate([f(inp["mix_ln_g"][L]), f(inp["mix_ln_b"][L]), f(inp["moe_rg_b"][L]), f(inp["moe_re_b"][L]).reshape(-1)])
        shared["bcm%d" % L] = np.ascontiguousarray(np.broadcast_to(row[None, :], (128, row.size)))
        row = np.concatenate([f(inp["ffn_ln_g"][L]), f(inp["ffn_ln_b"][L])])
        shared["bcf%d" % L] = np.ascontiguousarray(np.broadcast_to(row[None, :], (128, row.size)))
        shared["wr%d" % L] = np.ascontiguousarray(np.concatenate(
            [f(inp["moe_rg_w"][L]), f(inp["moe_re_w"][L]).transpose(1, 0, 2).reshape(D, 32)], axis=1))
    shared["moe_w_gate"] = f(inp["moe_w_gate"])
    shared["moe_w_up"] = f(inp["moe_w_up"])
    shared["moe_w_down"] = f(inp["moe_w_down"])
    x = f(inp["x"]).reshape(NCORES, T, D)
    return shared, x


_CACHE = {}


def kernel(**inputs):
    shared, x = prep_inputs(inputs)
    if "nc" not in _CACHE:
        _CACHE["nc"] = build_program(n_layers=2)[0]
    nc = _CACHE["nc"]
    in_maps = []
    for c in range(NCORES):
        m = dict(shared)
        m["x"] = x[c]
        in_maps.append(m)
    res = run_bass_kernel_spmd(nc, in_maps, core_ids=list(range(NCORES)))
    out = np.stack([np.asarray(r["out"]) for r in res.results], 0)
    return out.reshape(16, SEQ, D).astype(np.float32)
```
